# Optimizing a Trainium2 kernel written in Bass

```python
import jax
import jax.numpy as jnp
from jax import lax
import numpy as np

D_MODEL = 1024
BATCH = 32
SEQ = 2048
DEPTH = 1

CTX_LEN = 256
GRID_W = 64

POOL_WINDOWS = (2, 4, 8, 16)
POOL_GROUPS = len(POOL_WINDOWS)
POOL_GROUP_DIM = 128
POOL_WIDTH = POOL_GROUPS * POOL_GROUP_DIM

GLA_HEADS = 4
GLA_DK = D_MODEL // 2
GLA_DV = D_MODEL
GLA_HK = GLA_DK // GLA_HEADS
GLA_HV = GLA_DV // GLA_HEADS
GLA_GATE_RANK = 16
GLA_GATE_NORMALIZER = 16.0
GLA_CHUNK = 64

N_BRANCHES = 2
IN_WIDTHS = (POOL_WIDTH, GLA_DK, GLA_DK, GLA_DV, GLA_DV, N_BRANCHES * D_MODEL, 2 * GLA_GATE_RANK)
IN_SPLITS = tuple(int(s) for s in np.cumsum(IN_WIDTHS)[:-1])
D_IN = int(sum(IN_WIDTHS))

N_GROUPS = 4
EXPERTS_PER_GROUP = 8
N_EXPERTS = N_GROUPS * EXPERTS_PER_GROUP
TOP_K_IN_GROUP = 2
D_EXPERT = D_MODEL // 2

MOD_CHUNKS = 6
EPS = 1e-6

kernel_name = 'hybrid_pool_gla_hmoe_dit_block'


def _rmsnorm(x, g):
    xf = x.astype(jnp.float32)
    y = xf * lax.rsqrt(jnp.mean(xf * xf, axis=-1, keepdims=True) + EPS)
    return (y * g.astype(jnp.float32)).astype(x.dtype)


def _adaln(cond, w_mod, b_mod):
    return jnp.split(jnp.matmul(jax.nn.silu(cond), w_mod) + b_mod, MOD_CHUNKS, axis=-1)


def _heads(t, hd):
    b, l, _ = t.shape
    return t.reshape(b, l, GLA_HEADS, hd).transpose(0, 2, 1, 3)


def _flip(t):
    return jnp.flip(t, axis=2)


def _project(h, w_in, a2_f, ab_f, a2_b, ab_b):
    p = jnp.einsum('bld,de->ble', h, w_in)
    u, q, k, v, g, gates, lr = jnp.split(p, IN_SPLITS, axis=-1)
    lr_f, lr_b = jnp.split(lr, 2, axis=-1)
    la_f = jax.nn.log_sigmoid(jnp.matmul(lr_f, a2_f).astype(jnp.float32) + ab_f.astype(jnp.float32)) / GLA_GATE_NORMALIZER
    la_b = jax.nn.log_sigmoid(jnp.matmul(lr_b, a2_b).astype(jnp.float32) + ab_b.astype(jnp.float32)) / GLA_GATE_NORMALIZER
    return (u, _heads(q, GLA_HK) * (GLA_HK ** -0.5), _heads(k, GLA_HK), _heads(v, GLA_HV),
            _heads(la_f, GLA_HK), _heads(la_b, GLA_HK), g, gates)


def _centred_pool_minus_self(u):
    n, l, _ = u.shape
    uf = u.astype(jnp.float32)
    cs = jnp.concatenate([jnp.zeros((n, 1, POOL_WIDTH), jnp.float32), jnp.cumsum(uf, axis=1)], axis=1)
    t = np.arange(l)
    means = []
    for gi, w in enumerate(POOL_WINDOWS):
        lo = np.clip(t - w // 2, 0, l)
        hi = np.clip(t + w // 2, 0, l)
        cg = cs[..., gi * POOL_GROUP_DIM:(gi + 1) * POOL_GROUP_DIM]
        cnt = jnp.asarray((hi - lo)[:, None], jnp.float32)
        means.append((cg[:, hi] - cg[:, lo]) / cnt)
    return (jnp.concatenate(means, axis=-1) - uf).astype(u.dtype)


def _gla_chunked(q, k, v, log_a, s0):
    b, h, l, _ = q.shape
    dv = v.shape[-1]
    n = l // GLA_CHUNK

    def chunks(t):
        return t.astype(jnp.float32).reshape(b, h, n, GLA_CHUNK, t.shape[-1])

    qc, kc, vc, la = chunks(q), chunks(k), chunks(v), chunks(log_a)
    cum = jnp.cumsum(la, axis=3)
    last = cum[:, :, :, -1:, :]
    q_dec = qc * jnp.exp(cum)
    k_inv = kc * jnp.exp(-cum)
    k_rem = kc * jnp.exp(last - cum)
    lower = np.tril(np.ones((GLA_CHUNK, GLA_CHUNK), dtype=bool))
    scores = jnp.where(lower, jnp.einsum('bhnid,bhnjd->bhnij', q_dec, k_inv), 0.0)
    o_intra = jnp.einsum('bhnij,bhnjv->bhniv', scores, vc)
    decay = jnp.exp(last[:, :, :, 0, :])

    def step(state, inp):
        q_n, k_n, v_n, d_n = inp
        o_n = jnp.einsum('bhid,bhdv->bhiv', q_n, state)
        state = d_n[..., None] * state + jnp.einsum('bhid,bhiv->bhdv', k_n, v_n)
        return state, o_n

    xs = tuple(jnp.moveaxis(t, 2, 0) for t in (q_dec, k_rem, vc, decay))
    s_final, o_inter = lax.scan(step, s0, xs)
    o = o_intra + jnp.moveaxis(o_inter, 0, 2)
    return o.reshape(b, h, l, dv).astype(v.dtype), s_final


def _gla_final_state(k, v, log_a):
    cum = jnp.cumsum(log_a, axis=2)
    k_rem = k.astype(jnp.float32) * jnp.exp(cum[:, :, -1:, :] - cum)
    return jnp.einsum('bhtd,bhtv->bhdv', k_rem, v.astype(jnp.float32))


def _gla_bidirectional(q, k, v, la_f, la_b, s0_f, s0_b):
    o_f, _ = _gla_chunked(q, k, v, la_f, s0_f)
    o_b, _ = _gla_chunked(_flip(q), _flip(k), _flip(v), _flip(la_b), s0_b)
    return o_f + _flip(o_b)


def _mixer_out(pooled, o, g, gates, pool_w, pool_scale, gla_onorm_g, w_pool_br, w_gla_br, w_o):
    b, l, _ = pooled.shape
    y_pool = jnp.einsum('blgc,gce->blge', pooled.reshape(b, l, POOL_GROUPS, POOL_GROUP_DIM), pool_w)
    y_pool = jnp.matmul(y_pool.reshape(b, l, POOL_WIDTH) * pool_scale, w_pool_br)
    o = _rmsnorm(o, gla_onorm_g).transpose(0, 2, 1, 3).reshape(b, l, GLA_DV)
    y_gla = jnp.matmul(o * jax.nn.silu(g), w_gla_br)
    gate_pool, gate_gla = jnp.split(jax.nn.sigmoid(gates), N_BRANCHES, axis=-1)
    return jnp.matmul(gate_pool * y_pool + gate_gla * y_gla, w_o)


def _hier_moe(h, rg_w, rg_b, re_w, re_b, w1, w3, w2):
    b, l, d = h.shape
    t = h.reshape(b * l, d)
    grp_logits = (jnp.matmul(t, rg_w) + rg_b).astype(jnp.float32)
    grp_top, grp_idx = lax.top_k(grp_logits, 1)
    p_grp = jnp.exp(grp_top - jax.nn.logsumexp(grp_logits, axis=-1, keepdims=True))
    exp_logits = (jnp.matmul(t, re_w) + re_b).astype(jnp.float32).reshape(-1, N_GROUPS, EXPERTS_PER_GROUP)
    in_grp = jnp.einsum('tg,tge->te', jax.nn.one_hot(grp_idx[:, 0], N_GROUPS, dtype=jnp.float32), exp_logits)
    top_v, top_i = lax.top_k(in_grp, TOP_K_IN_GROUP)
    w = jax.nn.softmax(top_v, axis=-1) * p_grp
    expert_idx = grp_idx * EXPERTS_PER_GROUP + top_i
    combine = jnp.einsum('tk,tke->te', w, jax.nn.one_hot(expert_idx, N_EXPERTS, dtype=jnp.float32)).astype(h.dtype)
    out = jnp.zeros_like(t)
    for e in range(N_EXPERTS):
        hid = jax.nn.silu(jnp.matmul(t, w1[e])) * jnp.matmul(t, w3[e])
        out = out + combine[:, e:e + 1] * jnp.matmul(hid, w2[e])
    return out.reshape(b, l, d)


def setup_inputs(seed: int = 0) -> dict:
    key = jax.random.key(seed)
    ks = jax.random.split(key, 32)
    d = D_MODEL

    def nrm(k, shape, scale):
        return jax.random.normal(k, shape, jnp.float32) * scale

    return {
        'x': nrm(ks[0], (BATCH, SEQ, d), 1.0),
        'c': nrm(ks[1], (BATCH, d), 1.0),
        'ctx': nrm(ks[2], (BATCH, CTX_LEN, d), 1.0),
        'c_ctx': nrm(ks[3], (d,), 1.0),
        'w_mod': nrm(ks[4], (DEPTH, d, MOD_CHUNKS * d), 0.5 * d ** -0.5),
        'b_mod': nrm(ks[5], (DEPTH, MOD_CHUNKS * d), 0.02),
        'norm1_g': 1.0 + nrm(ks[6], (DEPTH, d), 0.05),
        'norm2_g': 1.0 + nrm(ks[7], (DEPTH, d), 0.05),
        'w_in': nrm(ks[8], (DEPTH, d, D_IN), d ** -0.5),
        'gla_a2_f': nrm(ks[9], (DEPTH, GLA_GATE_RANK, GLA_DK), GLA_GATE_RANK ** -0.5),
        'gla_ab_f': nrm(ks[10], (DEPTH, GLA_DK), 0.5),
        'gla_a2_b': nrm(ks[11], (DEPTH, GLA_GATE_RANK, GLA_DK), GLA_GATE_RANK ** -0.5),
        'gla_ab_b': nrm(ks[12], (DEPTH, GLA_DK), 0.5),
        'gla_onorm_g': 1.0 + nrm(ks[13], (DEPTH, GLA_HV), 0.05),
        'pool_w': nrm(ks[14], (DEPTH, POOL_GROUPS, POOL_GROUP_DIM, POOL_GROUP_DIM), POOL_GROUP_DIM ** -0.5),
        'pool_scale': 1.0 + nrm(ks[15], (DEPTH, POOL_WIDTH), 0.1),
        'w_pool_br': nrm(ks[16], (DEPTH, POOL_WIDTH, d), POOL_WIDTH ** -0.5),
        'w_gla_br': nrm(ks[17], (DEPTH, GLA_DV, d), GLA_DV ** -0.5),
        'w_o': nrm(ks[18], (DEPTH, d, d), d ** -0.5),
        'router_grp_w': nrm(ks[19], (DEPTH, d, N_GROUPS), d ** -0.5),
        'router_grp_b': nrm(ks[20], (DEPTH, N_GROUPS), 0.01),
        'router_exp_w': nrm(ks[21], (DEPTH, d, N_EXPERTS), d ** -0.5),
        'router_exp_b': nrm(ks[22], (DEPTH, N_EXPERTS), 0.01),
        'moe_w1': nrm(ks[23], (DEPTH, N_EXPERTS, d, D_EXPERT), d ** -0.5),
        'moe_w3': nrm(ks[24], (DEPTH, N_EXPERTS, d, D_EXPERT), d ** -0.5),
        'moe_w2': nrm(ks[25], (DEPTH, N_EXPERTS, D_EXPERT, d), D_EXPERT ** -0.5),
        'final_norm_g': 1.0 + nrm(ks[26], (d,), 0.05),
    }


def reference(x, c, ctx, c_ctx, w_mod, b_mod, norm1_g, norm2_g, w_in, gla_a2_f, gla_ab_f, gla_a2_b, gla_ab_b,
              gla_onorm_g, pool_w, pool_scale, w_pool_br, w_gla_br, w_o, router_grp_w, router_grp_b,
              router_exp_w, router_exp_b, moe_w1, moe_w3, moe_w2, final_norm_g):
    b, seq, d = x.shape
    rows = seq // GRID_W
    for i in range(DEPTH):
        proj_params = (w_in[i], gla_a2_f[i], gla_ab_f[i], gla_a2_b[i], gla_ab_b[i])
        mix_params = (pool_w[i], pool_scale[i], gla_onorm_g[i], w_pool_br[i], w_gla_br[i], w_o[i])
        moe_params = (router_grp_w[i], router_grp_b[i], router_exp_w[i], router_exp_b[i],
                      moe_w1[i], moe_w3[i], moe_w2[i])
        sh1, sc1, g1, sh2, sc2, g2 = [m[:, None, :] for m in _adaln(c, w_mod[i], b_mod[i])]
        csh1, csc1, cg1, csh2, csc2, cg2 = _adaln(c_ctx, w_mod[i], b_mod[i])

        hc = _rmsnorm(ctx, norm1_g[i]) * (1 + csc1) + csh1
        uc, qc, kc, vc, lfc, lbc, gc, gtc = _project(hc, *proj_params)
        st_f = _gla_final_state(kc, vc, lfc)
        st_b = _gla_final_state(_flip(kc), _flip(vc), _flip(lbc))

        hx = _rmsnorm(x, norm1_g[i]) * (1 + sc1) + sh1
        ux, qx, kx, vx, lfx, lbx, gx, gtx = _project(hx, *proj_params)
        pooled_x = _centred_pool_minus_self(ux.reshape(b * rows, GRID_W, POOL_WIDTH)).reshape(b, seq, POOL_WIDTH)
        ox = _gla_bidirectional(qx, kx, vx, lfx, lbx, st_f, st_b)
        x = x + g1 * _mixer_out(pooled_x, ox, gx, gtx, *mix_params)
        x = x + g2 * _hier_moe(_rmsnorm(x, norm2_g[i]) * (1 + sc2) + sh2, *moe_params)

        if i + 1 < DEPTH:
            zero_state = jnp.zeros_like(st_f)
            oc = _gla_bidirectional(qc, kc, vc, lfc, lbc, zero_state, zero_state)
            ctx = ctx + cg1 * _mixer_out(_centred_pool_minus_self(uc), oc, gc, gtc, *mix_params)
            ctx = ctx + cg2 * _hier_moe(_rmsnorm(ctx, norm2_g[i]) * (1 + csc2) + csh2, *moe_params)
    return _rmsnorm(x, final_norm_g)
```

```python
import contextlib
import os
import numpy as np
import concourse.bass as bass
import concourse.mybir as mybir
from concourse.bass_utils import run_bass_kernel_spmd

F32 = mybir.dt.float32
BF16 = mybir.dt.bfloat16
AF = mybir.ActivationFunctionType
ALU = mybir.AluOpType

D = 1024
SEQ = 2048
CTXL = 256
KD = 8
NXT = SEQ // 128
NCT = CTXL // 128
NTT = NXT + NCT
TOK = NTT * 128
D_IN = 5664
C_POOL, C_Q, C_K, C_V, C_G, C_GATES, C_LR = 0, 512, 1024, 1536, 2560, 3584, 5632
NHEAD = 4
HK = 128
HV = 256
NEXP_FULL = 32
DE = 512
EPS = 1e-6
NCORES = 8
STRICT_SAME = not os.environ.get("K_NOSTRICT")
POOLENG = "dve" if os.environ.get("K_NOPOOL") else "pool"


class Buf:
    __slots__ = ("lw", "rd", "excl")

    def __init__(self, excl=False):
        self.lw = None
        self.rd = {}
        self.excl = excl


def bufs(n):
    return [Buf() for _ in range(n)]


class DSem:
    def __init__(self, sem):
        self.sem = sem
        self.count = 0
        self.q = None
        self.maxw = 0


class Sched:
    def __init__(self, nc, es):
        self.nc = nc
        self.es = es
        self.E = {}
        for name, eng in (("pe", nc.tensor), ("act", nc.scalar), ("dve", nc.vector),
                          ("pool", nc.gpsimd), ("sp", nc.sync)):
            sem = es.enter_context(nc.semaphore("s_" + name))
            self.E[name] = dict(eng=eng, sem=sem, count=0, waited={})
        self.dsems = []
        self.nds = 0
        self.dsmap = {}
        self.dead = False

    def new_dsem(self):
        self.nds += 1
        ds = DSem(self.es.enter_context(self.nc.semaphore("d%d" % self.nds)))
        self.dsems.append(ds)
        self.dsmap[id(ds.sem)] = ds
        return ds

    def _wait(self, en, toks):
        E = self.E[en]
        for (sem, val, owner) in toks:
            if owner == en and (en == "pe" or not STRICT_SAME):
                continue
            k = id(sem)
            if k in self.dsmap and val > self.dsmap[k].maxw:
                self.dsmap[k].maxw = val
            if E["waited"].get(k, 0) >= val:
                continue
            E["eng"].wait_ge(sem, val)
            E["waited"][k] = val

    @staticmethod
    def _deps(r, w, en=None):
        toks = []
        for b in r:
            if b.lw:
                toks.append(b.lw)
            if b.excl:
                toks.extend(t for e_, t in b.rd.items() if e_ != en)
        for b in w:
            if b.lw:
                toks.append(b.lw)
            toks.extend(b.rd.values())
        return toks

    def op(self, en, fn, r=(), w=()):
        if self.dead:
            return
        self._wait(en, self._deps(r, w, en))
        E = self.E[en]
        ins = fn(E["eng"])
        E["count"] += 1
        ins.then_inc(E["sem"], 1)
        tok = (E["sem"], E["count"], en)
        for b in w:
            b.lw = tok
            b.rd = {}
        for b in r:
            b.rd[en] = tok

    def dma(self, qn, out, in_, ds, r=(), w=()):
        if self.dead:
            return
        self._wait(qn, self._deps(r, w))
        E = self.E[qn]
        assert ds.q in (None, qn), "DMA semaphore shared between queues"
        ds.q = qn
        if ds.maxw > 0:
            self._wait(qn, [(ds.sem, ds.maxw, None)])
        ins = E["eng"].dma_start(out=out, in_=in_)
        ds.count += 16
        ins.then_inc(ds.sem, 16)
        tok = (ds.sem, ds.count, None)
        for b in w:
            b.lw = tok
            b.rd = {}
        for b in r:
            b.rd["dma%d" % id(ds)] = tok

    def barrier(self):
        if self.dead:
            return
        for en, E in self.E.items():
            toks = []
            for en2, E2 in self.E.items():
                if en2 != en and E2["count"] > 0:
                    toks.append((E2["sem"], E2["count"], en2))
            for ds in self.dsems:
                if ds.count > 0:
                    toks.append((ds.sem, ds.count, None))
            self._wait(en, toks)


def pipeline(gens):
    it = iter(gens)
    active = []
    while True:
        g = next(it, None)
        if g is not None:
            active.append(g)
        if not active:
            break
        for g_ in list(active):
            try:
                next(g_)
            except StopIteration:
                active.remove(g_)


class _Stop(Exception):
    pass


def build_program(NB=4, NEXP=NEXP_FULL, debug=None, upto=99):
    nc = bass.Bass("TRN2", target_bir_lowering=False)

    def din(name, shape):
        return nc.dram_tensor(name, list(shape), F32, kind="ExternalInput").ap()

    x_d = din("x", (NB, SEQ, D))
    ctx_d = din("ctx", (NB, CTXL, D))
    cT_d = din("cT", (D, NB + 1))
    w_mod_d = din("w_mod", (D, 6 * D))
    b_mod_d = din("b_mod", (6 * D,))
    n1g_d = din("norm1_g", (D,))
    n2g_d = din("norm2_g", (D,))
    w_in_d = din("w_in", (D, D_IN))
    a2f_d = din("gla_a2_f", (16, 512))
    abf_d = din("gla_ab_f", (512,))
    a2b_d = din("gla_a2_b", (16, 512))
    abb_d = din("gla_ab_b", (512,))
    gn_d = din("gla_onorm_g", (HV,))
    pw_d = din("pool_w", (4, 128, 128))
    psc_d = din("pool_scale", (512,))
    wpb_d = din("w_pool_br", (512, D))
    wgb_d = din("w_gla_br", (D, D))
    wo_d = din("w_o", (D, D))
    rgw_d = din("router_grp_w", (D, 4))
    rgb_d = din("router_grp_b", (4,))
    rew_d = din("router_exp_w", (D, 32))
    reb_d = din("router_exp_b", (32,))
    w1_d = din("moe_w1", (NEXP_FULL, D, DE))
    w3_d = din("moe_w3", (NEXP_FULL, D, DE))
    w2_d = din("moe_w2", (NEXP_FULL, DE, D))
    fg_d = din("final_norm_g", (D,))
    cst_d = din("cst", (128, 898))
    poolP_d = din("poolP", (128, 512))
    sel_d = din("sel", (8, 8 * 128))
    out_d = nc.dram_tensor("out", [NB, SEQ, D], F32, kind="ExternalOutput").ap()
    dbg_d = None
    if debug:
        dbg_d = nc.dram_tensor("dbg", [128, debug], F32, kind="ExternalOutput").ap()

    NBC = NB + 1

    with contextlib.ExitStack() as es:
        S = Sched(nc, es)

        uid = [0]

        def sb(es_, name, shape, dt=F32):
            uid[0] += 1
            return es_.enter_context(nc.sbuf_tensor("%s_s%d" % (name, uid[0]), list(shape), dt))

        P = [es.enter_context(nc.psum_tensor("P%d" % i, [128, 512], F32)) for i in range(7)]
        PB = es.enter_context(nc.psum_tensor("PB", [128, 1024], BF16))
        Pb = [Buf(excl=True) for _ in range(7)]
        PBb = Buf(excl=True)

        cst = sb(es, "cst", (128, 898))
        identb = sb(es, "identb", (128, 128), BF16)
        poolP = sb(es, "poolP", (128, 512), BF16)
        sel = sb(es, "sel", (8, 1024))
        vstage = sb(es, "vstage", (68, 128))
        vT = sb(es, "vT", (128, 68))
        gnb = sb(es, "gnb", (128, HV))
        rbb = sb(es, "rbb", (128, 36))
        a2pf = sb(es, "a2pf", (32, 512), BF16)
        a2pb = sb(es, "a2pb", (32, 512), BF16)
        wlr = sb(es, "wlr", (128, KD, 32), BF16)
        rw = sb(es, "rw", (128, KD, 36), BF16)
        pwt = sb(es, "pwt", (128, 4, 128), BF16)
        sct = sb(es, "sct", (128, KD, NBC))
        modT = sb(es, "modT", (128, NBC, 48))
        msb = sb(es, "msb", (NBC, 2, D))
        A1 = sb(es, "A1", (128, NBC, KD))
        A2 = sb(es, "A2", (128, NBC, KD))
        cb = Buf()
        dsc = S.new_dsem()
        dscp = S.new_dsem()
        xid = [S.new_dsem() for _ in range(3)]
        wsem = S.new_dsem()
        wgsem = S.new_dsem()
        w2sem = S.new_dsem()
        dsl = S.new_dsem()
        dbgsem = S.new_dsem()
        wm13 = [S.new_dsem() for _ in range(2)]
        osem = [S.new_dsem() for _ in range(2)]

        ident = cst[:, 0:128]

        def tri(i):
            return cst[:, 128 + i * 128: 256 + i * 128]

        def msk(i):
            return cst[:, 640 + i * 128: 768 + i * 128]

        negcol = cst[:, 896:898]

        a2B = Buf()
        S.op("dve", lambda e: e.memset(a2pf[:], 0.0), w=[a2B])
        S.op("dve", lambda e: e.memset(a2pb[:], 0.0), w=[a2B])
        S.dma("sp", cst[:], cst_d, dsc, w=[cb])
        S.dma("sp", sel[:], sel_d, dsc, w=[cb])
        S.dma("sp", vstage[0:48, :], b_mod_d.rearrange("(j p) -> j p", p=128), dsc, w=[cb])
        S.dma("sp", vstage[48:56, :], n1g_d.rearrange("(j p) -> j p", p=128), dsc, w=[cb])
        S.dma("sp", vstage[56:64, :], n2g_d.rearrange("(j p) -> j p", p=128), dsc, w=[cb])
        S.dma("sp", vstage[64:68, :], psc_d.rearrange("(j p) -> j p", p=128), dsc, w=[cb])
        S.dma("sp", gnb[:], gn_d.rearrange("(o n) -> o n", o=1).to_broadcast([128, HV]), dsc, w=[cb])
        S.dma("sp", rbb[:, 0:4], rgb_d.rearrange("(o n) -> o n", o=1).to_broadcast([128, 4]), dsc, w=[cb])
        S.dma("sp", rbb[:, 4:36], reb_d.rearrange("(o n) -> o n", o=1).to_broadcast([128, 32]), dsc, w=[cb])
        S.dma("sp", sct[:], cT_d.rearrange("(k p) b -> p k b", p=128), dsc, w=[cb])
        S.dma("pool", a2pf[0:16, :], a2f_d, dscp, w=[cb, a2B])
        S.dma("pool", a2pb[16:32, :], a2b_d, dscp, w=[cb, a2B])
        S.dma("pool", poolP[:], poolP_d, dscp, w=[cb])
        S.dma("pool", wlr[:], w_in_d[:, C_LR:C_LR + 32].rearrange("(k p) n -> p k n", p=128), dscp, w=[cb])
        S.dma("pool", rw[:, :, 0:4], rgw_d.rearrange("(k p) n -> p k n", p=128), dscp, w=[cb])
        S.dma("pool", rw[:, :, 4:36], rew_d.rearrange("(k p) n -> p k n", p=128), dscp, w=[cb])
        S.dma("pool", pwt[:], pw_d.rearrange("g c e -> c g e"), dscp, w=[cb])
        S.op("dve", lambda e: e.tensor_copy(out=identb[:], in_=ident), r=[cb], w=[cb])
        S.op("act", lambda e: e.activation(out=sct[:], in_=sct[:], func=AF.Silu), r=[cb], w=[cb])
        S.op("pe", lambda e: e.transpose(P[0][:, 0:68], vstage[:], cst[0:68, 0:68]), r=[cb], w=[Pb[0]])
        S.op("dve", lambda e: e.tensor_copy(out=vT[:], in_=P[0][:, 0:68]), r=[Pb[0]], w=[cb])

        with contextlib.ExitStack() as p0:
            wm = [sb(p0, "wm%d" % i, (128, KD, 512)) for i in range(2)]
            bm5 = sb(p0, "bm5", (NBC, 2, D))
            for ci, ch in enumerate((2, 5)):
                S.dma("sp", bm5[:, ci, :],
                      b_mod_d[ch * D:(ch + 1) * D].rearrange("(o n) -> o n", o=1).to_broadcast([NBC, D]),
                      dsl, w=[cb])
            wmb = bufs(2)
            wmd = [S.new_dsem() for _ in range(2)]
            for cg in range(12):
                i = cg % 2
                S.dma("sp", wm[i][:], w_mod_d[:, cg * 512:(cg + 1) * 512].rearrange("(k p) n -> p k n", p=128),
                      wmd[i], w=[wmb[i]])
                for m in range(4):
                    j = cg * 4 + m
                    for k in range(KD):
                        S.op("pe", lambda e, i=i, m=m, k=k, j=j: e.matmul(
                            P[1][:, j * 8:j * 8 + NBC], lhsT=wm[i][:, k, m * 128:(m + 1) * 128],
                            rhs=sct[:, k, :], start=(k == 0), stop=(k == KD - 1)),
                            r=[wmb[i], cb], w=[Pb[1]])
                if cg // 2 in (2, 5):
                    ci = 0 if cg // 2 == 2 else 1
                    hf = cg % 2
                    for k in range(KD):
                        S.op("pe", lambda e, i=i, k=k: e.matmul(
                            P[2][0:NBC, :], lhsT=sct[:, k, :], rhs=wm[i][:, k, :],
                            start=(k == 0), stop=(k == KD - 1)), r=[wmb[i], cb], w=[Pb[2]])
                    S.op("dve", lambda e, ci=ci, hf=hf: e.tensor_tensor(
                        out=msb[:, ci, hf * 512:(hf + 1) * 512], in0=P[2][0:NBC, :],
                        in1=bm5[:, ci, hf * 512:(hf + 1) * 512], op=ALU.add), r=[Pb[2], cb], w=[cb])
            p1v = P[1][:, 0:384].rearrange("p (j e) -> p j e", e=8)
            for b in range(NBC):
                S.op("dve", lambda e, b=b: e.tensor_tensor(out=modT[:, b, :], in0=p1v[:, :, b], in1=vT[:, 0:48],
                                                           op=ALU.add), r=[Pb[1], cb], w=[cb])
            for b in range(NBC):
                S.op("dve", lambda e, b=b: e.scalar_tensor_tensor(
                    out=A1[:, b, :], in0=modT[:, b, 8:16], scalar=1.0, op0=ALU.add, in1=vT[:, 48:56], op1=ALU.mult),
                    r=[cb], w=[cb])
                S.op("dve", lambda e, b=b: e.scalar_tensor_tensor(
                    out=A2[:, b, :], in0=modT[:, b, 32:40], scalar=1.0, op0=ALU.add, in1=vT[:, 56:64], op1=ALU.mult),
                    r=[cb], w=[cb])
            S.barrier()

        dbg_off = [0]

        def dump(ap_, n, rb, np_=128):
            if dbg_d is None:
                return
            o = dbg_off[0]
            S.dma("pool", dbg_d[0:np_, o:o + n], ap_, dbgsem, r=rb)
            dbg_off[0] = o + n

        def rms_rstd(es_unused, src_ap, width, ss, rs, junk, rb, sb_):
            S.op("act", lambda e: e.activation(out=junk, in_=src_ap, func=AF.Square, accum_out=ss[:, 0:1]),
                 r=rb, w=[sb_])
            S.op("act", lambda e: e.activation(out=rs[:, 0:1], in_=ss[:, 0:1], func=AF.Sqrt, bias=EPS,
                                               scale=1.0 / width), r=[sb_], w=[sb_])
            S.op("dve", lambda e: e.reciprocal(out=rs[:, 0:1], in_=rs[:, 0:1]), r=[sb_], w=[sb_])

        try:
            for b in range(NB):
                with contextlib.ExitStack() as eb:
                    acc = sb(eb, "acc", (128, NXT, D))
                    accb = bufs(NXT)
                    with contextlib.ExitStack() as e13:
                        hT = sb(e13, "hT", (128, KD, TOK), BF16)
                        hTb = bufs(NTT)
                        lrT = sb(e13, "lrT", (32, TOK), BF16)
                        lrb = Buf()
                        with contextlib.ExitStack() as e1:
                            xin = [sb(e1, "xin%d" % i, (128, D)) for i in range(3)]
                            xib = bufs(3)
                            junk = sb(e1, "junk1", (128, D), BF16)
                            st = [sb(e1, "st1_%d" % i, (128, 2)) for i in range(2)]
                            stb = bufs(2)
                            jb = Buf()

                            def load_x(tt):
                                i = tt % 3
                                src = ctx_d[b, tt * 128:(tt + 1) * 128, :] if tt < NCT else \
                                    x_d[b, (tt - NCT) * 128:(tt - NCT + 1) * 128, :]
                                S.dma("sp", xin[i][:], src, xid[i], w=[xib[i]])

                            def p1_body(tt):
                                if tt + 2 < NTT:
                                    load_x(tt + 2)
                                i = tt % 3
                                s_ = st[tt % 2]
                                sbb = stb[tt % 2]
                                mb = NB if tt < NCT else b
                                S.op("act", lambda e: e.activation(
                                    out=junk[:], in_=xin[i][:], func=AF.Square, accum_out=s_[:, 0:1]),
                                    r=[xib[i]], w=[sbb, jb])
                                S.op("act", lambda e: e.activation(
                                    out=s_[:, 1:2], in_=s_[:, 0:1], func=AF.Sqrt, bias=EPS, scale=1.0 / D),
                                    r=[sbb], w=[sbb])
                                S.op("dve", lambda e: e.reciprocal(out=s_[:, 1:2], in_=s_[:, 1:2]),
                                     r=[sbb], w=[sbb])
                                S.op("act", lambda e: e.activation(
                                    out=xin[i][:], in_=xin[i][:], func=AF.Copy, scale=s_[:, 1:2]),
                                    r=[sbb, xib[i]], w=[xib[i]])
                                yield
                                pbase = (tt % 2) * 2
                                for k in range(KD):
                                    bk = pbase + k // 4
                                    S.op("pe", lambda e, k=k, bk=bk: e.transpose(
                                        P[bk][:, (k % 4) * 128:(k % 4 + 1) * 128], xin[i][:, k * 128:(k + 1) * 128], ident),
                                        r=[xib[i], cb], w=[Pb[bk]])
                                yield
                                for k in range(KD):
                                    bk = pbase + k // 4
                                    src = P[bk][:, (k % 4) * 128:(k % 4 + 1) * 128]
                                    dst = hT[:, k, tt * 128:(tt + 1) * 128]
                                    if k // 4 == 0:
                                        S.op("act", lambda e, src=src, dst=dst, k=k: e.activation(
                                            out=dst, in_=src, func=AF.Identity, scale=A1[:, mb, k:k + 1],
                                            bias=modT[:, mb, k:k + 1]), r=[Pb[bk], cb], w=[hTb[tt]])
                                    else:
                                        S.op("dve", lambda e, src=src, dst=dst, k=k: e.tensor_scalar(
                                            out=dst, in0=src, scalar1=A1[:, mb, k:k + 1], scalar2=modT[:, mb, k:k + 1],
                                            op0=ALU.mult, op1=ALU.add), r=[Pb[bk], cb], w=[hTb[tt]])

                            load_x(0)
                            load_x(1)
                            pipeline(p1_body(tt) for tt in range(NTT))
                            S.barrier()
                            if upto == 1:
                                S.dead = True
                        with contextlib.ExitStack() as e23:
                            whd = sb(e23, "whd", (128, KD, 768), BF16)
                            abfb = sb(e23, "abfb", (128, 512))
                            abbb = sb(e23, "abbb", (128, 512))
                            abB = Buf()
                            S.dma("sp", abfb[:], abf_d.rearrange("(o n) -> o n", o=1).to_broadcast([128, 512]), dsl, w=[abB])
                            S.dma("sp", abbb[:], abb_d.rearrange("(o n) -> o n", o=1).to_broadcast([128, 512]), dsl, w=[abB])
                            wgl = sb(e23, "wgl", (128, 2, D), BF16)
                            whb = Buf()
                            qT = sb(e23, "qT", (128, SEQ), BF16)
                            qTb = bufs(4)
                            kT = sb(e23, "kT", (128, TOK), BF16)
                            kTb = bufs(5)
                            ktok = sb(e23, "ktok", (128, NTT, 128), BF16)
                            ktb = bufs(3)
                            vtok = sb(e23, "vtok", (128, NTT, HV), BF16)
                            vtb = bufs(NTT)
                            sg = sb(e23, "sg", (128, NXT, HV), BF16)
                            sgb = bufs(NXT)
                            SBs = sb(e23, "SBs", (128, NXT, HV), BF16)
                            SBb = bufs(NXT)
                            SF = [sb(e23, "SF%d" % i, (128, HV), BF16) for i in range(2)]
                            SFb = bufs(2)
                            Sst = sb(e23, "Sst", (128, HV))
                            Sstb = Buf()
                            NSL = 3
                            NBIG = 2
                            zz = [sb(e23, "zz%d" % p_, (128, 256)) for p_ in range(NBIG)]
                            zzb = bufs(NBIG)
                            EE = [sb(e23, "EE%d" % p_, (128, 386)) for p_ in range(NBIG)]
                            EEb = bufs(NBIG)
                            E2 = [sb(e23, "E2_%d" % p_, (128, 256)) for p_ in range(NBIG)]
                            E2b = bufs(NBIG)
                            NDEC = 6
                            decs = sb(e23, "decs", (128, NDEC))
                            decb = bufs(NDEC)
                            kr = [sb(e23, "kr%d" % p_, (128, 128), BF16) for p_ in range(NSL)]
                            krb = bufs(NSL)
                            abh = sb(e23, "abh", (128, 256))
                            abhb = Buf()
                            qd = [[sb(e23, "qd%d%d" % (p_, d_), (128, 128), BF16) for d_ in range(2)] for p_ in range(NSL)]
                            qdb = [bufs(2) for _ in range(NSL)]
                            ki = [[sb(e23, "ki%d%d" % (p_, d_), (128, 128), BF16) for d_ in range(2)] for p_ in range(NSL)]
                            kib = [bufs(2) for _ in range(NSL)]
                            sT = [[sb(e23, "sT%d%d" % (p_, d_), (128, 128), BF16) for d_ in range(2)] for p_ in range(NSL)]
                            sTb = [bufs(2) for _ in range(NSL)]
                            otmp = [sb(e23, "otmp%d" % p_, (128, HV)) for p_ in range(NBIG)]
                            otb = bufs(NBIG)
                            og = [sb(e23, "og%d" % p_, (128, HV), BF16) for p_ in range(NBIG)]
                            ogb = bufs(NBIG)
                            ogT = [sb(e23, "ogT%d" % p_, (128, 2, 128), BF16) for p_ in range(NBIG)]
                            ogTb = bufs(NBIG)
                            st2 = [sb(e23, "st2_%d" % p_, (128, 2)) for p_ in range(NSL)]
                            st2b = bufs(NSL)
                            junk2 = sb(e23, "junk2", (128, HV), BF16)
                            vglock = Buf()
                            j2b = Buf()
                            nbk = [0]

                            def rot():
                                v_ = nbk[0]
                                nbk[0] = (v_ + 1) % 7
                                return v_

                            def kgrp(tt):
                                return 0 if tt < NCT else 1 + (tt - NCT) // 4

                            tokgroups = [(0, 256)] + [(256 + g * 512, 512) for g in range(4)]

                            wglb = Buf()

                            def load_whd(h_):
                                for (dst0, src0, n_) in ((0, C_Q + h_ * 128, 128), (128, C_K + h_ * 128, 128),
                                                         (256, C_V + h_ * 256, 256), (512, C_G + h_ * 256, 256)):
                                    S.dma("pool", whd[:, :, dst0:dst0 + n_],
                                          w_in_d[:, src0:src0 + n_].rearrange("(k p) n -> p k n", p=128), wsem, w=[whb])

                            def load_wgl(h_):
                                S.dma("pool", wgl[:], wgb_d[h_ * 256:(h_ + 1) * 256, :].rearrange("(c p) n -> p c n", p=128),
                                      wgsem, w=[wglb])

                            for h in range(NHEAD):
                                if h == 0:
                                    load_whd(0)
                                    load_wgl(0)
                                for g in range(4):
                                    bk = rot()
                                    for k in range(KD):
                                        S.op("pe", lambda e, k=k, g=g, bk=bk: e.matmul(
                                            P[bk][:, :], lhsT=whd[:, k, 0:128], rhs=hT[:, k, 256 + g * 512:768 + g * 512],
                                            start=(k == 0), stop=(k == KD - 1)),
                                            r=[whb] + hTb[2 + 4 * g:6 + 4 * g], w=[Pb[bk]])
                                    S.op("act", lambda e, g=g, bk=bk: e.activation(
                                        out=qT[:, g * 512:(g + 1) * 512], in_=P[bk][:, :], func=AF.Copy, scale=HK ** -0.5),
                                        r=[Pb[bk]], w=[qTb[g]])
                                for g, (t0, n_) in enumerate(tokgroups):
                                    bk = rot()
                                    for k in range(KD):
                                        S.op("pe", lambda e, k=k, bk=bk, t0=t0, n_=n_: e.matmul(
                                            P[bk][:, 0:n_], lhsT=whd[:, k, 128:256], rhs=hT[:, k, t0:t0 + n_],
                                            start=(k == 0), stop=(k == KD - 1)),
                                            r=[whb] + hTb[t0 // 128:(t0 + n_) // 128], w=[Pb[bk]])
                                    S.op("dve", lambda e, bk=bk, t0=t0, n_=n_: e.tensor_copy(
                                        out=kT[:, t0:t0 + n_], in_=P[bk][:, 0:n_]), r=[Pb[bk]], w=[kTb[g]])
                                    if h == 0:
                                        bk = rot()
                                        for k in range(KD):
                                            S.op("pe", lambda e, k=k, bk=bk, t0=t0, n_=n_: e.matmul(
                                                P[bk][0:32, 0:n_], lhsT=wlr[:, k, :], rhs=hT[:, k, t0:t0 + n_],
                                                start=(k == 0), stop=(k == KD - 1)),
                                                r=[cb] + hTb[t0 // 128:(t0 + n_) // 128], w=[Pb[bk]])
                                        S.op("act", lambda e, bk=bk, t0=t0, n_=n_: e.activation(
                                            out=lrT[:, t0:t0 + n_], in_=P[bk][0:32, 0:n_], func=AF.Copy),
                                            r=[Pb[bk]], w=[lrb])
                                if upto == 20 and h == 0:
                                    S.dead = True
                                for bi, (tt0, nt_) in enumerate(((0, 8), (8, 8), (16, 2))):
                                    for j in range(nt_):
                                        tt = tt0 + j
                                        S.op("pe", lambda e, j=j, tt=tt: e.transpose(
                                            PB[:, j * 128:(j + 1) * 128], kT[:, tt * 128:(tt + 1) * 128], identb[:]),
                                            r=[kTb[kgrp(tt)], cb], w=[PBb])
                                    S.op("act" if bi % 2 else "dve", (lambda e, tt0=tt0, nt_=nt_: e.activation(
                                        out=ktok[:, tt0:tt0 + nt_, :], in_=PB[:, 0:nt_ * 128].rearrange("p (t d) -> p t d", d=128),
                                        func=AF.Copy)) if bi % 2 else (lambda e, tt0=tt0, nt_=nt_: e.tensor_copy(
                                            out=ktok[:, tt0:tt0 + nt_, :],
                                            in_=PB[:, 0:nt_ * 128].rearrange("p (t d) -> p t d", d=128))),
                                        r=[PBb], w=[ktb[bi]])
                                if upto == 21 and h == 0:
                                    S.dead = True
                                for tt in range(NTT):
                                    bk = rot()
                                    n_ = 256 if tt < NCT else 512
                                    for k in range(KD):
                                        S.op("pe", lambda e, k=k, bk=bk, tt=tt, n_=n_: e.matmul(
                                            P[bk][:, 0:n_], lhsT=hT[:, k, tt * 128:(tt + 1) * 128], rhs=whd[:, k, 256:256 + n_],
                                            start=(k == 0), stop=(k == KD - 1)), r=[whb, hTb[tt]], w=[Pb[bk]])
                                    S.op("dve", lambda e, bk=bk, tt=tt: e.tensor_copy(out=vtok[:, tt, :], in_=P[bk][:, 0:256]),
                                         r=[Pb[bk]], w=[vtb[tt], vglock])
                                    if tt >= NCT:
                                        S.op("act", lambda e, bk=bk, tt=tt: e.activation(
                                            out=sg[:, tt - NCT, :], in_=P[bk][:, 256:512], func=AF.Silu),
                                            r=[Pb[bk]], w=[sgb[tt - NCT], vglock])

                                if h + 1 < NHEAD:
                                    load_whd(h + 1)
                                if upto == 22 and h == 0:
                                    S.dead = True
                                S.op("dve", lambda e: e.tensor_copy(out=abh[:, 0:128], in_=abfb[:, h * 128:(h + 1) * 128]),
                                     r=[abB], w=[abhb])
                                S.op("dve", lambda e: e.tensor_copy(out=abh[:, 128:256], in_=abbb[:, h * 128:(h + 1) * 128]),
                                     r=[abB], w=[abhb])

                                def la_stage(tt, dirs, ix):
                                    lo, hi = dirs[0] * 128, (dirs[-1] + 1) * 128
                                    for di in dirs:
                                        a2p = a2pf if di == 0 else a2pb
                                        S.op("pe", lambda e, di=di, a2p=a2p: e.matmul(
                                            P[0][:, di * 128:(di + 1) * 128], lhsT=lrT[:, tt * 128:(tt + 1) * 128],
                                            rhs=a2p[:, h * 128:(h + 1) * 128], start=True, stop=True),
                                            r=[lrb, cb], w=[Pb[0]])
                                    yield
                                    S.op("dve", lambda e: e.tensor_tensor(out=zz[ix % NBIG][:, lo:hi], in0=P[0][:, lo:hi],
                                                                          in1=abh[:, lo:hi], op=ALU.add),
                                         r=[Pb[0], abhb], w=[zzb[ix % NBIG]])
                                    yield
                                    S.op("act", lambda e: e.activation(out=zz[ix % NBIG][:, lo:hi], in_=zz[ix % NBIG][:, lo:hi],
                                                                       func=AF.Exp, scale=-1.0), r=[zzb[ix % NBIG]], w=[zzb[ix % NBIG]])
                                    S.op("act", lambda e: e.activation(out=zz[ix % NBIG][:, lo:hi], in_=zz[ix % NBIG][:, lo:hi],
                                                                       func=AF.Ln, bias=1.0), r=[zzb[ix % NBIG]], w=[zzb[ix % NBIG]])
                                    yield

                                def rem_tot(di, ix):
                                    trm = tri(1) if di == 0 else tri(3)
                                    sp_ = zz[ix % NBIG][:, di * 128:(di + 1) * 128]
                                    S.op("pe", lambda e: e.matmul(P[1][:, 256:384], lhsT=trm, rhs=sp_,
                                                                  start=True, stop=True), r=[zzb[ix % NBIG], cb], w=[Pb[1]])
                                    S.op("pe", lambda e: e.matmul(P[1][:, 384:386], lhsT=sp_, rhs=negcol,
                                                                  start=True, stop=True), r=[zzb[ix % NBIG], cb], w=[Pb[1]])

                                def state_post(tt, ix, dst, dstb, bk=6, c0=0):
                                    S.op("pe", lambda e: e.matmul(P[bk][:, c0:c0 + HV], lhsT=kr[ix % NSL][:], rhs=vtok[:, tt, :],
                                                                  start=True, stop=True), r=[krb[ix % NSL], vtb[tt]], w=[Pb[bk]])
                                    yield
                                    S.op("dve", lambda e: e.scalar_tensor_tensor(
                                        out=Sst[:], in0=Sst[:], scalar=decs[:, ix % NDEC:ix % NDEC + 1], op0=ALU.mult,
                                        in1=P[bk][:, c0:c0 + HV], op1=ALU.add), r=[Sstb, decb[ix % NDEC], Pb[bk]], w=[Sstb])
                                    if dst is not None:
                                        S.op("dve", lambda e: e.tensor_copy(out=dst, in_=Sst[:]), r=[Sstb], w=[dstb])

                                def st_body(di, tt, ix, dst, dstb):
                                    yield from la_stage(tt, (di,), ix)
                                    rem_tot(di, ix)
                                    yield
                                    S.op("act", lambda e: e.activation(out=EE[ix % NBIG][:, 256:386], in_=P[1][:, 256:386], func=AF.Exp),
                                         r=[Pb[1]], w=[EEb[ix % NBIG]])
                                    yield
                                    S.op("dve", lambda e: e.tensor_copy(out=decs[:, ix % NDEC:ix % NDEC + 1], in_=EE[ix % NBIG][:, 384:385]),
                                         r=[EEb[ix % NBIG]], w=[decb[ix % NDEC]])
                                    S.op("dve", lambda e: e.tensor_tensor(out=kr[ix % NSL][:], in0=ktok[:, tt, :],
                                                                          in1=EE[ix % NBIG][:, 256:384], op=ALU.mult),
                                         r=[ktb[tt // 8], EEb[ix % NBIG]], w=[krb[ix % NSL]])
                                    yield
                                    yield from state_post(tt, ix, dst, dstb)

                                def f_body(n, ix):
                                    tt = n + NCT
                                    cur = n % 2
                                    yield from la_stage(tt, (0, 1), ix)
                                    for di in (0, 1):
                                        tin = tri(0) if di == 0 else tri(2)
                                        S.op("pe", lambda e, di=di, tin=tin: e.matmul(
                                            P[1][:, di * 128:(di + 1) * 128], lhsT=zz[ix % NBIG][:, di * 128:(di + 1) * 128], rhs=tin,
                                            start=True, stop=True), r=[zzb[ix % NBIG], cb], w=[Pb[1]])
                                    rem_tot(0, ix)
                                    yield
                                    S.op("act", lambda e: e.activation(out=EE[ix % NBIG][:, 0:386], in_=P[1][:, 0:386], func=AF.Exp),
                                         r=[Pb[1]], w=[EEb[ix % NBIG]])
                                    S.op("act", lambda e: e.activation(out=E2[ix % NBIG][:, 0:256], in_=P[1][:, 0:256], func=AF.Exp,
                                                                       scale=-1.0), r=[Pb[1]], w=[E2b[ix % NBIG]])
                                    yield
                                    for di in (0, 1):
                                        S.op("dve", lambda e, di=di: e.tensor_tensor(
                                            out=qd[ix % NSL][di][:], in0=qT[:, n * 128:(n + 1) * 128],
                                            in1=EE[ix % NBIG][:, di * 128:(di + 1) * 128],
                                            op=ALU.mult), r=[qTb[n // 4], EEb[ix % NBIG]], w=[qdb[ix % NSL][di]])
                                        S.op(POOLENG, lambda e, di=di: e.tensor_tensor(
                                            out=ki[ix % NSL][di][:], in0=kT[:, tt * 128:(tt + 1) * 128],
                                            in1=E2[ix % NBIG][:, di * 128:(di + 1) * 128],
                                            op=ALU.mult), r=[kTb[kgrp(tt)], E2b[ix % NBIG]], w=[kib[ix % NSL][di]])
                                    if n < NXT - 1:
                                        S.op("dve", lambda e: e.tensor_copy(out=decs[:, ix % NDEC:ix % NDEC + 1], in_=EE[ix % NBIG][:, 384:385]),
                                         r=[EEb[ix % NBIG]], w=[decb[ix % NDEC]])
                                    S.op("dve", lambda e: e.tensor_tensor(out=kr[ix % NSL][:], in0=ktok[:, tt, :],
                                                                              in1=EE[ix % NBIG][:, 256:384], op=ALU.mult),
                                             r=[ktb[tt // 8], EEb[ix % NBIG]], w=[krb[ix % NSL]])
                                    yield
                                    for di in (0, 1):
                                        S.op("pe", lambda e, di=di: e.matmul(
                                            P[2][:, di * 128:(di + 1) * 128], lhsT=ki[ix % NSL][di][:], rhs=qd[ix % NSL][di][:],
                                            start=True, stop=True), r=[kib[ix % NSL][di], qdb[ix % NSL][di]], w=[Pb[2]])
                                    yield
                                    for di in (0, 1):
                                        S.op("dve", lambda e, di=di: e.tensor_tensor(
                                            out=sT[ix % NSL][di][:], in0=P[2][:, di * 128:(di + 1) * 128], in1=msk(di),
                                            op=ALU.mult), r=[Pb[2], cb], w=[sTb[ix % NSL][di]])
                                    yield
                                    ops_o = ((sT[ix % NSL][0][:], vtok[:, tt, :], [sTb[ix % NSL][0], vtb[tt]]),
                                             (qd[ix % NSL][0][:], SF[cur][:], [qdb[ix % NSL][0], SFb[cur]]),
                                             (sT[ix % NSL][1][:], vtok[:, tt, :], [sTb[ix % NSL][1], vtb[tt]]),
                                             (qd[ix % NSL][1][:], SBs[:, n, :], [qdb[ix % NSL][1], SBb[n]]))
                                    ob = 3 if ix % 2 == 0 else 6
                                    for oi, (l_, r_, rb_) in enumerate(ops_o):
                                        S.op("pe", lambda e, l_=l_, r_=r_, oi=oi: e.matmul(
                                            P[ob][:, 0:HV], lhsT=l_, rhs=r_, start=(oi == 0), stop=(oi == 3)),
                                            r=rb_, w=[Pb[ob]])
                                    if n < NXT - 1:
                                        spost = state_post(tt, ix, SF[1 - cur][:], SFb[1 - cur], bk=ob, c0=256)
                                        next(spost)
                                    else:
                                        spost = iter(())
                                    yield
                                    S.op("act", lambda e: e.activation(out=junk2[:], in_=P[ob][:, 0:HV], func=AF.Square,
                                                                       accum_out=st2[ix % NSL][:, 0:1]),
                                         r=[Pb[ob]], w=[st2b[ix % NSL], j2b])
                                    S.op("act", lambda e: e.activation(out=st2[ix % NSL][:, 1:2], in_=st2[ix % NSL][:, 0:1], func=AF.Ln,
                                                                       bias=EPS, scale=1.0 / HV), r=[st2b[ix % NSL]], w=[st2b[ix % NSL]])
                                    S.op("act", lambda e: e.activation(out=st2[ix % NSL][:, 1:2], in_=st2[ix % NSL][:, 1:2], func=AF.Exp,
                                                                       scale=-0.5), r=[st2b[ix % NSL]], w=[st2b[ix % NSL]])
                                    next(spost, None)
                                    yield
                                    S.op("dve", lambda e: e.scalar_tensor_tensor(
                                        out=otmp[ix % NBIG][:], in0=P[ob][:, 0:HV], scalar=st2[ix % NSL][:, 1:2], op0=ALU.mult,
                                        in1=gnb[:], op1=ALU.mult), r=[Pb[ob], st2b[ix % NSL], cb], w=[otb[ix % NBIG]])
                                    S.op(POOLENG, lambda e: e.tensor_tensor(out=og[ix % NBIG][:], in0=otmp[ix % NBIG][:], in1=sg[:, n, :],
                                                                           op=ALU.mult), r=[otb[ix % NBIG], sgb[n]], w=[ogb[ix % NBIG]])
                                    yield
                                    for c in range(2):
                                        S.op("pe", lambda e, c=c: e.transpose(PB[:, c * 128:(c + 1) * 128],
                                                                              og[ix % NBIG][:, c * 128:(c + 1) * 128], identb[:]),
                                             r=[ogb[ix % NBIG], cb], w=[PBb])
                                    yield
                                    S.op("act", lambda e: e.activation(
                                        out=ogT[ix % NBIG][:, :, :], in_=PB[:, 0:256].rearrange("p (c t) -> p c t", t=128),
                                        func=AF.Copy), r=[PBb], w=[ogTb[ix % NBIG]])
                                    yield
                                    for hf in range(2):
                                        for c in range(2):
                                            S.op("pe", lambda e, hf=hf, c=c: e.matmul(
                                                P[4 + hf][:, :], lhsT=ogT[ix % NBIG][:, c, :], rhs=wgl[:, c, hf * 512:(hf + 1) * 512],
                                                start=(c == 0), stop=(c == 1)), r=[ogTb[ix % NBIG], wglb], w=[Pb[4 + hf]])
                                    yield
                                    for hf in range(2):
                                        dst = acc[:, n, hf * 512:(hf + 1) * 512]
                                        if h == 0:
                                            S.op("act", lambda e, hf=hf, dst=dst: e.activation(out=dst, in_=P[4 + hf][:, :],
                                                                                               func=AF.Copy),
                                                 r=[Pb[4 + hf]], w=[accb[n]])
                                        else:
                                            S.op("dve", lambda e, hf=hf, dst=dst: e.tensor_tensor(
                                                out=dst, in0=dst, in1=P[4 + hf][:, :], op=ALU.add),
                                                r=[Pb[4 + hf], accb[n]], w=[accb[n]])

                                S.op("dve", lambda e: e.memset(Sst[:], 0.0), w=[Sstb])
                                gens = [st_body(1, 1, 0, None, None), st_body(1, 0, 1, SBs[:, NXT - 1, :], SBb[NXT - 1])]
                                for n in range(NXT - 1, 0, -1):
                                    gens.append(st_body(1, n + NCT, len(gens), SBs[:, n - 1, :], SBb[n - 1]))
                                pipeline(gens)
                                if upto == 23 and h == 0:
                                    S.dead = True
                                S.op("dve", lambda e: e.memset(Sst[:], 0.0), w=[Sstb])
                                gens = [st_body(0, 0, 0, None, None), st_body(0, 1, 1, SF[0][:], SFb[0])]
                                for n in range(NXT):
                                    gens.append(f_body(n, len(gens)))
                                pipeline(gens)
                                if h + 1 < NHEAD:
                                    load_wgl(h + 1)
                            S.barrier()
                            if upto == 2:
                                S.dead = True
                        S.barrier()
                    h2T = sb(eb, "h2T", (128, NXT, KD, 128), BF16)
                    h2b = bufs(NXT)
                    cw = sb(eb, "cw", (128, NXT, 32))
                    cwb = bufs(NXT)

                    def bcast_row(dst, ci):
                        for hf in range(2):
                            S.op("pe", lambda e, hf=hf: e.matmul(
                                P[hf][:, :], lhsT=sel[0:NBC, b * 128:(b + 1) * 128], rhs=msb[:, ci, hf * 512:(hf + 1) * 512],
                                start=True, stop=True), r=[cb], w=[Pb[hf]])
                            S.op("act", lambda e, hf=hf: e.activation(out=dst[:, hf * 512:(hf + 1) * 512], in_=P[hf][:, :],
                                                                      func=AF.Copy), r=[Pb[hf]], w=[cb])

                    with contextlib.ExitStack() as e4:
                        wu = sb(e4, "wu", (128, KD, 512), BF16)
                        wgt = sb(e4, "wgt", (128, KD, 2048), BF16)
                        wpb = sb(e4, "wpb", (128, 4, D), BF16)
                        w4b = Buf()
                        S.dma("pool", wu[:], w_in_d[:, C_POOL:C_POOL + 512].rearrange("(k p) n -> p k n", p=128), wsem, w=[w4b])
                        for j in range(4):
                            S.dma("pool", wgt[:, :, j * 512:(j + 1) * 512],
                                  w_in_d[:, C_GATES + j * 512:C_GATES + (j + 1) * 512].rearrange("(k p) n -> p k n", p=128),
                                  wsem, w=[w4b])
                        S.dma("pool", wpb[:], wpb_d.rearrange("(g p) n -> p g n", p=128), wsem, w=[w4b])
                        xin = [sb(e4, "xin4_%d" % i, (128, D)) for i in range(2)]
                        xib = bufs(2)
                        xs = sb(e4, "xs4", (128, D))
                        xsb = Buf()
                        junk = sb(e4, "junk4", (128, D), BF16)
                        jb = Buf()
                        st = [sb(e4, "st4_%d" % i, (128, 2)) for i in range(2)]
                        stb = bufs(2)
                        hTt = [sb(e4, "hTt%d" % i, (128, KD, 128), BF16) for i in range(2)]
                        hTtb = bufs(2)
                        u_sb = sb(e4, "u_sb", (128, 512), BF16)
                        ub = Buf()
                        pT = sb(e4, "pT", (128, 512), BF16)
                        pTb = Buf()
                        y1T = sb(e4, "y1T", (128, 4, 128), BF16)
                        y1b = Buf()
                        sgt = [sb(e4, "sgt%d" % i, (128, 512)) for i in range(2)]
                        sgtb = bufs(2)
                        t1 = sb(e4, "t1", (128, D))
                        t1b = bufs(2)
                        tmp = [sb(e4, "tmp4_%d" % i, (128, 512)) for i in range(2)]
                        tmpb = bufs(2)

                        def load_x4(n):
                            S.dma("sp", xin[n % 2][:], x_d[b, n * 128:(n + 1) * 128, :], xid[n % 2], w=[xib[n % 2]])

                        def p4a_body(n):
                            if n + 1 < NXT:
                                load_x4(n + 1)
                            i = n % 2
                            par = n % 2
                            S.op("act", lambda e: e.activation(out=junk[:], in_=xin[i][:], func=AF.Square,
                                                               accum_out=st[par][:, 0:1]), r=[xib[i]], w=[stb[par], jb])
                            S.op("act", lambda e: e.activation(out=st[par][:, 1:2], in_=st[par][:, 0:1], func=AF.Sqrt,
                                                               bias=EPS, scale=1.0 / D), r=[stb[par]], w=[stb[par]])
                            S.op("dve", lambda e: e.reciprocal(out=st[par][:, 1:2], in_=st[par][:, 1:2]),
                                 r=[stb[par]], w=[stb[par]])
                            S.op("act", lambda e: e.activation(out=xs[:], in_=xin[i][:], func=AF.Copy, scale=st[par][:, 1:2]),
                                 r=[stb[par], xib[i]], w=[xsb])
                            for k in range(KD):
                                S.op("pe", lambda e, k=k: e.transpose(P[k // 4][:, (k % 4) * 128:(k % 4 + 1) * 128],
                                                                      xs[:, k * 128:(k + 1) * 128], ident),
                                     r=[xsb, cb], w=[Pb[k // 4]])
                            for k in range(KD):
                                src = P[k // 4][:, (k % 4) * 128:(k % 4 + 1) * 128]
                                dst = hTt[par][:, k, :]
                                if k // 4 == 0:
                                    S.op("act", lambda e, src=src, dst=dst, k=k: e.activation(
                                        out=dst, in_=src, func=AF.Identity, scale=A1[:, b, k:k + 1], bias=modT[:, b, k:k + 1]),
                                        r=[Pb[k // 4], cb], w=[hTtb[par]])
                                else:
                                    S.op("dve", lambda e, src=src, dst=dst, k=k: e.tensor_scalar(
                                        out=dst, in0=src, scalar1=A1[:, b, k:k + 1], scalar2=modT[:, b, k:k + 1],
                                        op0=ALU.mult, op1=ALU.add), r=[Pb[k // 4], cb], w=[hTtb[par]])
                            yield
                            for k in range(KD):
                                S.op("pe", lambda e, k=k: e.matmul(P[2][:, :], lhsT=hTt[par][:, k, :], rhs=wu[:, k, :],
                                                                   start=(k == 0), stop=(k == KD - 1)),
                                     r=[hTtb[par], w4b], w=[Pb[2]])
                            S.op("dve", lambda e: e.tensor_copy(out=u_sb[:], in_=P[2][:, :]), r=[Pb[2]], w=[ub])
                            for g in range(4):
                                S.op("pe", lambda e, g=g: e.matmul(P[3][:, g * 128:(g + 1) * 128],
                                                                   lhsT=u_sb[:, g * 128:(g + 1) * 128],
                                                                   rhs=poolP[:, g * 128:(g + 1) * 128], start=True, stop=True),
                                     r=[ub, cb], w=[Pb[3]])
                            S.op("act", lambda e: e.activation(out=pT[:], in_=P[3][:, :], func=AF.Copy), r=[Pb[3]], w=[pTb])
                            for g in range(4):
                                S.op("pe", lambda e, g=g: e.matmul(P[2][:, g * 128:(g + 1) * 128], lhsT=pwt[:, g, :],
                                                                   rhs=pT[:, g * 128:(g + 1) * 128], start=True, stop=True),
                                     r=[pTb, cb], w=[Pb[2]])
                            for g in range(4):
                                S.op("act", lambda e, g=g: e.activation(out=y1T[:, g, :], in_=P[2][:, g * 128:(g + 1) * 128],
                                                                        func=AF.Copy, scale=vT[:, 64 + g:65 + g]),
                                     r=[Pb[2], cb], w=[y1b])
                            for hf in range(2):
                                for g in range(4):
                                    S.op("pe", lambda e, hf=hf, g=g: e.matmul(
                                        P[4 + hf][:, :], lhsT=y1T[:, g, :], rhs=wpb[:, g, hf * 512:(hf + 1) * 512],
                                        start=(g == 0), stop=(g == 3)), r=[y1b, w4b], w=[Pb[4 + hf]])
                            yield
                            for j in range(4):
                                bk = 6 if j % 2 == 0 else 3
                                hf = j % 2
                                for k in range(KD):
                                    S.op("pe", lambda e, k=k, j=j, bk=bk: e.matmul(
                                        P[bk][:, :], lhsT=hTt[par][:, k, :], rhs=wgt[:, k, j * 512:(j + 1) * 512],
                                        start=(k == 0), stop=(k == KD - 1)), r=[hTtb[par], w4b], w=[Pb[bk]])
                                S.op("act", lambda e, bk=bk, hf=hf: e.activation(out=sgt[hf][:], in_=P[bk][:, :],
                                                                                 func=AF.Sigmoid),
                                     r=[Pb[bk]], w=[sgtb[hf]])
                                if j < 2:
                                    S.op("dve", lambda e, hf=hf: e.tensor_tensor(
                                        out=t1[:, hf * 512:(hf + 1) * 512], in0=sgt[hf][:], in1=P[4 + hf][:, :], op=ALU.mult),
                                        r=[sgtb[hf], Pb[4 + hf]], w=[t1b[hf]])
                                else:
                                    S.op("dve", lambda e, hf=hf: e.tensor_tensor(
                                        out=tmp[hf][:], in0=sgt[hf][:], in1=acc[:, n, hf * 512:(hf + 1) * 512], op=ALU.mult),
                                        r=[sgtb[hf], accb[n]], w=[tmpb[hf]])
                                    S.op("dve", lambda e, hf=hf: e.tensor_tensor(
                                        out=h2T[:, n, hf * 4:(hf + 1) * 4, :], in0=tmp[hf][:].rearrange("p (k t) -> p k t", t=128),
                                        in1=t1[:, hf * 512:(hf + 1) * 512].rearrange("p (k t) -> p k t", t=128), op=ALU.add),
                                        r=[tmpb[hf], t1b[hf]], w=[h2b[n]])

                        load_x4(0)
                        pipeline(p4a_body(n) for n in range(NXT))
                        S.barrier()
                        if upto == 3:
                            S.dead = True

                    with contextlib.ExitStack() as e4:
                        wo = sb(e4, "wo", (128, KD, D), BF16)
                        w4b = Buf()
                        S.dma("pool", wo[:], wo_d.rearrange("(k p) n -> p k n", p=128), wsem, w=[w4b])
                        g1b = sb(e4, "g1b", (128, D))
                        bcast_row(g1b, 0)
                        xin = [sb(e4, "xin5_%d" % i, (128, D)) for i in range(2)]
                        xib = bufs(2)
                        mTt = [sb(e4, "mTt%d" % i, (128, KD, 128), BF16) for i in range(2)]
                        mTb = bufs(2)
                        tmp2 = sb(e4, "tmp2", (128, D))
                        tmp2b = bufs(2)
                        xs = sb(e4, "xs5", (128, D))
                        xsb = Buf()
                        junk = sb(e4, "junk5", (128, D), BF16)
                        jb = Buf()
                        st = [sb(e4, "st5_%d" % i, (128, 2)) for i in range(2)]
                        stb = bufs(2)
                        lgall = sb(e4, "lgall", (128, NXT, 36))
                        lgb = Buf()
                        rs_ = sb(e4, "rs_", (128, 10, NXT))
                        rg_ = sb(e4, "rg_", (128, 3, NXT, 4))
                        re_ = sb(e4, "re_", (128, 5, NXT, 32))
                        rtb_ = Buf()

                        def load_x5(n):
                            S.dma("sp", xin[n % 2][:], x_d[b, n * 128:(n + 1) * 128, :], xid[n % 2], w=[xib[n % 2]])

                        def p4b_body(n):
                            i = n % 2
                            par = n % 2
                            mv = h2T[:, n, :, :]
                            for k in range(KD):
                                S.op("pe", lambda e, k=k: e.transpose(PB[:, k * 128:(k + 1) * 128], mv[:, k, :], identb[:]),
                                     r=[h2b[n], cb], w=[PBb])
                            yield
                            S.op("act", lambda e: e.activation(out=mTt[par][:, :, :],
                                                               in_=PB[:, :].rearrange("p (k t) -> p k t", t=128), func=AF.Copy),
                                 r=[PBb], w=[mTb[par]])
                            yield
                            if n + 1 < NXT:
                                load_x5(n + 1)
                            for hf in range(2):
                                for k in range(KD):
                                    S.op("pe", lambda e, hf=hf, k=k: e.matmul(
                                        P[hf][:, :], lhsT=mTt[par][:, k, :], rhs=wo[:, k, hf * 512:(hf + 1) * 512],
                                        start=(k == 0), stop=(k == KD - 1)), r=[mTb[par], w4b], w=[Pb[hf]])
                            yield
                            for hf in range(2):
                                sl = slice(hf * 512, (hf + 1) * 512)
                                S.op("dve", lambda e, hf=hf, sl=sl: e.tensor_tensor(out=tmp2[:, sl], in0=P[hf][:, :],
                                                                                    in1=g1b[:, sl], op=ALU.mult),
                                     r=[Pb[hf], cb], w=[tmp2b[hf]])
                                S.op("dve", lambda e, sl=sl: e.tensor_tensor(out=acc[:, n, sl], in0=tmp2[:, sl],
                                                                              in1=xin[i][:, sl], op=ALU.add),
                                     r=[tmp2b[hf], xib[i]], w=[accb[n]])
                            yield
                            S.op("act", lambda e: e.activation(out=junk[:], in_=acc[:, n, :], func=AF.Square,
                                                               accum_out=st[par][:, 0:1]), r=[accb[n]], w=[stb[par], jb])
                            S.op("act", lambda e: e.activation(out=st[par][:, 1:2], in_=st[par][:, 0:1], func=AF.Ln,
                                                               bias=EPS, scale=1.0 / D), r=[stb[par]], w=[stb[par]])
                            S.op("act", lambda e: e.activation(out=st[par][:, 1:2], in_=st[par][:, 1:2], func=AF.Exp,
                                                               scale=-0.5), r=[stb[par]], w=[stb[par]])
                            S.op("act", lambda e: e.activation(out=xs[:], in_=acc[:, n, :], func=AF.Copy,
                                                               scale=st[par][:, 1:2]), r=[stb[par], accb[n]], w=[xsb])
                            yield
                            for k in range(KD):
                                S.op("pe", lambda e, k=k: e.transpose(P[2 + k // 4][:, (k % 4) * 128:(k % 4 + 1) * 128],
                                                                      xs[:, k * 128:(k + 1) * 128], ident),
                                     r=[xsb, cb], w=[Pb[2 + k // 4]])
                            yield
                            for k in range(KD):
                                src = P[2 + k // 4][:, (k % 4) * 128:(k % 4 + 1) * 128]
                                dst = h2T[:, n, k, :]
                                if k // 4 == 0:
                                    S.op("act", lambda e, src=src, dst=dst, k=k: e.activation(
                                        out=dst, in_=src, func=AF.Identity, scale=A2[:, b, k:k + 1],
                                        bias=modT[:, b, 24 + k:25 + k]), r=[Pb[2 + k // 4], cb], w=[h2b[n]])
                                else:
                                    S.op("dve", lambda e, src=src, dst=dst, k=k: e.tensor_scalar(
                                        out=dst, in0=src, scalar1=A2[:, b, k:k + 1], scalar2=modT[:, b, 24 + k:25 + k],
                                        op0=ALU.mult, op1=ALU.add), r=[Pb[2 + k // 4], cb], w=[h2b[n]])
                            yield
                            for k in range(KD):
                                S.op("pe", lambda e, k=k: e.matmul(P[4][:, 0:36], lhsT=h2T[:, n, k, :], rhs=rw[:, k, :],
                                                                   start=(k == 0), stop=(k == KD - 1)),
                                     r=[h2b[n], cb], w=[Pb[4]])
                            yield
                            S.op("dve", lambda e: e.tensor_tensor(out=lgall[:, n, :], in0=P[4][:, 0:36], in1=rbb[:], op=ALU.add),
                                 r=[Pb[4], cb], w=[lgb])

                        load_x5(0)
                        pipeline(p4b_body(n) for n in range(NXT))
                        T_ = NXT
                        X = mybir.AxisListType.X
                        gl = lgall[:, :, 0:4]
                        el4 = lgall[:, :, 4:36].rearrange("p t (g e) -> p t g e", e=8)
                        gmax, ngs, pg, m1, m2, dd, e2, den, w1_, w2_ = (rs_[:, q, :] for q in range(10))
                        gm, pen, ge = (rg_[:, q, :, :] for q in range(3))
                        em, oh1, em2, oh2, cwt = (re_[:, q, :, :] for q in range(5))

                        def bc(ap2, width):
                            return ap2.unsqueeze(2).to_broadcast([128, T_, width])

                        def dv(fn):
                            S.op("dve", fn, r=[lgb, rtb_], w=[rtb_])

                        dv(lambda e: e.tensor_reduce(out=gmax, in_=gl, axis=X, op=ALU.max))
                        dv(lambda e: e.tensor_tensor(out=gm, in0=gl, in1=bc(gmax, 4), op=ALU.is_equal))
                        dv(lambda e: e.tensor_tensor(out=ge, in0=gl, in1=bc(gmax, 4), op=ALU.subtract))
                        S.op("act", lambda e: e.activation(out=ge, in_=ge, func=AF.Exp), r=[rtb_], w=[rtb_])
                        dv(lambda e: e.tensor_reduce(out=ngs, in_=ge, axis=X, op=ALU.add))
                        dv(lambda e: e.reciprocal(out=pg, in_=ngs))
                        dv(lambda e: e.tensor_scalar(out=pen, in0=gm, scalar1=-1.0, scalar2=1e30, op0=ALU.add, op1=ALU.mult))
                        em4 = em.rearrange("p t (g e) -> p t g e", e=8)
                        dv(lambda e: e.tensor_tensor(out=em4, in0=el4,
                                                     in1=pen.unsqueeze(3).to_broadcast([128, T_, 4, 8]), op=ALU.add))
                        dv(lambda e: e.tensor_reduce(out=m1, in_=em, axis=X, op=ALU.max))
                        dv(lambda e: e.tensor_tensor(out=oh1, in0=em, in1=bc(m1, 32), op=ALU.is_equal))
                        dv(lambda e: e.scalar_tensor_tensor(out=em2.rearrange("p t e -> p (t e)"),
                                                            in0=oh1.rearrange("p t e -> p (t e)"), scalar=-1e30, op0=ALU.mult,
                                                            in1=em.rearrange("p t e -> p (t e)"), op1=ALU.add))
                        dv(lambda e: e.tensor_reduce(out=m2, in_=em2, axis=X, op=ALU.max))
                        dv(lambda e: e.tensor_tensor(out=oh2, in0=em2, in1=bc(m2, 32), op=ALU.is_equal))
                        dv(lambda e: e.tensor_tensor(out=dd, in0=m2, in1=m1, op=ALU.subtract))
                        S.op("act", lambda e: e.activation(out=e2, in_=dd, func=AF.Exp), r=[rtb_], w=[rtb_])
                        dv(lambda e: e.tensor_scalar(out=den, in0=e2, scalar1=1.0, scalar2=None, op0=ALU.add))
                        dv(lambda e: e.reciprocal(out=den, in_=den))
                        dv(lambda e: e.tensor_tensor(out=w1_, in0=den, in1=pg, op=ALU.mult))
                        dv(lambda e: e.tensor_tensor(out=w2_, in0=w1_, in1=e2, op=ALU.mult))
                        dv(lambda e: e.tensor_tensor(out=cwt, in0=oh1, in1=bc(w1_, 32), op=ALU.mult))
                        dv(lambda e: e.tensor_tensor(out=oh2, in0=oh2, in1=bc(w2_, 32), op=ALU.mult))
                        S.op("dve", lambda e: e.tensor_tensor(out=cw[:, :, :], in0=cwt, in1=oh2, op=ALU.add),
                             r=[rtb_], w=cwb)
                        if debug:
                            for n in range(2):
                                dump(acc[:, n, :], D, accb)
                            dump(cw[:, 0, :], 32, cwb)
                            dump(cw[:, 1, :], 32, cwb)
                        S.barrier()
                        if upto == 4:
                            S.dead = True

                    with contextlib.ExitStack() as e5:
                        g2b = sb(e5, "g2b", (128, D))
                        bcast_row(g2b, 1)
                        w1b = [sb(e5, "w1b%d" % i, (128, KD, DE), BF16) for i in range(2)]
                        w3b = [sb(e5, "w3b%d" % i, (128, KD, DE), BF16) for i in range(2)]
                        w2s = sb(e5, "w2s", (128, 4, D))
                        w2b = [sb(e5, "w2b%d" % i, (128, 4, D), BF16) for i in range(2)]
                        wb13 = bufs(2)
                        w2sb = Buf()
                        w2bb = bufs(2)
                        sa = [sb(e5, "sa%d" % i, (128, 512)) for i in range(2)]
                        sab = bufs(2)
                        hid = [sb(e5, "hid%d" % i, (128, 4, 512), BF16) for i in range(2)]
                        hidb = bufs(2)

                        def load_w(e_):
                            sl_ = e_ % 2
                            S.dma("pool", w1b[sl_][:], w1_d[e_].rearrange("(k p) f -> p k f", p=128), wm13[sl_], w=[wb13[sl_]])
                            S.dma("pool", w3b[sl_][:], w3_d[e_].rearrange("(k p) f -> p k f", p=128), wm13[sl_], w=[wb13[sl_]])
                            S.dma("sp", w2s[:], w2_d[e_].rearrange("(c p) n -> p c n", p=128), w2sem, w=[w2sb])
                            for c in range(4):
                                S.op("dve", lambda e, c=c, sl_=sl_: e.tensor_tensor(out=w2b[sl_][:, c, :], in0=w2s[:, c, :],
                                                                                     in1=g2b[:], op=ALU.mult),
                                     r=[w2sb, cb], w=[w2bb[sl_]])

                        items = [(e_, tg) for e_ in range(NEXP) for tg in range(4)]

                        def emit_ab(idx):
                            e_, tg = items[idx]
                            sl_ = e_ % 2
                            hp = idx % 2
                            for f in range(4):
                                ba, bb_ = (0, 1) if f % 2 == 0 else (2, 3)
                                sp_ = f % 2
                                for (wsrc, bk) in ((w1b, ba), (w3b, bb_)):
                                    for k in range(KD):
                                        S.op("pe", lambda e, wsrc=wsrc, bk=bk, k=k, f=f: e.matmul(
                                            P[bk][:, :].rearrange("p (t d) -> p t d", d=128),
                                            lhsT=wsrc[sl_][:, k, f * 128:(f + 1) * 128],
                                            rhs=h2T[:, tg * 4:(tg + 1) * 4, k, :], start=(k == 0), stop=(k == KD - 1)),
                                            r=[wb13[sl_]] + h2b[tg * 4:(tg + 1) * 4], w=[Pb[bk]])
                                S.op("act", lambda e, ba=ba, sp_=sp_: e.activation(out=sa[sp_][:], in_=P[ba][:, :],
                                                                                   func=AF.Silu),
                                     r=[Pb[ba]], w=[sab[sp_]])
                                S.op("dve", lambda e, bb_=bb_, sp_=sp_, f=f: e.tensor_tensor(
                                    out=hid[hp][:, f, :], in0=sa[sp_][:], in1=P[bb_][:, :], op=ALU.mult),
                                    r=[sab[sp_], Pb[bb_]], w=[hidb[hp]])

                        def emit_w2(idx):
                            e_, tg = items[idx]
                            sl_ = e_ % 2
                            hp = idx % 2
                            for j in range(4):
                                n = tg * 4 + j
                                for hf in range(2):
                                    bk = 4 + (j * 2 + hf) % 3
                                    for f in range(4):
                                        S.op("pe", lambda e, bk=bk, f=f, j=j, hf=hf: e.matmul(
                                            P[bk][:, :], lhsT=hid[hp][:, f, j * 128:(j + 1) * 128],
                                            rhs=w2b[sl_][:, f, hf * 512:(hf + 1) * 512], start=(f == 0), stop=(f == 3)),
                                            r=[hidb[hp], w2bb[sl_]], w=[Pb[bk]])
                                    dst = acc[:, n, hf * 512:(hf + 1) * 512]
                                    S.op("dve", lambda e, bk=bk, dst=dst, n=n: e.scalar_tensor_tensor(
                                        out=dst, in0=P[bk][:, :], scalar=cw[:, n, e_:e_ + 1], op0=ALU.mult, in1=dst,
                                        op1=ALU.add), r=[Pb[bk], cwb[n], accb[n]], w=[accb[n]])

                        load_w(0)
                        if NEXP > 1:
                            load_w(1)
                        emit_ab(0)
                        for idx in range(len(items)):
                            if idx + 1 < len(items):
                                emit_ab(idx + 1)
                            emit_w2(idx)
                            e_, tg = items[idx]
                            if tg == 3 and e_ + 2 < NEXP:
                                load_w(e_ + 2)
                        S.barrier()
                        if upto == 5:
                            S.dead = True

                    with contextlib.ExitStack() as e6:
                        fgb = sb(e6, "fgb", (128, D))
                        fb = Buf()
                        S.dma("sp", fgb[:], fg_d.rearrange("(o n) -> o n", o=1).to_broadcast([128, D]), dsl, w=[fb])
                        ot = [sb(e6, "ot%d" % i, (128, D)) for i in range(2)]
                        otb_ = bufs(2)
                        junk = sb(e6, "junk6", (128, D), BF16)
                        jb = Buf()
                        st = [sb(e6, "st6_%d" % i, (128, 2)) for i in range(2)]
                        stb = bufs(2)
                        for n in range(NXT):
                            par = n % 2
                            S.op("act", lambda e: e.activation(out=junk[:], in_=acc[:, n, :], func=AF.Square,
                                                               accum_out=st[par][:, 0:1]), r=[accb[n]], w=[stb[par], jb])
                            S.op("act", lambda e: e.activation(out=st[par][:, 1:2], in_=st[par][:, 0:1], func=AF.Sqrt,
                                                               bias=EPS, scale=1.0 / D), r=[stb[par]], w=[stb[par]])
                            S.op("dve", lambda e: e.reciprocal(out=st[par][:, 1:2], in_=st[par][:, 1:2]),
                                 r=[stb[par]], w=[stb[par]])
                            S.op("dve", lambda e: e.scalar_tensor_tensor(out=ot[par][:], in0=acc[:, n, :],
                                                                         scalar=st[par][:, 1:2], op0=ALU.mult, in1=fgb[:],
                                                                         op1=ALU.mult), r=[accb[n], stb[par], fb], w=[otb_[par]])
                            S.dma("sp", out_d[b, n * 128:(n + 1) * 128, :], ot[par][:], osem[par], r=[otb_[par]])
                        S.barrier()
                        if upto == 6:
                            S.dead = True
                    S.barrier()
        except _Stop:
            pass
        S.dead = False
        S.barrier()
    return nc


_CONSTS = None


def _consts():
    global _CONSTS
    if _CONSTS is None:
        j = np.arange(128)[:, None]
        i = np.arange(128)[None, :]
        cst = np.zeros((128, 898), np.float32)
        cst[:, 0:128] = np.eye(128, dtype=np.float32)
        s = -1.0 / 16.0
        cst[:, 128:256] = (j <= i) * s
        cst[:, 256:384] = (j > i) * s
        cst[:, 384:512] = (j >= i) * s
        cst[:, 512:640] = (j < i) * s
        cst[:, 640:768] = (j <= i)
        cst[:, 768:896] = (j >= i)
        cst[:, 896:898] = s
        poolP = np.zeros((128, 4, 128), np.float32)
        for gi, w in enumerate((2, 4, 8, 16)):
            for t in range(128):
                r0 = (t // 64) * 64
                lo = max(t - w // 2, r0)
                hi = min(t + w // 2, r0 + 64)
                poolP[lo:hi, gi, t] = 1.0 / (hi - lo)
                poolP[t, gi, t] -= 1.0
        sel = np.zeros((8, 8, 128), np.float32)
        for r in range(8):
            sel[r, r, :] = 1.0
        _CONSTS = (cst, poolP.reshape(128, 512), sel.reshape(8, 1024))
    return _CONSTS


_PER_LAYER = ("w_mod", "b_mod", "norm1_g", "norm2_g", "w_in", "gla_a2_f", "gla_ab_f", "gla_a2_b", "gla_ab_b",
              "gla_onorm_g", "pool_w", "pool_scale", "w_pool_br", "w_gla_br", "w_o", "router_grp_w", "router_grp_b",
              "router_exp_w", "router_exp_b", "moe_w1", "moe_w3", "moe_w2")


def kernel(**inputs):
    NB = 4
    x = np.asarray(inputs["x"], np.float32)
    c = np.asarray(inputs["c"], np.float32)
    ctx = np.asarray(inputs["ctx"], np.float32)
    c_ctx = np.asarray(inputs["c_ctx"], np.float32)
    cst, poolP, sel = _consts()
    shared = {k: np.ascontiguousarray(np.asarray(inputs[k], np.float32)[0]) for k in _PER_LAYER}
    shared["final_norm_g"] = np.ascontiguousarray(np.asarray(inputs["final_norm_g"], np.float32))
    shared["cst"] = cst
    shared["poolP"] = poolP
    shared["sel"] = sel
    in_maps = []
    for core in range(NCORES):
        b0 = core * NB
        m = dict(shared)
        m["x"] = np.ascontiguousarray(x[b0:b0 + NB])
        m["ctx"] = np.ascontiguousarray(ctx[b0:b0 + NB])
        m["cT"] = np.ascontiguousarray(np.concatenate([c[b0:b0 + NB], c_ctx[None, :]], axis=0).T)
        in_maps.append(m)
    nc = build_program(NB=NB)
    res = run_bass_kernel_spmd(nc, in_maps, core_ids=list(range(NCORES)))
    return np.concatenate([np.asarray(r["out"], np.float32) for r in res.results], axis=0)
```

```python
import contextlib
import os
import numpy as np
import concourse.bass as bass
import concourse.mybir as mybir
from concourse.bass_utils import run_bass_kernel_spmd

F32 = mybir.dt.float32
BF16 = mybir.dt.bfloat16
AF = mybir.ActivationFunctionType
ALU = mybir.AluOpType

D = 1024
SEQ = 2048
CTXL = 256
KD = 8
NXT = SEQ // 128
NCT = CTXL // 128
NTT = NXT + NCT
TOK = NTT * 128
D_IN = 5664
C_POOL, C_Q, C_K, C_V, C_G, C_GATES, C_LR = 0, 512, 1024, 1536, 2560, 3584, 5632
NHEAD = 4
HK = 128
HV = 256
NEXP_FULL = 32
DE = 512
EPS = 1e-6
NCORES = 8
STRICT_SAME = not os.environ.get("K_NOSTRICT")
POOLENG = "dve" if os.environ.get("K_NOPOOL") else "pool"


class Buf:
    __slots__ = ("lw", "rd", "excl")

    def __init__(self, excl=False):
        self.lw = None
        self.rd = {}
        self.excl = excl


def bufs(n):
    return [Buf() for _ in range(n)]


class DSem:
    def __init__(self, sem):
        self.sem = sem
        self.count = 0
        self.q = None
        self.maxw = 0


class Sched:
    def __init__(self, nc, es):
        self.nc = nc
        self.es = es
        self.E = {}
        for name, eng in (("pe", nc.tensor), ("act", nc.scalar), ("dve", nc.vector),
                          ("pool", nc.gpsimd), ("sp", nc.sync)):
            sem = es.enter_context(nc.semaphore("s_" + name))
            self.E[name] = dict(eng=eng, sem=sem, count=0, waited={})
        self.dsems = []
        self.nds = 0
        self.dsmap = {}
        self.dead = False

    def new_dsem(self):
        self.nds += 1
        ds = DSem(self.es.enter_context(self.nc.semaphore("d%d" % self.nds)))
        self.dsems.append(ds)
        self.dsmap[id(ds.sem)] = ds
        return ds

    def _wait(self, en, toks):
        E = self.E[en]
        for (sem, val, owner) in toks:
            if owner == en and (en == "pe" or not STRICT_SAME):
                continue
            k = id(sem)
            if k in self.dsmap and val > self.dsmap[k].maxw:
                self.dsmap[k].maxw = val
            if E["waited"].get(k, 0) >= val:
                continue
            E["eng"].wait_ge(sem, val)
            E["waited"][k] = val

    @staticmethod
    def _deps(r, w, en=None):
        toks = []
        for b in r:
            if b.lw:
                toks.append(b.lw)
            if b.excl:
                toks.extend(t for e_, t in b.rd.items() if e_ != en)
        for b in w:
            if b.lw:
                toks.append(b.lw)
            toks.extend(b.rd.values())
        return toks

    def op(self, en, fn, r=(), w=()):
        if self.dead:
            return
        self._wait(en, self._deps(r, w, en))
        E = self.E[en]
        ins = fn(E["eng"])
        E["count"] += 1
        ins.then_inc(E["sem"], 1)
        tok = (E["sem"], E["count"], en)
        for b in w:
            b.lw = tok
            b.rd = {}
        for b in r:
            b.rd[en] = tok

    def dma(self, qn, out, in_, ds, r=(), w=()):
        if self.dead:
            return
        self._wait(qn, self._deps(r, w))
        E = self.E[qn]
        assert ds.q in (None, qn), "DMA semaphore shared between queues"
        ds.q = qn
        if ds.maxw > 0:
            self._wait(qn, [(ds.sem, ds.maxw, None)])
        ins = E["eng"].dma_start(out=out, in_=in_)
        ds.count += 16
        ins.then_inc(ds.sem, 16)
        tok = (ds.sem, ds.count, None)
        for b in w:
            b.lw = tok
            b.rd = {}
        for b in r:
            b.rd["dma%d" % id(ds)] = tok

    def barrier(self):
        if self.dead:
            return
        for en, E in self.E.items():
            toks = []
            for en2, E2 in self.E.items():
                if en2 != en and E2["count"] > 0:
                    toks.append((E2["sem"], E2["count"], en2))
            for ds in self.dsems:
                if ds.count > 0:
                    toks.append((ds.sem, ds.count, None))
            self._wait(en, toks)


def pipeline(gens):
    it = iter(gens)
    active = []
    while True:
        g = next(it, None)
        if g is not None:
            active.append(g)
        if not active:
            break
        for g_ in list(active):
            try:
                next(g_)
            except StopIteration:
                active.remove(g_)


class _Stop(Exception):
    pass


def build_program(NB=4, NEXP=NEXP_FULL, debug=None, upto=99):
    nc = bass.Bass("TRN2", target_bir_lowering=False)

    def din(name, shape):
        return nc.dram_tensor(name, list(shape), F32, kind="ExternalInput").ap()

    x_d = din("x", (NB, SEQ, D))
    ctx_d = din("ctx", (NB, CTXL, D))
    cT_d = din("cT", (D, NB + 1))
    w_mod_d = din("w_mod", (D, 6 * D))
    b_mod_d = din("b_mod", (6 * D,))
    n1g_d = din("norm1_g", (D,))
    n2g_d = din("norm2_g", (D,))
    w_in_d = din("w_in", (D, D_IN))
    a2f_d = din("gla_a2_f", (16, 512))
    abf_d = din("gla_ab_f", (512,))
    a2b_d = din("gla_a2_b", (16, 512))
    abb_d = din("gla_ab_b", (512,))
    gn_d = din("gla_onorm_g", (HV,))
    pw_d = din("pool_w", (4, 128, 128))
    psc_d = din("pool_scale", (512,))
    wpb_d = din("w_pool_br", (512, D))
    wgb_d = din("w_gla_br", (D, D))
    wo_d = din("w_o", (D, D))
    rgw_d = din("router_grp_w", (D, 4))
    rgb_d = din("router_grp_b", (4,))
    rew_d = din("router_exp_w", (D, 32))
    reb_d = din("router_exp_b", (32,))
    w1_d = din("moe_w1", (NEXP_FULL, D, DE))
    w3_d = din("moe_w3", (NEXP_FULL, D, DE))
    w2_d = din("moe_w2", (NEXP_FULL, DE, D))
    fg_d = din("final_norm_g", (D,))
    cst_d = din("cst", (128, 898))
    poolP_d = din("poolP", (128, 512))
    sel_d = din("sel", (8, 8 * 128))
    out_d = nc.dram_tensor("out", [NB, SEQ, D], F32, kind="ExternalOutput").ap()
    dbg_d = None
    if debug:
        dbg_d = nc.dram_tensor("dbg", [128, debug], F32, kind="ExternalOutput").ap()

    NBC = NB + 1

    with contextlib.ExitStack() as es:
        S = Sched(nc, es)

        uid = [0]

        def sb(es_, name, shape, dt=F32):
            uid[0] += 1
            return es_.enter_context(nc.sbuf_tensor("%s_s%d" % (name, uid[0]), list(shape), dt))

        P = [es.enter_context(nc.psum_tensor("P%d" % i, [128, 512], F32)) for i in range(7)]
        PB = es.enter_context(nc.psum_tensor("PB", [128, 1024], BF16))
        Pb = [Buf(excl=True) for _ in range(7)]
        PBb = Buf(excl=True)

        cst = sb(es, "cst", (128, 898))
        identb = sb(es, "identb", (128, 128), BF16)
        poolP = sb(es, "poolP", (128, 512), BF16)
        sel = sb(es, "sel", (8, 1024))
        vstage = sb(es, "vstage", (68, 128))
        vT = sb(es, "vT", (128, 68))
        gnb = sb(es, "gnb", (128, HV))
        rbb = sb(es, "rbb", (128, 36))
        a2pf = sb(es, "a2pf", (32, 512), BF16)
        a2pb = sb(es, "a2pb", (32, 512), BF16)
        wlr = sb(es, "wlr", (128, KD, 32), BF16)
        rw = sb(es, "rw", (128, KD, 36), BF16)
        pwt = sb(es, "pwt", (128, 4, 128), BF16)
        sct = sb(es, "sct", (128, KD, NBC))
        modT = sb(es, "modT", (128, NBC, 48))
        msb = sb(es, "msb", (NBC, 2, D))
        A1 = sb(es, "A1", (128, NBC, KD))
        A2 = sb(es, "A2", (128, NBC, KD))
        cb = Buf()
        dsc = S.new_dsem()
        dscp = S.new_dsem()
        xid = [S.new_dsem() for _ in range(3)]
        wsem = S.new_dsem()
        wgsem = S.new_dsem()
        w2sem = S.new_dsem()
        dsl = S.new_dsem()
        dbgsem = S.new_dsem()
        wm13 = [S.new_dsem() for _ in range(2)]
        osem = [S.new_dsem() for _ in range(2)]

        ident = cst[:, 0:128]

        def tri(i):
            return cst[:, 128 + i * 128: 256 + i * 128]

        def msk(i):
            return cst[:, 640 + i * 128: 768 + i * 128]

        negcol = cst[:, 896:898]

        a2B = Buf()
        S.op("dve", lambda e: e.memset(a2pf[:], 0.0), w=[a2B])
        S.op("dve", lambda e: e.memset(a2pb[:], 0.0), w=[a2B])
        S.dma("sp", cst[:], cst_d, dsc, w=[cb])
        S.dma("sp", sel[:], sel_d, dsc, w=[cb])
        S.dma("sp", vstage[0:48, :], b_mod_d.rearrange("(j p) -> j p", p=128), dsc, w=[cb])
        S.dma("sp", vstage[48:56, :], n1g_d.rearrange("(j p) -> j p", p=128), dsc, w=[cb])
        S.dma("sp", vstage[56:64, :], n2g_d.rearrange("(j p) -> j p", p=128), dsc, w=[cb])
        S.dma("sp", vstage[64:68, :], psc_d.rearrange("(j p) -> j p", p=128), dsc, w=[cb])
        S.dma("sp", gnb[:], gn_d.rearrange("(o n) -> o n", o=1).to_broadcast([128, HV]), dsc, w=[cb])
        S.dma("sp", rbb[:, 0:4], rgb_d.rearrange("(o n) -> o n", o=1).to_broadcast([128, 4]), dsc, w=[cb])
        S.dma("sp", rbb[:, 4:36], reb_d.rearrange("(o n) -> o n", o=1).to_broadcast([128, 32]), dsc, w=[cb])
        S.dma("sp", sct[:], cT_d.rearrange("(k p) b -> p k b", p=128), dsc, w=[cb])
        S.dma("pool", a2pf[0:16, :], a2f_d, dscp, w=[cb, a2B])
        S.dma("pool", a2pb[16:32, :], a2b_d, dscp, w=[cb, a2B])
        S.dma("pool", poolP[:], poolP_d, dscp, w=[cb])
        S.dma("pool", wlr[:], w_in_d[:, C_LR:C_LR + 32].rearrange("(k p) n -> p k n", p=128), dscp, w=[cb])
        S.dma("pool", rw[:, :, 0:4], rgw_d.rearrange("(k p) n -> p k n", p=128), dscp, w=[cb])
        S.dma("pool", rw[:, :, 4:36], rew_d.rearrange("(k p) n -> p k n", p=128), dscp, w=[cb])
        S.dma("pool", pwt[:], pw_d.rearrange("g c e -> c g e"), dscp, w=[cb])
        S.op("dve", lambda e: e.tensor_copy(out=identb[:], in_=ident), r=[cb], w=[cb])
        S.op("act", lambda e: e.activation(out=sct[:], in_=sct[:], func=AF.Silu), r=[cb], w=[cb])
        S.op("pe", lambda e: e.transpose(P[0][:, 0:68], vstage[:], cst[0:68, 0:68]), r=[cb], w=[Pb[0]])
        S.op("dve", lambda e: e.tensor_copy(out=vT[:], in_=P[0][:, 0:68]), r=[Pb[0]], w=[cb])

        with contextlib.ExitStack() as p0:
            wm = [sb(p0, "wm%d" % i, (128, KD, 512)) for i in range(2)]
            bm5 = sb(p0, "bm5", (NBC, 2, D))
            for ci, ch in enumerate((2, 5)):
                S.dma("sp", bm5[:, ci, :],
                      b_mod_d[ch * D:(ch + 1) * D].rearrange("(o n) -> o n", o=1).to_broadcast([NBC, D]),
                      dsl, w=[cb])
            wmb = bufs(2)
            wmd = [S.new_dsem() for _ in range(2)]
            for cg in range(12):
                i = cg % 2
                S.dma("sp", wm[i][:], w_mod_d[:, cg * 512:(cg + 1) * 512].rearrange("(k p) n -> p k n", p=128),
                      wmd[i], w=[wmb[i]])
                for m in range(4):
                    j = cg * 4 + m
                    for k in range(KD):
                        S.op("pe", lambda e, i=i, m=m, k=k, j=j: e.matmul(
                            P[1][:, j * 8:j * 8 + NBC], lhsT=wm[i][:, k, m * 128:(m + 1) * 128],
                            rhs=sct[:, k, :], start=(k == 0), stop=(k == KD - 1)),
                            r=[wmb[i], cb], w=[Pb[1]])
                if cg // 2 in (2, 5):
                    ci = 0 if cg // 2 == 2 else 1
                    hf = cg % 2
                    for k in range(KD):
                        S.op("pe", lambda e, i=i, k=k: e.matmul(
                            P[2][0:NBC, :], lhsT=sct[:, k, :], rhs=wm[i][:, k, :],
                            start=(k == 0), stop=(k == KD - 1)), r=[wmb[i], cb], w=[Pb[2]])
                    S.op("dve", lambda e, ci=ci, hf=hf: e.tensor_tensor(
                        out=msb[:, ci, hf * 512:(hf + 1) * 512], in0=P[2][0:NBC, :],
                        in1=bm5[:, ci, hf * 512:(hf + 1) * 512], op=ALU.add), r=[Pb[2], cb], w=[cb])
            p1v = P[1][:, 0:384].rearrange("p (j e) -> p j e", e=8)
            for b in range(NBC):
                S.op("dve", lambda e, b=b: e.tensor_tensor(out=modT[:, b, :], in0=p1v[:, :, b], in1=vT[:, 0:48],
                                                           op=ALU.add), r=[Pb[1], cb], w=[cb])
            for b in range(NBC):
                S.op("dve", lambda e, b=b: e.scalar_tensor_tensor(
                    out=A1[:, b, :], in0=modT[:, b, 8:16], scalar=1.0, op0=ALU.add, in1=vT[:, 48:56], op1=ALU.mult),
                    r=[cb], w=[cb])
                S.op("dve", lambda e, b=b: e.scalar_tensor_tensor(
                    out=A2[:, b, :], in0=modT[:, b, 32:40], scalar=1.0, op0=ALU.add, in1=vT[:, 56:64], op1=ALU.mult),
                    r=[cb], w=[cb])
            S.barrier()

        dbg_off = [0]

        def dump(ap_, n, rb, np_=128):
            if dbg_d is None:
                return
            o = dbg_off[0]
            S.dma("pool", dbg_d[0:np_, o:o + n], ap_, dbgsem, r=rb)
            dbg_off[0] = o + n

        def rms_rstd(es_unused, src_ap, width, ss, rs, junk, rb, sb_):
            S.op("act", lambda e: e.activation(out=junk, in_=src_ap, func=AF.Square, accum_out=ss[:, 0:1]),
                 r=rb, w=[sb_])
            S.op("act", lambda e: e.activation(out=rs[:, 0:1], in_=ss[:, 0:1], func=AF.Sqrt, bias=EPS,
                                               scale=1.0 / width), r=[sb_], w=[sb_])
            S.op("dve", lambda e: e.reciprocal(out=rs[:, 0:1], in_=rs[:, 0:1]), r=[sb_], w=[sb_])

        try:
            for b in range(NB):
                with contextlib.ExitStack() as eb:
                    acc = sb(eb, "acc", (128, NXT, D))
                    accb = bufs(NXT)
                    with contextlib.ExitStack() as e13:
                        hT = sb(e13, "hT", (128, KD, TOK), BF16)
                        hTb = bufs(NTT)
                        lrT = sb(e13, "lrT", (32, TOK), BF16)
                        lrb = Buf()
                        with contextlib.ExitStack() as e1:
                            xin = [sb(e1, "xin%d" % i, (128, D)) for i in range(3)]
                            xib = bufs(3)
                            junk = sb(e1, "junk1", (128, D), BF16)
                            st = [sb(e1, "st1_%d" % i, (128, 2)) for i in range(2)]
                            stb = bufs(2)
                            jb = Buf()

                            def load_x(tt):
                                i = tt % 3
                                src = ctx_d[b, tt * 128:(tt + 1) * 128, :] if tt < NCT else \
                                    x_d[b, (tt - NCT) * 128:(tt - NCT + 1) * 128, :]
                                S.dma("sp", xin[i][:], src, xid[i], w=[xib[i]])

                            def p1_body(tt):
                                if tt + 2 < NTT:
                                    load_x(tt + 2)
                                i = tt % 3
                                s_ = st[tt % 2]
                                sbb = stb[tt % 2]
                                mb = NB if tt < NCT else b
                                S.op("act", lambda e: e.activation(
                                    out=junk[:], in_=xin[i][:], func=AF.Square, accum_out=s_[:, 0:1]),
                                    r=[xib[i]], w=[sbb, jb])
                                S.op("act", lambda e: e.activation(
                                    out=s_[:, 1:2], in_=s_[:, 0:1], func=AF.Sqrt, bias=EPS, scale=1.0 / D),
                                    r=[sbb], w=[sbb])
                                S.op("dve", lambda e: e.reciprocal(out=s_[:, 1:2], in_=s_[:, 1:2]),
                                     r=[sbb], w=[sbb])
                                S.op("act", lambda e: e.activation(
                                    out=xin[i][:], in_=xin[i][:], func=AF.Copy, scale=s_[:, 1:2]),
                                    r=[sbb, xib[i]], w=[xib[i]])
                                yield
                                pbase = (tt % 2) * 2
                                for k in range(KD):
                                    bk = pbase + k // 4
                                    S.op("pe", lambda e, k=k, bk=bk: e.transpose(
                                        P[bk][:, (k % 4) * 128:(k % 4 + 1) * 128], xin[i][:, k * 128:(k + 1) * 128], ident),
                                        r=[xib[i], cb], w=[Pb[bk]])
                                yield
                                for k in range(KD):
                                    bk = pbase + k // 4
                                    src = P[bk][:, (k % 4) * 128:(k % 4 + 1) * 128]
                                    dst = hT[:, k, tt * 128:(tt + 1) * 128]
                                    if k // 4 == 0:
                                        S.op("act", lambda e, src=src, dst=dst, k=k: e.activation(
                                            out=dst, in_=src, func=AF.Identity, scale=A1[:, mb, k:k + 1],
                                            bias=modT[:, mb, k:k + 1]), r=[Pb[bk], cb], w=[hTb[tt]])
                                    else:
                                        S.op("dve", lambda e, src=src, dst=dst, k=k: e.tensor_scalar(
                                            out=dst, in0=src, scalar1=A1[:, mb, k:k + 1], scalar2=modT[:, mb, k:k + 1],
                                            op0=ALU.mult, op1=ALU.add), r=[Pb[bk], cb], w=[hTb[tt]])

                            load_x(0)
                            load_x(1)
                            pipeline(p1_body(tt) for tt in range(NTT))
                            S.barrier()
                            if upto == 1:
                                S.dead = True
                        with contextlib.ExitStack() as e23:
                            whd = sb(e23, "whd", (128, KD, 768), BF16)
                            abfb = sb(e23, "abfb", (128, 512))
                            abbb = sb(e23, "abbb", (128, 512))
                            abB = Buf()
                            S.dma("sp", abfb[:], abf_d.rearrange("(o n) -> o n", o=1).to_broadcast([128, 512]), dsl, w=[abB])
                            S.dma("sp", abbb[:], abb_d.rearrange("(o n) -> o n", o=1).to_broadcast([128, 512]), dsl, w=[abB])
                            wgl = sb(e23, "wgl", (128, 2, D), BF16)
                            whb = Buf()
                            qT = sb(e23, "qT", (128, SEQ), BF16)
                            qTb = bufs(4)
                            kT = sb(e23, "kT", (128, TOK), BF16)
                            kTb = bufs(5)
                            ktok = sb(e23, "ktok", (128, NTT, 128), BF16)
                            ktb = bufs(3)
                            vtok = sb(e23, "vtok", (128, NTT, HV), BF16)
                            vtb = bufs(NTT)
                            sg = sb(e23, "sg", (128, NXT, HV), BF16)
                            sgb = bufs(NXT)
                            SBs = sb(e23, "SBs", (128, NXT, HV), BF16)
                            SBb = bufs(NXT)
                            SF = [sb(e23, "SF%d" % i, (128, HV), BF16) for i in range(2)]
                            SFb = bufs(2)
                            Sst = sb(e23, "Sst", (128, HV))
                            Sstb = Buf()
                            NSL = 3
                            NBIG = 2
                            zz = [sb(e23, "zz%d" % p_, (128, 256)) for p_ in range(NBIG)]
                            zzb = bufs(NBIG)
                            EE = [sb(e23, "EE%d" % p_, (128, 386)) for p_ in range(NBIG)]
                            EEb = bufs(NBIG)
                            E2 = [sb(e23, "E2_%d" % p_, (128, 256)) for p_ in range(NBIG)]
                            E2b = bufs(NBIG)
                            NDEC = 6
                            decs = sb(e23, "decs", (128, NDEC))
                            decb = bufs(NDEC)
                            kr = [sb(e23, "kr%d" % p_, (128, 128), BF16) for p_ in range(NSL)]
                            krb = bufs(NSL)
                            abh = sb(e23, "abh", (128, 256))
                            abhb = Buf()
                            qd = [[sb(e23, "qd%d%d" % (p_, d_), (128, 128), BF16) for d_ in range(2)] for p_ in range(NSL)]
                            qdb = [bufs(2) for _ in range(NSL)]
                            ki = [[sb(e23, "ki%d%d" % (p_, d_), (128, 128), BF16) for d_ in range(2)] for p_ in range(NSL)]
                            kib = [bufs(2) for _ in range(NSL)]
                            sT = [[sb(e23, "sT%d%d" % (p_, d_), (128, 128), BF16) for d_ in range(2)] for p_ in range(NSL)]
                            sTb = [bufs(2) for _ in range(NSL)]
                            otmp = [sb(e23, "otmp%d" % p_, (128, HV)) for p_ in range(NBIG)]
                            otb = bufs(NBIG)
                            og = [sb(e23, "og%d" % p_, (128, HV), BF16) for p_ in range(NBIG)]
                            ogb = bufs(NBIG)
                            ogT = [sb(e23, "ogT%d" % p_, (128, 2, 128), BF16) for p_ in range(NBIG)]
                            ogTb = bufs(NBIG)
                            st2 = [sb(e23, "st2_%d" % p_, (128, 2)) for p_ in range(NSL)]
                            st2b = bufs(NSL)
                            junk2 = sb(e23, "junk2", (128, HV), BF16)
                            vglock = Buf()
                            j2b = Buf()
                            nbk = [0]

                            def rot():
                                v_ = nbk[0]
                                nbk[0] = (v_ + 1) % 7
                                return v_

                            def kgrp(tt):
                                return 0 if tt < NCT else 1 + (tt - NCT) // 4

                            tokgroups = [(0, 256)] + [(256 + g * 512, 512) for g in range(4)]

                            wglb = Buf()

                            def load_whd(h_):
                                for (dst0, src0, n_) in ((0, C_Q + h_ * 128, 128), (128, C_K + h_ * 128, 128),
                                                         (256, C_V + h_ * 256, 256), (512, C_G + h_ * 256, 256)):
                                    S.dma("pool", whd[:, :, dst0:dst0 + n_],
                                          w_in_d[:, src0:src0 + n_].rearrange("(k p) n -> p k n", p=128), wsem, w=[whb])

                            def load_wgl(h_):
                                S.dma("pool", wgl[:], wgb_d[h_ * 256:(h_ + 1) * 256, :].rearrange("(c p) n -> p c n", p=128),
                                      wgsem, w=[wglb])

                            for h in range(NHEAD):
                                if h == 0:
                                    load_whd(0)
                                    load_wgl(0)
                                for g in range(4):
                                    bk = rot()
                                    for k in range(KD):
                                        S.op("pe", lambda e, k=k, g=g, bk=bk: e.matmul(
                                            P[bk][:, :], lhsT=whd[:, k, 0:128], rhs=hT[:, k, 256 + g * 512:768 + g * 512],
                                            start=(k == 0), stop=(k == KD - 1)),
                                            r=[whb] + hTb[2 + 4 * g:6 + 4 * g], w=[Pb[bk]])
                                    S.op("act", lambda e, g=g, bk=bk: e.activation(
                                        out=qT[:, g * 512:(g + 1) * 512], in_=P[bk][:, :], func=AF.Copy, scale=HK ** -0.5),
                                        r=[Pb[bk]], w=[qTb[g]])
                                for g, (t0, n_) in enumerate(tokgroups):
                                    bk = rot()
                                    for k in range(KD):
                                        S.op("pe", lambda e, k=k, bk=bk, t0=t0, n_=n_: e.matmul(
                                            P[bk][:, 0:n_], lhsT=whd[:, k, 128:256], rhs=hT[:, k, t0:t0 + n_],
                                            start=(k == 0), stop=(k == KD - 1)),
                                            r=[whb] + hTb[t0 // 128:(t0 + n_) // 128], w=[Pb[bk]])
                                    S.op("dve", lambda e, bk=bk, t0=t0, n_=n_: e.tensor_copy(
                                        out=kT[:, t0:t0 + n_], in_=P[bk][:, 0:n_]), r=[Pb[bk]], w=[kTb[g]])
                                    if h == 0:
                                        bk = rot()
                                        for k in range(KD):
                                            S.op("pe", lambda e, k=k, bk=bk, t0=t0, n_=n_: e.matmul(
                                                P[bk][0:32, 0:n_], lhsT=wlr[:, k, :], rhs=hT[:, k, t0:t0 + n_],
                                                start=(k == 0), stop=(k == KD - 1)),
                                                r=[cb] + hTb[t0 // 128:(t0 + n_) // 128], w=[Pb[bk]])
                                        S.op("act", lambda e, bk=bk, t0=t0, n_=n_: e.activation(
                                            out=lrT[:, t0:t0 + n_], in_=P[bk][0:32, 0:n_], func=AF.Copy),
                                            r=[Pb[bk]], w=[lrb])
                                if upto == 20 and h == 0:
                                    S.dead = True
                                for bi, (tt0, nt_) in enumerate(((0, 8), (8, 8), (16, 2))):
                                    for j in range(nt_):
                                        tt = tt0 + j
                                        S.op("pe", lambda e, j=j, tt=tt: e.transpose(
                                            PB[:, j * 128:(j + 1) * 128], kT[:, tt * 128:(tt + 1) * 128], identb[:]),
                                            r=[kTb[kgrp(tt)], cb], w=[PBb])
                                    S.op("act" if bi % 2 else "dve", (lambda e, tt0=tt0, nt_=nt_: e.activation(
                                        out=ktok[:, tt0:tt0 + nt_, :], in_=PB[:, 0:nt_ * 128].rearrange("p (t d) -> p t d", d=128),
                                        func=AF.Copy)) if bi % 2 else (lambda e, tt0=tt0, nt_=nt_: e.tensor_copy(
                                            out=ktok[:, tt0:tt0 + nt_, :],
                                            in_=PB[:, 0:nt_ * 128].rearrange("p (t d) -> p t d", d=128))),
                                        r=[PBb], w=[ktb[bi]])
                                if upto == 21 and h == 0:
                                    S.dead = True
                                for tt in range(NTT):
                                    bk = rot()
                                    n_ = 256 if tt < NCT else 512
                                    for k in range(KD):
                                        S.op("pe", lambda e, k=k, bk=bk, tt=tt, n_=n_: e.matmul(
                                            P[bk][:, 0:n_], lhsT=hT[:, k, tt * 128:(tt + 1) * 128], rhs=whd[:, k, 256:256 + n_],
                                            start=(k == 0), stop=(k == KD - 1)), r=[whb, hTb[tt]], w=[Pb[bk]])
                                    S.op("dve", lambda e, bk=bk, tt=tt: e.tensor_copy(out=vtok[:, tt, :], in_=P[bk][:, 0:256]),
                                         r=[Pb[bk]], w=[vtb[tt], vglock])
                                    if tt >= NCT:
                                        S.op("act", lambda e, bk=bk, tt=tt: e.activation(
                                            out=sg[:, tt - NCT, :], in_=P[bk][:, 256:512], func=AF.Silu),
                                            r=[Pb[bk]], w=[sgb[tt - NCT], vglock])

                                if h + 1 < NHEAD:
                                    load_whd(h + 1)
                                if upto == 22 and h == 0:
                                    S.dead = True
                                S.op("dve", lambda e: e.tensor_copy(out=abh[:, 0:128], in_=abfb[:, h * 128:(h + 1) * 128]),
                                     r=[abB], w=[abhb])
                                S.op("dve", lambda e: e.tensor_copy(out=abh[:, 128:256], in_=abbb[:, h * 128:(h + 1) * 128]),
                                     r=[abB], w=[abhb])

                                def la_stage(tt, dirs, ix):
                                    lo, hi = dirs[0] * 128, (dirs[-1] + 1) * 128
                                    for di in dirs:
                                        a2p = a2pf if di == 0 else a2pb
                                        S.op("pe", lambda e, di=di, a2p=a2p: e.matmul(
                                            P[0][:, di * 128:(di + 1) * 128], lhsT=lrT[:, tt * 128:(tt + 1) * 128],
                                            rhs=a2p[:, h * 128:(h + 1) * 128], start=True, stop=True),
                                            r=[lrb, cb], w=[Pb[0]])
                                    yield
                                    S.op("dve", lambda e: e.tensor_tensor(out=zz[ix % NBIG][:, lo:hi], in0=P[0][:, lo:hi],
                                                                          in1=abh[:, lo:hi], op=ALU.add),
                                         r=[Pb[0], abhb], w=[zzb[ix % NBIG]])
                                    yield
                                    S.op("act", lambda e: e.activation(out=zz[ix % NBIG][:, lo:hi], in_=zz[ix % NBIG][:, lo:hi],
                                                                       func=AF.Exp, scale=-1.0), r=[zzb[ix % NBIG]], w=[zzb[ix % NBIG]])
                                    S.op("act", lambda e: e.activation(out=zz[ix % NBIG][:, lo:hi], in_=zz[ix % NBIG][:, lo:hi],
                                                                       func=AF.Ln, bias=1.0), r=[zzb[ix % NBIG]], w=[zzb[ix % NBIG]])
                                    yield

                                def rem_tot(di, ix):
                                    trm = tri(1) if di == 0 else tri(3)
                                    sp_ = zz[ix % NBIG][:, di * 128:(di + 1) * 128]
                                    S.op("pe", lambda e: e.matmul(P[1][:, 256:384], lhsT=trm, rhs=sp_,
                                                                  start=True, stop=True), r=[zzb[ix % NBIG], cb], w=[Pb[1]])
                                    S.op("pe", lambda e: e.matmul(P[1][:, 384:386], lhsT=sp_, rhs=negcol,
                                                                  start=True, stop=True), r=[zzb[ix % NBIG], cb], w=[Pb[1]])

                                def state_post(tt, ix, dst, dstb, bk=6, c0=0):
                                    S.op("pe", lambda e: e.matmul(P[bk][:, c0:c0 + HV], lhsT=kr[ix % NSL][:], rhs=vtok[:, tt, :],
                                                                  start=True, stop=True), r=[krb[ix % NSL], vtb[tt]], w=[Pb[bk]])
                                    yield
                                    S.op("dve", lambda e: e.scalar_tensor_tensor(
                                        out=Sst[:], in0=Sst[:], scalar=decs[:, ix % NDEC:ix % NDEC + 1], op0=ALU.mult,
                                        in1=P[bk][:, c0:c0 + HV], op1=ALU.add), r=[Sstb, decb[ix % NDEC], Pb[bk]], w=[Sstb])
                                    if dst is not None:
                                        S.op("dve", lambda e: e.tensor_copy(out=dst, in_=Sst[:]), r=[Sstb], w=[dstb])

                                def st_body(di, tt, ix, dst, dstb):
                                    yield from la_stage(tt, (di,), ix)
                                    rem_tot(di, ix)
                                    yield
                                    S.op("act", lambda e: e.activation(out=EE[ix % NBIG][:, 256:386], in_=P[1][:, 256:386], func=AF.Exp),
                                         r=[Pb[1]], w=[EEb[ix % NBIG]])
                                    yield
                                    S.op("dve", lambda e: e.tensor_copy(out=decs[:, ix % NDEC:ix % NDEC + 1], in_=EE[ix % NBIG][:, 384:385]),
                                         r=[EEb[ix % NBIG]], w=[decb[ix % NDEC]])
                                    S.op("dve", lambda e: e.tensor_tensor(out=kr[ix % NSL][:], in0=ktok[:, tt, :],
                                                                          in1=EE[ix % NBIG][:, 256:384], op=ALU.mult),
                                         r=[ktb[tt // 8], EEb[ix % NBIG]], w=[krb[ix % NSL]])
                                    yield
                                    yield from state_post(tt, ix, dst, dstb)

                                def f_body(n, ix):
                                    tt = n + NCT
                                    cur = n % 2
                                    yield from la_stage(tt, (0, 1), ix)
                                    for di in (0, 1):
                                        tin = tri(0) if di == 0 else tri(2)
                                        S.op("pe", lambda e, di=di, tin=tin: e.matmul(
                                            P[1][:, di * 128:(di + 1) * 128], lhsT=zz[ix % NBIG][:, di * 128:(di + 1) * 128], rhs=tin,
                                            start=True, stop=True), r=[zzb[ix % NBIG], cb], w=[Pb[1]])
                                    rem_tot(0, ix)
                                    yield
                                    S.op("act", lambda e: e.activation(out=EE[ix % NBIG][:, 0:386], in_=P[1][:, 0:386], func=AF.Exp),
                                         r=[Pb[1]], w=[EEb[ix % NBIG]])
                                    S.op("act", lambda e: e.activation(out=E2[ix % NBIG][:, 0:256], in_=P[1][:, 0:256], func=AF.Exp,
                                                                       scale=-1.0), r=[Pb[1]], w=[E2b[ix % NBIG]])
                                    yield
                                    for di in (0, 1):
                                        S.op("dve", lambda e, di=di: e.tensor_tensor(
                                            out=qd[ix % NSL][di][:], in0=qT[:, n * 128:(n + 1) * 128],
                                            in1=EE[ix % NBIG][:, di * 128:(di + 1) * 128],
                                            op=ALU.mult), r=[qTb[n // 4], EEb[ix % NBIG]], w=[qdb[ix % NSL][di]])
                                        S.op(POOLENG, lambda e, di=di: e.tensor_tensor(
                                            out=ki[ix % NSL][di][:], in0=kT[:, tt * 128:(tt + 1) * 128],
                                            in1=E2[ix % NBIG][:, di * 128:(di + 1) * 128],
                                            op=ALU.mult), r=[kTb[kgrp(tt)], E2b[ix % NBIG]], w=[kib[ix % NSL][di]])
                                    if n < NXT - 1:
                                        S.op("dve", lambda e: e.tensor_copy(out=decs[:, ix % NDEC:ix % NDEC + 1], in_=EE[ix % NBIG][:, 384:385]),
                                         r=[EEb[ix % NBIG]], w=[decb[ix % NDEC]])
                                    S.op("dve", lambda e: e.tensor_tensor(out=kr[ix % NSL][:], in0=ktok[:, tt, :],
                                                                              in1=EE[ix % NBIG][:, 256:384], op=ALU.mult),
                                             r=[ktb[tt // 8], EEb[ix % NBIG]], w=[krb[ix % NSL]])
                                    yield
                                    for di in (0, 1):
                                        S.op("pe", lambda e, di=di: e.matmul(
                                            P[2][:, di * 128:(di + 1) * 128], lhsT=ki[ix % NSL][di][:], rhs=qd[ix % NSL][di][:],
                                            start=True, stop=True), r=[kib[ix % NSL][di], qdb[ix % NSL][di]], w=[Pb[2]])
                                    yield
                                    for di in (0, 1):
                                        S.op("dve", lambda e, di=di: e.tensor_tensor(
                                            out=sT[ix % NSL][di][:], in0=P[2][:, di * 128:(di + 1) * 128], in1=msk(di),
                                            op=ALU.mult), r=[Pb[2], cb], w=[sTb[ix % NSL][di]])
                                    yield
                                    ops_o = ((sT[ix % NSL][0][:], vtok[:, tt, :], [sTb[ix % NSL][0], vtb[tt]]),
                                             (qd[ix % NSL][0][:], SF[cur][:], [qdb[ix % NSL][0], SFb[cur]]),
                                             (sT[ix % NSL][1][:], vtok[:, tt, :], [sTb[ix % NSL][1], vtb[tt]]),
                                             (qd[ix % NSL][1][:], SBs[:, n, :], [qdb[ix % NSL][1], SBb[n]]))
                                    ob = 3 if ix % 2 == 0 else 6
                                    for oi, (l_, r_, rb_) in enumerate(ops_o):
                                        S.op("pe", lambda e, l_=l_, r_=r_, oi=oi: e.matmul(
                                            P[ob][:, 0:HV], lhsT=l_, rhs=r_, start=(oi == 0), stop=(oi == 3)),
                                            r=rb_, w=[Pb[ob]])
                                    if n < NXT - 1:
                                        spost = state_post(tt, ix, SF[1 - cur][:], SFb[1 - cur], bk=ob, c0=256)
                                        next(spost)
                                    else:
                                        spost = iter(())
                                    yield
                                    S.op("act", lambda e: e.activation(out=junk2[:], in_=P[ob][:, 0:HV], func=AF.Square,
                                                                       accum_out=st2[ix % NSL][:, 0:1]),
                                         r=[Pb[ob]], w=[st2b[ix % NSL], j2b])
                                    S.op("act", lambda e: e.activation(out=st2[ix % NSL][:, 1:2], in_=st2[ix % NSL][:, 0:1], func=AF.Ln,
                                                                       bias=EPS, scale=1.0 / HV), r=[st2b[ix % NSL]], w=[st2b[ix % NSL]])
                                    S.op("act", lambda e: e.activation(out=st2[ix % NSL][:, 1:2], in_=st2[ix % NSL][:, 1:2], func=AF.Exp,
                                                                       scale=-0.5), r=[st2b[ix % NSL]], w=[st2b[ix % NSL]])
                                    next(spost, None)
                                    yield
                                    S.op("dve", lambda e: e.scalar_tensor_tensor(
                                        out=otmp[ix % NBIG][:], in0=P[ob][:, 0:HV], scalar=st2[ix % NSL][:, 1:2], op0=ALU.mult,
                                        in1=gnb[:], op1=ALU.mult), r=[Pb[ob], st2b[ix % NSL], cb], w=[otb[ix % NBIG]])
                                    S.op(POOLENG, lambda e: e.tensor_tensor(out=og[ix % NBIG][:], in0=otmp[ix % NBIG][:], in1=sg[:, n, :],
                                                                           op=ALU.mult), r=[otb[ix % NBIG], sgb[n]], w=[ogb[ix % NBIG]])
                                    yield
                                    for c in range(2):
                                        S.op("pe", lambda e, c=c: e.transpose(PB[:, c * 128:(c + 1) * 128],
                                                                              og[ix % NBIG][:, c * 128:(c + 1) * 128], identb[:]),
                                             r=[ogb[ix % NBIG], cb], w=[PBb])
                                    yield
                                    S.op("act", lambda e: e.activation(
                                        out=ogT[ix % NBIG][:, :, :], in_=PB[:, 0:256].rearrange("p (c t) -> p c t", t=128),
                                        func=AF.Copy), r=[PBb], w=[ogTb[ix % NBIG]])
                                    yield
                                    for hf in range(2):
                                        for c in range(2):
                                            S.op("pe", lambda e, hf=hf, c=c: e.matmul(
                                                P[4 + hf][:, :], lhsT=ogT[ix % NBIG][:, c, :], rhs=wgl[:, c, hf * 512:(hf + 1) * 512],
                                                start=(c == 0), stop=(c == 1)), r=[ogTb[ix % NBIG], wglb], w=[Pb[4 + hf]])
                                    yield
                                    for hf in range(2):
                                        dst = acc[:, n, hf * 512:(hf + 1) * 512]
                                        if h == 0:
                                            S.op("act", lambda e, hf=hf, dst=dst: e.activation(out=dst, in_=P[4 + hf][:, :],
                                                                                               func=AF.Copy),
                                                 r=[Pb[4 + hf]], w=[accb[n]])
                                        else:
                                            S.op("dve", lambda e, hf=hf, dst=dst: e.tensor_tensor(
                                                out=dst, in0=dst, in1=P[4 + hf][:, :], op=ALU.add),
                                                r=[Pb[4 + hf], accb[n]], w=[accb[n]])

                                S.op("dve", lambda e: e.memset(Sst[:], 0.0), w=[Sstb])
                                gens = [st_body(1, 1, 0, None, None), st_body(1, 0, 1, SBs[:, NXT - 1, :], SBb[NXT - 1])]
                                for n in range(NXT - 1, 0, -1):
                                    gens.append(st_body(1, n + NCT, len(gens), SBs[:, n - 1, :], SBb[n - 1]))
                                pipeline(gens)
                                if upto == 23 and h == 0:
                                    S.dead = True
                                S.op("dve", lambda e: e.memset(Sst[:], 0.0), w=[Sstb])
                                gens = [st_body(0, 0, 0, None, None), st_body(0, 1, 1, SF[0][:], SFb[0])]
                                for n in range(NXT):
                                    gens.append(f_body(n, len(gens)))
                                pipeline(gens)
                                if h + 1 < NHEAD:
                                    load_wgl(h + 1)
                            S.barrier()
                            if upto == 2:
                                S.dead = True
                        S.barrier()
                    h2T = sb(eb, "h2T", (128, NXT, KD, 128), BF16)
                    h2b = bufs(NXT)
                    cw = sb(eb, "cw", (128, NXT, 32))
                    cwb = bufs(NXT)

                    def bcast_row(dst, ci):
                        for hf in range(2):
                            S.op("pe", lambda e, hf=hf: e.matmul(
                                P[hf][:, :], lhsT=sel[0:NBC, b * 128:(b + 1) * 128], rhs=msb[:, ci, hf * 512:(hf + 1) * 512],
                                start=True, stop=True), r=[cb], w=[Pb[hf]])
                            S.op("act", lambda e, hf=hf: e.activation(out=dst[:, hf * 512:(hf + 1) * 512], in_=P[hf][:, :],
                                                                      func=AF.Copy), r=[Pb[hf]], w=[cb])

                    with contextlib.ExitStack() as e4:
                        wu = sb(e4, "wu", (128, KD, 512), BF16)
                        wgt = sb(e4, "wgt", (128, KD, 2048), BF16)
                        wpb = sb(e4, "wpb", (128, 4, D), BF16)
                        w4b = Buf()
                        S.dma("pool", wu[:], w_in_d[:, C_POOL:C_POOL + 512].rearrange("(k p) n -> p k n", p=128), wsem, w=[w4b])
                        for j in range(4):
                            S.dma("pool", wgt[:, :, j * 512:(j + 1) * 512],
                                  w_in_d[:, C_GATES + j * 512:C_GATES + (j + 1) * 512].rearrange("(k p) n -> p k n", p=128),
                                  wsem, w=[w4b])
                        S.dma("pool", wpb[:], wpb_d.rearrange("(g p) n -> p g n", p=128), wsem, w=[w4b])
                        xin = [sb(e4, "xin4_%d" % i, (128, D)) for i in range(2)]
                        xib = bufs(2)
                        xs = sb(e4, "xs4", (128, D))
                        xsb = Buf()
                        junk = sb(e4, "junk4", (128, D), BF16)
                        jb = Buf()
                        st = [sb(e4, "st4_%d" % i, (128, 2)) for i in range(2)]
                        stb = bufs(2)
                        hTt = [sb(e4, "hTt%d" % i, (128, KD, 128), BF16) for i in range(2)]
                        hTtb = bufs(2)
                        u_sb = sb(e4, "u_sb", (128, 512), BF16)
                        ub = Buf()
                        pT = sb(e4, "pT", (128, 512), BF16)
                        pTb = Buf()
                        y1T = sb(e4, "y1T", (128, 4, 128), BF16)
                        y1b = Buf()
                        sgt = [sb(e4, "sgt%d" % i, (128, 512)) for i in range(2)]
                        sgtb = bufs(2)
                        t1 = sb(e4, "t1", (128, D))
                        t1b = bufs(2)
                        tmp = [sb(e4, "tmp4_%d" % i, (128, 512)) for i in range(2)]
                        tmpb = bufs(2)

                        def load_x4(n):
                            S.dma("sp", xin[n % 2][:], x_d[b, n * 128:(n + 1) * 128, :], xid[n % 2], w=[xib[n % 2]])

                        def p4a_body(n):
                            if n + 1 < NXT:
                                load_x4(n + 1)
                            i = n % 2
                            par = n % 2
                            S.op("act", lambda e: e.activation(out=junk[:], in_=xin[i][:], func=AF.Square,
                                                               accum_out=st[par][:, 0:1]), r=[xib[i]], w=[stb[par], jb])
                            S.op("act", lambda e: e.activation(out=st[par][:, 1:2], in_=st[par][:, 0:1], func=AF.Ln,
                                                               bias=EPS, scale=1.0 / D), r=[stb[par]], w=[stb[par]])
                            S.op("act", lambda e: e.activation(out=st[par][:, 1:2], in_=st[par][:, 1:2], func=AF.Exp,
                                                               scale=-0.5), r=[stb[par]], w=[stb[par]])
                            S.op("act", lambda e: e.activation(out=xs[:], in_=xin[i][:], func=AF.Copy, scale=st[par][:, 1:2]),
                                 r=[stb[par], xib[i]], w=[xsb])
                            yield
                            for k in range(KD):
                                S.op("pe", lambda e, k=k: e.transpose(P[k // 4][:, (k % 4) * 128:(k % 4 + 1) * 128],
                                                                      xs[:, k * 128:(k + 1) * 128], ident),
                                     r=[xsb, cb], w=[Pb[k // 4]])
                            yield
                            for k in range(KD):
                                src = P[k // 4][:, (k % 4) * 128:(k % 4 + 1) * 128]
                                dst = hTt[par][:, k, :]
                                if k // 4 == 0:
                                    S.op("act", lambda e, src=src, dst=dst, k=k: e.activation(
                                        out=dst, in_=src, func=AF.Identity, scale=A1[:, b, k:k + 1], bias=modT[:, b, k:k + 1]),
                                        r=[Pb[k // 4], cb], w=[hTtb[par]])
                                else:
                                    S.op("dve", lambda e, src=src, dst=dst, k=k: e.tensor_scalar(
                                        out=dst, in0=src, scalar1=A1[:, b, k:k + 1], scalar2=modT[:, b, k:k + 1],
                                        op0=ALU.mult, op1=ALU.add), r=[Pb[k // 4], cb], w=[hTtb[par]])
                            yield
                            for k in range(KD):
                                S.op("pe", lambda e, k=k: e.matmul(P[2][:, :], lhsT=hTt[par][:, k, :], rhs=wu[:, k, :],
                                                                   start=(k == 0), stop=(k == KD - 1)),
                                     r=[hTtb[par], w4b], w=[Pb[2]])
                            S.op("dve", lambda e: e.tensor_copy(out=u_sb[:], in_=P[2][:, :]), r=[Pb[2]], w=[ub])
                            for g in range(4):
                                S.op("pe", lambda e, g=g: e.matmul(P[3][:, g * 128:(g + 1) * 128],
                                                                   lhsT=u_sb[:, g * 128:(g + 1) * 128],
                                                                   rhs=poolP[:, g * 128:(g + 1) * 128], start=True, stop=True),
                                     r=[ub, cb], w=[Pb[3]])
                            S.op("act", lambda e: e.activation(out=pT[:], in_=P[3][:, :], func=AF.Copy), r=[Pb[3]], w=[pTb])
                            for g in range(4):
                                S.op("pe", lambda e, g=g: e.matmul(P[2][:, g * 128:(g + 1) * 128], lhsT=pwt[:, g, :],
                                                                   rhs=pT[:, g * 128:(g + 1) * 128], start=True, stop=True),
                                     r=[pTb, cb], w=[Pb[2]])
                            for g in range(4):
                                S.op("act", lambda e, g=g: e.activation(out=y1T[:, g, :], in_=P[2][:, g * 128:(g + 1) * 128],
                                                                        func=AF.Copy, scale=vT[:, 64 + g:65 + g]),
                                     r=[Pb[2], cb], w=[y1b])
                            for hf in range(2):
                                for g in range(4):
                                    S.op("pe", lambda e, hf=hf, g=g: e.matmul(
                                        P[4 + hf][:, :], lhsT=y1T[:, g, :], rhs=wpb[:, g, hf * 512:(hf + 1) * 512],
                                        start=(g == 0), stop=(g == 3)), r=[y1b, w4b], w=[Pb[4 + hf]])
                            yield
                            for j in range(4):
                                bk = 6 if j % 2 == 0 else 3
                                hf = j % 2
                                for k in range(KD):
                                    S.op("pe", lambda e, k=k, j=j, bk=bk: e.matmul(
                                        P[bk][:, :], lhsT=hTt[par][:, k, :], rhs=wgt[:, k, j * 512:(j + 1) * 512],
                                        start=(k == 0), stop=(k == KD - 1)), r=[hTtb[par], w4b], w=[Pb[bk]])
                                S.op("act", lambda e, bk=bk, hf=hf: e.activation(out=sgt[hf][:], in_=P[bk][:, :],
                                                                                 func=AF.Sigmoid),
                                     r=[Pb[bk]], w=[sgtb[hf]])
                                if j < 2:
                                    S.op("dve", lambda e, hf=hf: e.tensor_tensor(
                                        out=t1[:, hf * 512:(hf + 1) * 512], in0=sgt[hf][:], in1=P[4 + hf][:, :], op=ALU.mult),
                                        r=[sgtb[hf], Pb[4 + hf]], w=[t1b[hf]])
                                else:
                                    S.op("dve", lambda e, hf=hf: e.tensor_tensor(
                                        out=tmp[hf][:], in0=sgt[hf][:], in1=acc[:, n, hf * 512:(hf + 1) * 512], op=ALU.mult),
                                        r=[sgtb[hf], accb[n]], w=[tmpb[hf]])
                                    S.op("dve", lambda e, hf=hf: e.tensor_tensor(
                                        out=h2T[:, n, hf * 4:(hf + 1) * 4, :], in0=tmp[hf][:].rearrange("p (k t) -> p k t", t=128),
                                        in1=t1[:, hf * 512:(hf + 1) * 512].rearrange("p (k t) -> p k t", t=128), op=ALU.add),
                                        r=[tmpb[hf], t1b[hf]], w=[h2b[n]])

                        load_x4(0)
                        pipeline(p4a_body(n) for n in range(NXT))
                        S.barrier()
                        if upto == 3:
                            S.dead = True

                    with contextlib.ExitStack() as e4:
                        wo = sb(e4, "wo", (128, KD, D), BF16)
                        w4b = Buf()
                        S.dma("pool", wo[:], wo_d.rearrange("(k p) n -> p k n", p=128), wsem, w=[w4b])
                        g1b = sb(e4, "g1b", (128, D))
                        bcast_row(g1b, 0)
                        xin = [sb(e4, "xin5_%d" % i, (128, D)) for i in range(2)]
                        xib = bufs(2)
                        mTt = [sb(e4, "mTt%d" % i, (128, KD, 128), BF16) for i in range(2)]
                        mTb = bufs(2)
                        tmp2 = sb(e4, "tmp2", (128, D))
                        tmp2b = bufs(2)
                        xs = sb(e4, "xs5", (128, D))
                        xsb = Buf()
                        junk = sb(e4, "junk5", (128, D), BF16)
                        jb = Buf()
                        st = [sb(e4, "st5_%d" % i, (128, 2)) for i in range(2)]
                        stb = bufs(2)
                        lgall = sb(e4, "lgall", (128, NXT, 36))
                        lgb = Buf()
                        rs_ = sb(e4, "rs_", (128, 10, NXT))
                        rg_ = sb(e4, "rg_", (128, 3, NXT, 4))
                        re_ = sb(e4, "re_", (128, 5, NXT, 32))
                        rtb_ = Buf()

                        def load_x5(n):
                            S.dma("sp", xin[n % 2][:], x_d[b, n * 128:(n + 1) * 128, :], xid[n % 2], w=[xib[n % 2]])

                        def p4b_body(n):
                            i = n % 2
                            par = n % 2
                            mv = h2T[:, n, :, :]
                            for k in range(KD):
                                S.op("pe", lambda e, k=k: e.transpose(PB[:, k * 128:(k + 1) * 128], mv[:, k, :], identb[:]),
                                     r=[h2b[n], cb], w=[PBb])
                            yield
                            S.op("act", lambda e: e.activation(out=mTt[par][:, :, :],
                                                               in_=PB[:, :].rearrange("p (k t) -> p k t", t=128), func=AF.Copy),
                                 r=[PBb], w=[mTb[par]])
                            yield
                            if n + 1 < NXT:
                                load_x5(n + 1)
                            for hf in range(2):
                                for k in range(KD):
                                    S.op("pe", lambda e, hf=hf, k=k: e.matmul(
                                        P[hf][:, :], lhsT=mTt[par][:, k, :], rhs=wo[:, k, hf * 512:(hf + 1) * 512],
                                        start=(k == 0), stop=(k == KD - 1)), r=[mTb[par], w4b], w=[Pb[hf]])
                            yield
                            for hf in range(2):
                                sl = slice(hf * 512, (hf + 1) * 512)
                                S.op("dve", lambda e, hf=hf, sl=sl: e.tensor_tensor(out=tmp2[:, sl], in0=P[hf][:, :],
                                                                                    in1=g1b[:, sl], op=ALU.mult),
                                     r=[Pb[hf], cb], w=[tmp2b[hf]])
                                S.op("dve", lambda e, sl=sl: e.tensor_tensor(out=acc[:, n, sl], in0=tmp2[:, sl],
                                                                              in1=xin[i][:, sl], op=ALU.add),
                                     r=[tmp2b[hf], xib[i]], w=[accb[n]])
                            yield
                            S.op("act", lambda e: e.activation(out=junk[:], in_=acc[:, n, :], func=AF.Square,
                                                               accum_out=st[par][:, 0:1]), r=[accb[n]], w=[stb[par], jb])
                            S.op("act", lambda e: e.activation(out=st[par][:, 1:2], in_=st[par][:, 0:1], func=AF.Ln,
                                                               bias=EPS, scale=1.0 / D), r=[stb[par]], w=[stb[par]])
                            S.op("act", lambda e: e.activation(out=st[par][:, 1:2], in_=st[par][:, 1:2], func=AF.Exp,
                                                               scale=-0.5), r=[stb[par]], w=[stb[par]])
                            S.op("act", lambda e: e.activation(out=xs[:], in_=acc[:, n, :], func=AF.Copy,
                                                               scale=st[par][:, 1:2]), r=[stb[par], accb[n]], w=[xsb])
                            yield
                            for k in range(KD):
                                S.op("pe", lambda e, k=k: e.transpose(P[2 + k // 4][:, (k % 4) * 128:(k % 4 + 1) * 128],
                                                                      xs[:, k * 128:(k + 1) * 128], ident),
                                     r=[xsb, cb], w=[Pb[2 + k // 4]])
                            yield
                            for k in range(KD):
                                src = P[2 + k // 4][:, (k % 4) * 128:(k % 4 + 1) * 128]
                                dst = h2T[:, n, k, :]
                                if k // 4 == 0:
                                    S.op("act", lambda e, src=src, dst=dst, k=k: e.activation(
                                        out=dst, in_=src, func=AF.Identity, scale=A2[:, b, k:k + 1],
                                        bias=modT[:, b, 24 + k:25 + k]), r=[Pb[2 + k // 4], cb], w=[h2b[n]])
                                else:
                                    S.op("dve", lambda e, src=src, dst=dst, k=k: e.tensor_scalar(
                                        out=dst, in0=src, scalar1=A2[:, b, k:k + 1], scalar2=modT[:, b, 24 + k:25 + k],
                                        op0=ALU.mult, op1=ALU.add), r=[Pb[2 + k // 4], cb], w=[h2b[n]])
                            yield
                            for k in range(KD):
                                S.op("pe", lambda e, k=k: e.matmul(P[4][:, 0:36], lhsT=h2T[:, n, k, :], rhs=rw[:, k, :],
                                                                   start=(k == 0), stop=(k == KD - 1)),
                                     r=[h2b[n], cb], w=[Pb[4]])
                            yield
                            S.op("dve", lambda e: e.tensor_tensor(out=lgall[:, n, :], in0=P[4][:, 0:36], in1=rbb[:], op=ALU.add),
                                 r=[Pb[4], cb], w=[lgb])

                        load_x5(0)
                        pipeline(p4b_body(n) for n in range(NXT))
                        T_ = NXT
                        X = mybir.AxisListType.X
                        gl = lgall[:, :, 0:4]
                        el4 = lgall[:, :, 4:36].rearrange("p t (g e) -> p t g e", e=8)
                        gmax, ngs, pg, m1, m2, dd, e2, den, w1_, w2_ = (rs_[:, q, :] for q in range(10))
                        gm, pen, ge = (rg_[:, q, :, :] for q in range(3))
                        em, oh1, em2, oh2, cwt = (re_[:, q, :, :] for q in range(5))

                        def bc(ap2, width):
                            return ap2.unsqueeze(2).to_broadcast([128, T_, width])

                        def dv(fn):
                            S.op("dve", fn, r=[lgb, rtb_], w=[rtb_])

                        dv(lambda e: e.tensor_reduce(out=gmax, in_=gl, axis=X, op=ALU.max))
                        dv(lambda e: e.tensor_tensor(out=gm, in0=gl, in1=bc(gmax, 4), op=ALU.is_equal))
                        dv(lambda e: e.tensor_tensor(out=ge, in0=gl, in1=bc(gmax, 4), op=ALU.subtract))
                        S.op("act", lambda e: e.activation(out=ge, in_=ge, func=AF.Exp), r=[rtb_], w=[rtb_])
                        dv(lambda e: e.tensor_reduce(out=ngs, in_=ge, axis=X, op=ALU.add))
                        dv(lambda e: e.reciprocal(out=pg, in_=ngs))
                        dv(lambda e: e.tensor_scalar(out=pen, in0=gm, scalar1=-1.0, scalar2=1e30, op0=ALU.add, op1=ALU.mult))
                        em4 = em.rearrange("p t (g e) -> p t g e", e=8)
                        dv(lambda e: e.tensor_tensor(out=em4, in0=el4,
                                                     in1=pen.unsqueeze(3).to_broadcast([128, T_, 4, 8]), op=ALU.add))
                        dv(lambda e: e.tensor_reduce(out=m1, in_=em, axis=X, op=ALU.max))
                        dv(lambda e: e.tensor_tensor(out=oh1, in0=em, in1=bc(m1, 32), op=ALU.is_equal))
                        dv(lambda e: e.scalar_tensor_tensor(out=em2.rearrange("p t e -> p (t e)"),
                                                            in0=oh1.rearrange("p t e -> p (t e)"), scalar=-1e30, op0=ALU.mult,
                                                            in1=em.rearrange("p t e -> p (t e)"), op1=ALU.add))
                        dv(lambda e: e.tensor_reduce(out=m2, in_=em2, axis=X, op=ALU.max))
                        dv(lambda e: e.tensor_tensor(out=oh2, in0=em2, in1=bc(m2, 32), op=ALU.is_equal))
                        dv(lambda e: e.tensor_tensor(out=dd, in0=m2, in1=m1, op=ALU.subtract))
                        S.op("act", lambda e: e.activation(out=e2, in_=dd, func=AF.Exp), r=[rtb_], w=[rtb_])
                        dv(lambda e: e.tensor_scalar(out=den, in0=e2, scalar1=1.0, scalar2=None, op0=ALU.add))
                        dv(lambda e: e.reciprocal(out=den, in_=den))
                        dv(lambda e: e.tensor_tensor(out=w1_, in0=den, in1=pg, op=ALU.mult))
                        dv(lambda e: e.tensor_tensor(out=w2_, in0=w1_, in1=e2, op=ALU.mult))
                        dv(lambda e: e.tensor_tensor(out=cwt, in0=oh1, in1=bc(w1_, 32), op=ALU.mult))
                        dv(lambda e: e.tensor_tensor(out=oh2, in0=oh2, in1=bc(w2_, 32), op=ALU.mult))
                        S.op("dve", lambda e: e.tensor_tensor(out=cw[:, :, :], in0=cwt, in1=oh2, op=ALU.add),
                             r=[rtb_], w=cwb)
                        if debug:
                            for n in range(2):
                                dump(acc[:, n, :], D, accb)
                            dump(cw[:, 0, :], 32, cwb)
                            dump(cw[:, 1, :], 32, cwb)
                        S.barrier()
                        if upto == 4:
                            S.dead = True

                    with contextlib.ExitStack() as e5:
                        g2b = sb(e5, "g2b", (128, D))
                        bcast_row(g2b, 1)
                        w1b = [sb(e5, "w1b%d" % i, (128, KD, DE), BF16) for i in range(2)]
                        w3b = [sb(e5, "w3b%d" % i, (128, KD, DE), BF16) for i in range(2)]
                        w2s = sb(e5, "w2s", (128, 4, D))
                        w2b = [sb(e5, "w2b%d" % i, (128, 4, D), BF16) for i in range(2)]
                        wb13 = bufs(2)
                        w2sb = Buf()
                        w2bb = bufs(2)
                        sa = [sb(e5, "sa%d" % i, (128, 512)) for i in range(2)]
                        sab = bufs(2)
                        hid = [sb(e5, "hid%d" % i, (128, 4, 512), BF16) for i in range(2)]
                        hidb = bufs(2)

                        def load_w(e_):
                            sl_ = e_ % 2
                            S.dma("pool", w1b[sl_][:], w1_d[e_].rearrange("(k p) f -> p k f", p=128), wm13[sl_], w=[wb13[sl_]])
                            S.dma("pool", w3b[sl_][:], w3_d[e_].rearrange("(k p) f -> p k f", p=128), wm13[sl_], w=[wb13[sl_]])
                            S.dma("sp", w2s[:], w2_d[e_].rearrange("(c p) n -> p c n", p=128), w2sem, w=[w2sb])
                            for c in range(4):
                                S.op("dve", lambda e, c=c, sl_=sl_: e.tensor_tensor(out=w2b[sl_][:, c, :], in0=w2s[:, c, :],
                                                                                     in1=g2b[:], op=ALU.mult),
                                     r=[w2sb, cb], w=[w2bb[sl_]])

                        items = [(e_, tg) for e_ in range(NEXP) for tg in range(4)]

                        def emit_ab(idx):
                            e_, tg = items[idx]
                            sl_ = e_ % 2
                            hp = idx % 2
                            for f in range(4):
                                ba, bb_ = (0, 1) if f % 2 == 0 else (2, 3)
                                sp_ = f % 2
                                for (wsrc, bk) in ((w1b, ba), (w3b, bb_)):
                                    for k in range(KD):
                                        S.op("pe", lambda e, wsrc=wsrc, bk=bk, k=k, f=f: e.matmul(
                                            P[bk][:, :].rearrange("p (t d) -> p t d", d=128),
                                            lhsT=wsrc[sl_][:, k, f * 128:(f + 1) * 128],
                                            rhs=h2T[:, tg * 4:(tg + 1) * 4, k, :], start=(k == 0), stop=(k == KD - 1)),
                                            r=[wb13[sl_]] + h2b[tg * 4:(tg + 1) * 4], w=[Pb[bk]])
                                S.op("act", lambda e, ba=ba, sp_=sp_: e.activation(out=sa[sp_][:], in_=P[ba][:, :],
                                                                                   func=AF.Silu),
                                     r=[Pb[ba]], w=[sab[sp_]])
                                S.op("dve", lambda e, bb_=bb_, sp_=sp_, f=f: e.tensor_tensor(
                                    out=hid[hp][:, f, :], in0=sa[sp_][:], in1=P[bb_][:, :], op=ALU.mult),
                                    r=[sab[sp_], Pb[bb_]], w=[hidb[hp]])

                        def emit_w2(idx):
                            e_, tg = items[idx]
                            sl_ = e_ % 2
                            hp = idx % 2
                            for j in range(4):
                                n = tg * 4 + j
                                for hf in range(2):
                                    bk = 4 + (j * 2 + hf) % 3
                                    for f in range(4):
                                        S.op("pe", lambda e, bk=bk, f=f, j=j, hf=hf: e.matmul(
                                            P[bk][:, :], lhsT=hid[hp][:, f, j * 128:(j + 1) * 128],
                                            rhs=w2b[sl_][:, f, hf * 512:(hf + 1) * 512], start=(f == 0), stop=(f == 3)),
                                            r=[hidb[hp], w2bb[sl_]], w=[Pb[bk]])
                                    dst = acc[:, n, hf * 512:(hf + 1) * 512]
                                    S.op("dve", lambda e, bk=bk, dst=dst, n=n: e.scalar_tensor_tensor(
                                        out=dst, in0=P[bk][:, :], scalar=cw[:, n, e_:e_ + 1], op0=ALU.mult, in1=dst,
                                        op1=ALU.add), r=[Pb[bk], cwb[n], accb[n]], w=[accb[n]])

                        load_w(0)
                        if NEXP > 1:
                            load_w(1)
                        emit_ab(0)
                        for idx in range(len(items)):
                            if idx + 1 < len(items):
                                emit_ab(idx + 1)
                            emit_w2(idx)
                            e_, tg = items[idx]
                            if tg == 3 and e_ + 2 < NEXP:
                                load_w(e_ + 2)
                        S.barrier()
                        if upto == 5:
                            S.dead = True

                    with contextlib.ExitStack() as e6:
                        fgb = sb(e6, "fgb", (128, D))
                        fb = Buf()
                        S.dma("sp", fgb[:], fg_d.rearrange("(o n) -> o n", o=1).to_broadcast([128, D]), dsl, w=[fb])
                        ot = [sb(e6, "ot%d" % i, (128, D)) for i in range(2)]
                        otb_ = bufs(2)
                        junk = sb(e6, "junk6", (128, D), BF16)
                        jb = Buf()
                        st = [sb(e6, "st6_%d" % i, (128, 2)) for i in range(2)]
                        stb = bufs(2)
                        for n in range(NXT):
                            par = n % 2
                            S.op("act", lambda e: e.activation(out=junk[:], in_=acc[:, n, :], func=AF.Square,
                                                               accum_out=st[par][:, 0:1]), r=[accb[n]], w=[stb[par], jb])
                            S.op("act", lambda e: e.activation(out=st[par][:, 1:2], in_=st[par][:, 0:1], func=AF.Sqrt,
                                                               bias=EPS, scale=1.0 / D), r=[stb[par]], w=[stb[par]])
                            S.op("dve", lambda e: e.reciprocal(out=st[par][:, 1:2], in_=st[par][:, 1:2]),
                                 r=[stb[par]], w=[stb[par]])
                            S.op("dve", lambda e: e.scalar_tensor_tensor(out=ot[par][:], in0=acc[:, n, :],
                                                                         scalar=st[par][:, 1:2], op0=ALU.mult, in1=fgb[:],
                                                                         op1=ALU.mult), r=[accb[n], stb[par], fb], w=[otb_[par]])
                            S.dma("sp", out_d[b, n * 128:(n + 1) * 128, :], ot[par][:], osem[par], r=[otb_[par]])
                        S.barrier()
                        if upto == 6:
                            S.dead = True
                    S.barrier()
        except _Stop:
            pass
        S.dead = False
        S.barrier()
    return nc


_CONSTS = None


def _consts():
    global _CONSTS
    if _CONSTS is None:
        j = np.arange(128)[:, None]
        i = np.arange(128)[None, :]
        cst = np.zeros((128, 898), np.float32)
        cst[:, 0:128] = np.eye(128, dtype=np.float32)
        s = -1.0 / 16.0
        cst[:, 128:256] = (j <= i) * s
        cst[:, 256:384] = (j > i) * s
        cst[:, 384:512] = (j >= i) * s
        cst[:, 512:640] = (j < i) * s
        cst[:, 640:768] = (j <= i)
        cst[:, 768:896] = (j >= i)
        cst[:, 896:898] = s
        poolP = np.zeros((128, 4, 128), np.float32)
        for gi, w in enumerate((2, 4, 8, 16)):
            for t in range(128):
                r0 = (t // 64) * 64
                lo = max(t - w // 2, r0)
                hi = min(t + w // 2, r0 + 64)
                poolP[lo:hi, gi, t] = 1.0 / (hi - lo)
                poolP[t, gi, t] -= 1.0
        sel = np.zeros((8, 8, 128), np.float32)
        for r in range(8):
            sel[r, r, :] = 1.0
        _CONSTS = (cst, poolP.reshape(128, 512), sel.reshape(8, 1024))
    return _CONSTS


_PER_LAYER = ("w_mod", "b_mod", "norm1_g", "norm2_g", "w_in", "gla_a2_f", "gla_ab_f", "gla_a2_b", "gla_ab_b",
              "gla_onorm_g", "pool_w", "pool_scale", "w_pool_br", "w_gla_br", "w_o", "router_grp_w", "router_grp_b",
              "router_exp_w", "router_exp_b", "moe_w1", "moe_w3", "moe_w2")


def kernel(**inputs):
    NB = 4
    x = np.asarray(inputs["x"], np.float32)
    c = np.asarray(inputs["c"], np.float32)
    ctx = np.asarray(inputs["ctx"], np.float32)
    c_ctx = np.asarray(inputs["c_ctx"], np.float32)
    cst, poolP, sel = _consts()
    shared = {k: np.ascontiguousarray(np.asarray(inputs[k], np.float32)[0]) for k in _PER_LAYER}
    shared["final_norm_g"] = np.ascontiguousarray(np.asarray(inputs["final_norm_g"], np.float32))
    shared["cst"] = cst
    shared["poolP"] = poolP
    shared["sel"] = sel
    in_maps = []
    for core in range(NCORES):
        b0 = core * NB
        m = dict(shared)
        m["x"] = np.ascontiguousarray(x[b0:b0 + NB])
        m["ctx"] = np.ascontiguousarray(ctx[b0:b0 + NB])
        m["cT"] = np.ascontiguousarray(np.concatenate([c[b0:b0 + NB], c_ctx[None, :]], axis=0).T)
        in_maps.append(m)
    nc = build_program(NB=NB)
    res = run_bass_kernel_spmd(nc, in_maps, core_ids=list(range(NCORES)))
    return np.concatenate([np.asarray(r["out"], np.float32) for r in res.results], axis=0)
```

```python
import contextlib
import os
import numpy as np
import concourse.bass as bass
import concourse.mybir as mybir
from concourse.bass_utils import run_bass_kernel_spmd

F32 = mybir.dt.float32
BF16 = mybir.dt.bfloat16
AF = mybir.ActivationFunctionType
ALU = mybir.AluOpType

D = 1024
SEQ = 2048
CTXL = 256
KD = 8
NXT = SEQ // 128
NCT = CTXL // 128
NTT = NXT + NCT
TOK = NTT * 128
D_IN = 5664
C_POOL, C_Q, C_K, C_V, C_G, C_GATES, C_LR = 0, 512, 1024, 1536, 2560, 3584, 5632
NHEAD = 4
HK = 128
HV = 256
NEXP_FULL = 32
DE = 512
EPS = 1e-6
NCORES = 8
STRICT_SAME = not os.environ.get("K_NOSTRICT")
POOLENG = "dve" if os.environ.get("K_NOPOOL") else "pool"


class Buf:
    __slots__ = ("lw", "rd", "excl")

    def __init__(self, excl=False):
        self.lw = None
        self.rd = {}
        self.excl = excl


def bufs(n):
    return [Buf() for _ in range(n)]


class DSem:
    def __init__(self, sem):
        self.sem = sem
        self.count = 0
        self.q = None
        self.maxw = 0


class Sched:
    def __init__(self, nc, es):
        self.nc = nc
        self.es = es
        self.E = {}
        for name, eng in (("pe", nc.tensor), ("act", nc.scalar), ("dve", nc.vector),
                          ("pool", nc.gpsimd), ("sp", nc.sync)):
            sem = es.enter_context(nc.semaphore("s_" + name))
            self.E[name] = dict(eng=eng, sem=sem, count=0, waited={})
        self.dsems = []
        self.nds = 0
        self.dsmap = {}
        self.dead = False

    def new_dsem(self):
        self.nds += 1
        ds = DSem(self.es.enter_context(self.nc.semaphore("d%d" % self.nds)))
        self.dsems.append(ds)
        self.dsmap[id(ds.sem)] = ds
        return ds

    def _wait(self, en, toks):
        E = self.E[en]
        for (sem, val, owner) in toks:
            if owner == en and (en == "pe" or not STRICT_SAME):
                continue
            k = id(sem)
            if k in self.dsmap and val > self.dsmap[k].maxw:
                self.dsmap[k].maxw = val
            if E["waited"].get(k, 0) >= val:
                continue
            E["eng"].wait_ge(sem, val)
            E["waited"][k] = val

    @staticmethod
    def _deps(r, w, en=None):
        toks = []
        for b in r:
            if b.lw:
                toks.append(b.lw)
            if b.excl:
                toks.extend(t for e_, t in b.rd.items() if e_ != en)
        for b in w:
            if b.lw:
                toks.append(b.lw)
            toks.extend(b.rd.values())
        return toks

    def op(self, en, fn, r=(), w=()):
        if self.dead:
            return
        self._wait(en, self._deps(r, w, en))
        E = self.E[en]
        ins = fn(E["eng"])
        E["count"] += 1
        ins.then_inc(E["sem"], 1)
        tok = (E["sem"], E["count"], en)
        for b in w:
            b.lw = tok
            b.rd = {}
        for b in r:
            b.rd[en] = tok

    def dma(self, qn, out, in_, ds, r=(), w=()):
        if self.dead:
            return
        self._wait(qn, self._deps(r, w))
        E = self.E[qn]
        assert ds.q in (None, qn), "DMA semaphore shared between queues"
        ds.q = qn
        if ds.maxw > 0:
            self._wait(qn, [(ds.sem, ds.maxw, None)])
        ins = E["eng"].dma_start(out=out, in_=in_)
        ds.count += 16
        ins.then_inc(ds.sem, 16)
        tok = (ds.sem, ds.count, None)
        for b in w:
            b.lw = tok
            b.rd = {}
        for b in r:
            b.rd["dma%d" % id(ds)] = tok

    def barrier(self):
        if self.dead:
            return
        for en, E in self.E.items():
            toks = []
            for en2, E2 in self.E.items():
                if en2 != en and E2["count"] > 0:
                    toks.append((E2["sem"], E2["count"], en2))
            for ds in self.dsems:
                if ds.count > 0:
                    toks.append((ds.sem, ds.count, None))
            self._wait(en, toks)


def pipeline(gens):
    it = iter(gens)
    active = []
    while True:
        g = next(it, None)
        if g is not None:
            active.append(g)
        if not active:
            break
        for g_ in list(active):
            try:
                next(g_)
            except StopIteration:
                active.remove(g_)


class _Stop(Exception):
    pass


def build_program(NB=4, NEXP=NEXP_FULL, debug=None, upto=99):
    nc = bass.Bass("TRN2", target_bir_lowering=False)

    def din(name, shape):
        return nc.dram_tensor(name, list(shape), F32, kind="ExternalInput").ap()

    x_d = din("x", (NB, SEQ, D))
    ctx_d = din("ctx", (NB, CTXL, D))
    cT_d = din("cT", (D, NB + 1))
    w_mod_d = din("w_mod", (D, 6 * D))
    b_mod_d = din("b_mod", (6 * D,))
    n1g_d = din("norm1_g", (D,))
    n2g_d = din("norm2_g", (D,))
    w_in_d = din("w_in", (D, D_IN))
    a2f_d = din("gla_a2_f", (16, 512))
    abf_d = din("gla_ab_f", (512,))
    a2b_d = din("gla_a2_b", (16, 512))
    abb_d = din("gla_ab_b", (512,))
    gn_d = din("gla_onorm_g", (HV,))
    pw_d = din("pool_w", (4, 128, 128))
    psc_d = din("pool_scale", (512,))
    wpb_d = din("w_pool_br", (512, D))
    wgb_d = din("w_gla_br", (D, D))
    wo_d = din("w_o", (D, D))
    rgw_d = din("router_grp_w", (D, 4))
    rgb_d = din("router_grp_b", (4,))
    rew_d = din("router_exp_w", (D, 32))
    reb_d = din("router_exp_b", (32,))
    w1_d = din("moe_w1", (NEXP_FULL, D, DE))
    w3_d = din("moe_w3", (NEXP_FULL, D, DE))
    w2_d = din("moe_w2", (NEXP_FULL, DE, D))
    fg_d = din("final_norm_g", (D,))
    cst_d = din("cst", (128, 898))
    poolP_d = din("poolP", (128, 512))
    sel_d = din("sel", (8, 8 * 128))
    out_d = nc.dram_tensor("out", [NB, SEQ, D], F32, kind="ExternalOutput").ap()
    dbg_d = None
    if debug:
        dbg_d = nc.dram_tensor("dbg", [128, debug], F32, kind="ExternalOutput").ap()

    NBC = NB + 1

    with contextlib.ExitStack() as es:
        S = Sched(nc, es)

        uid = [0]

        def sb(es_, name, shape, dt=F32):
            uid[0] += 1
            return es_.enter_context(nc.sbuf_tensor("%s_s%d" % (name, uid[0]), list(shape), dt))

        P = [es.enter_context(nc.psum_tensor("P%d" % i, [128, 512], F32)) for i in range(7)]
        PB = es.enter_context(nc.psum_tensor("PB", [128, 1024], BF16))
        Pb = [Buf(excl=True) for _ in range(7)]
        PBb = Buf(excl=True)

        cst = sb(es, "cst", (128, 898))
        identb = sb(es, "identb", (128, 128), BF16)
        poolP = sb(es, "poolP", (128, 512), BF16)
        sel = sb(es, "sel", (8, 1024))
        vstage = sb(es, "vstage", (68, 128))
        vT = sb(es, "vT", (128, 68))
        gnb = sb(es, "gnb", (128, HV))
        rbb = sb(es, "rbb", (128, 36))
        a2pf = sb(es, "a2pf", (32, 512), BF16)
        a2pb = sb(es, "a2pb", (32, 512), BF16)
        wlr = sb(es, "wlr", (128, KD, 32), BF16)
        rw = sb(es, "rw", (128, KD, 36), BF16)
        pwt = sb(es, "pwt", (128, 4, 128), BF16)
        sct = sb(es, "sct", (128, KD, NBC))
        modT = sb(es, "modT", (128, NBC, 48))
        msb = sb(es, "msb", (NBC, 2, D))
        A1 = sb(es, "A1", (128, NBC, KD))
        A2 = sb(es, "A2", (128, NBC, KD))
        cb = Buf()
        dsc = S.new_dsem()
        dscp = S.new_dsem()
        xid = [S.new_dsem() for _ in range(3)]
        wsem = S.new_dsem()
        wgsem = S.new_dsem()
        w2sem = S.new_dsem()
        dsl = S.new_dsem()
        dbgsem = S.new_dsem()
        wm13 = [S.new_dsem() for _ in range(2)]
        osem = [S.new_dsem() for _ in range(2)]

        ident = cst[:, 0:128]

        def tri(i):
            return cst[:, 128 + i * 128: 256 + i * 128]

        def msk(i):
            return cst[:, 640 + i * 128: 768 + i * 128]

        negcol = cst[:, 896:898]

        a2B = Buf()
        S.op("dve", lambda e: e.memset(a2pf[:], 0.0), w=[a2B])
        S.op("dve", lambda e: e.memset(a2pb[:], 0.0), w=[a2B])
        S.dma("sp", cst[:], cst_d, dsc, w=[cb])
        S.dma("sp", sel[:], sel_d, dsc, w=[cb])
        S.dma("sp", vstage[0:48, :], b_mod_d.rearrange("(j p) -> j p", p=128), dsc, w=[cb])
        S.dma("sp", vstage[48:56, :], n1g_d.rearrange("(j p) -> j p", p=128), dsc, w=[cb])
        S.dma("sp", vstage[56:64, :], n2g_d.rearrange("(j p) -> j p", p=128), dsc, w=[cb])
        S.dma("sp", vstage[64:68, :], psc_d.rearrange("(j p) -> j p", p=128), dsc, w=[cb])
        S.dma("sp", gnb[:], gn_d.rearrange("(o n) -> o n", o=1).to_broadcast([128, HV]), dsc, w=[cb])
        S.dma("sp", rbb[:, 0:4], rgb_d.rearrange("(o n) -> o n", o=1).to_broadcast([128, 4]), dsc, w=[cb])
        S.dma("sp", rbb[:, 4:36], reb_d.rearrange("(o n) -> o n", o=1).to_broadcast([128, 32]), dsc, w=[cb])
        S.dma("sp", sct[:], cT_d.rearrange("(k p) b -> p k b", p=128), dsc, w=[cb])
        S.dma("pool", a2pf[0:16, :], a2f_d, dscp, w=[cb, a2B])
        S.dma("pool", a2pb[16:32, :], a2b_d, dscp, w=[cb, a2B])
        S.dma("pool", poolP[:], poolP_d, dscp, w=[cb])
        S.dma("pool", wlr[:], w_in_d[:, C_LR:C_LR + 32].rearrange("(k p) n -> p k n", p=128), dscp, w=[cb])
        S.dma("pool", rw[:, :, 0:4], rgw_d.rearrange("(k p) n -> p k n", p=128), dscp, w=[cb])
        S.dma("pool", rw[:, :, 4:36], rew_d.rearrange("(k p) n -> p k n", p=128), dscp, w=[cb])
        S.dma("pool", pwt[:], pw_d.rearrange("g c e -> c g e"), dscp, w=[cb])
        S.op("dve", lambda e: e.tensor_copy(out=identb[:], in_=ident), r=[cb], w=[cb])
        S.op("act", lambda e: e.activation(out=sct[:], in_=sct[:], func=AF.Silu), r=[cb], w=[cb])
        S.op("pe", lambda e: e.transpose(P[0][:, 0:68], vstage[:], cst[0:68, 0:68]), r=[cb], w=[Pb[0]])
        S.op("dve", lambda e: e.tensor_copy(out=vT[:], in_=P[0][:, 0:68]), r=[Pb[0]], w=[cb])

        with contextlib.ExitStack() as p0:
            wm = [sb(p0, "wm%d" % i, (128, KD, 512)) for i in range(2)]
            bm5 = sb(p0, "bm5", (NBC, 2, D))
            for ci, ch in enumerate((2, 5)):
                S.dma("sp", bm5[:, ci, :],
                      b_mod_d[ch * D:(ch + 1) * D].rearrange("(o n) -> o n", o=1).to_broadcast([NBC, D]),
                      dsl, w=[cb])
            wmb = bufs(2)
            wmd = [S.new_dsem() for _ in range(2)]
            for cg in range(12):
                i = cg % 2
                S.dma("sp", wm[i][:], w_mod_d[:, cg * 512:(cg + 1) * 512].rearrange("(k p) n -> p k n", p=128),
                      wmd[i], w=[wmb[i]])
                for m in range(4):
                    j = cg * 4 + m
                    for k in range(KD):
                        S.op("pe", lambda e, i=i, m=m, k=k, j=j: e.matmul(
                            P[1][:, j * 8:j * 8 + NBC], lhsT=wm[i][:, k, m * 128:(m + 1) * 128],
                            rhs=sct[:, k, :], start=(k == 0), stop=(k == KD - 1)),
                            r=[wmb[i], cb], w=[Pb[1]])
                if cg // 2 in (2, 5):
                    ci = 0 if cg // 2 == 2 else 1
                    hf = cg % 2
                    for k in range(KD):
                        S.op("pe", lambda e, i=i, k=k: e.matmul(
                            P[2][0:NBC, :], lhsT=sct[:, k, :], rhs=wm[i][:, k, :],
                            start=(k == 0), stop=(k == KD - 1)), r=[wmb[i], cb], w=[Pb[2]])
                    S.op("dve", lambda e, ci=ci, hf=hf: e.tensor_tensor(
                        out=msb[:, ci, hf * 512:(hf + 1) * 512], in0=P[2][0:NBC, :],
                        in1=bm5[:, ci, hf * 512:(hf + 1) * 512], op=ALU.add), r=[Pb[2], cb], w=[cb])
            p1v = P[1][:, 0:384].rearrange("p (j e) -> p j e", e=8)
            for b in range(NBC):
                S.op("dve", lambda e, b=b: e.tensor_tensor(out=modT[:, b, :], in0=p1v[:, :, b], in1=vT[:, 0:48],
                                                           op=ALU.add), r=[Pb[1], cb], w=[cb])
            for b in range(NBC):
                S.op("dve", lambda e, b=b: e.scalar_tensor_tensor(
                    out=A1[:, b, :], in0=modT[:, b, 8:16], scalar=1.0, op0=ALU.add, in1=vT[:, 48:56], op1=ALU.mult),
                    r=[cb], w=[cb])
                S.op("dve", lambda e, b=b: e.scalar_tensor_tensor(
                    out=A2[:, b, :], in0=modT[:, b, 32:40], scalar=1.0, op0=ALU.add, in1=vT[:, 56:64], op1=ALU.mult),
                    r=[cb], w=[cb])
            S.barrier()

        dbg_off = [0]

        def dump(ap_, n, rb, np_=128):
            if dbg_d is None:
                return
            o = dbg_off[0]
            S.dma("pool", dbg_d[0:np_, o:o + n], ap_, dbgsem, r=rb)
            dbg_off[0] = o + n

        def rms_rstd(es_unused, src_ap, width, ss, rs, junk, rb, sb_):
            S.op("act", lambda e: e.activation(out=junk, in_=src_ap, func=AF.Square, accum_out=ss[:, 0:1]),
                 r=rb, w=[sb_])
            S.op("act", lambda e: e.activation(out=rs[:, 0:1], in_=ss[:, 0:1], func=AF.Sqrt, bias=EPS,
                                               scale=1.0 / width), r=[sb_], w=[sb_])
            S.op("dve", lambda e: e.reciprocal(out=rs[:, 0:1], in_=rs[:, 0:1]), r=[sb_], w=[sb_])

        try:
            for b in range(NB):
                with contextlib.ExitStack() as eb:
                    acc = sb(eb, "acc", (128, NXT, D))
                    accb = bufs(NXT)
                    with contextlib.ExitStack() as e13:
                        hT = sb(e13, "hT", (128, KD, TOK), BF16)
                        hTb = bufs(NTT)
                        lrT = sb(e13, "lrT", (32, TOK), BF16)
                        lrb = Buf()
                        with contextlib.ExitStack() as e1:
                            xin = [sb(e1, "xin%d" % i, (128, D)) for i in range(3)]
                            xib = bufs(3)
                            junk = sb(e1, "junk1", (128, D), BF16)
                            st = [sb(e1, "st1_%d" % i, (128, 2)) for i in range(2)]
                            stb = bufs(2)
                            jb = Buf()

                            def load_x(tt):
                                i = tt % 3
                                src = ctx_d[b, tt * 128:(tt + 1) * 128, :] if tt < NCT else \
                                    x_d[b, (tt - NCT) * 128:(tt - NCT + 1) * 128, :]
                                S.dma("sp", xin[i][:], src, xid[i], w=[xib[i]])

                            def p1_body(tt):
                                if tt + 2 < NTT:
                                    load_x(tt + 2)
                                i = tt % 3
                                s_ = st[tt % 2]
                                sbb = stb[tt % 2]
                                mb = NB if tt < NCT else b
                                S.op("act", lambda e: e.activation(
                                    out=junk[:], in_=xin[i][:], func=AF.Square, accum_out=s_[:, 0:1]),
                                    r=[xib[i]], w=[sbb, jb])
                                S.op("act", lambda e: e.activation(
                                    out=s_[:, 1:2], in_=s_[:, 0:1], func=AF.Sqrt, bias=EPS, scale=1.0 / D),
                                    r=[sbb], w=[sbb])
                                S.op("dve", lambda e: e.reciprocal(out=s_[:, 1:2], in_=s_[:, 1:2]),
                                     r=[sbb], w=[sbb])
                                S.op("dve", lambda e: e.tensor_scalar(
                                    out=xin[i][:], in0=xin[i][:], scalar1=s_[:, 1:2], scalar2=None, op0=ALU.mult),
                                    r=[sbb, xib[i]], w=[xib[i]])
                                yield
                                pbase = (tt % 2) * 2
                                for k in range(KD):
                                    bk = pbase + k // 4
                                    S.op("pe", lambda e, k=k, bk=bk: e.transpose(
                                        P[bk][:, (k % 4) * 128:(k % 4 + 1) * 128], xin[i][:, k * 128:(k + 1) * 128], ident),
                                        r=[xib[i], cb], w=[Pb[bk]])
                                yield
                                for k in range(KD):
                                    bk = pbase + k // 4
                                    src = P[bk][:, (k % 4) * 128:(k % 4 + 1) * 128]
                                    dst = hT[:, k, tt * 128:(tt + 1) * 128]
                                    if k // 4 == 0:
                                        S.op("act", lambda e, src=src, dst=dst, k=k: e.activation(
                                            out=dst, in_=src, func=AF.Identity, scale=A1[:, mb, k:k + 1],
                                            bias=modT[:, mb, k:k + 1]), r=[Pb[bk], cb], w=[hTb[tt]])
                                    else:
                                        S.op("dve", lambda e, src=src, dst=dst, k=k: e.tensor_scalar(
                                            out=dst, in0=src, scalar1=A1[:, mb, k:k + 1], scalar2=modT[:, mb, k:k + 1],
                                            op0=ALU.mult, op1=ALU.add), r=[Pb[bk], cb], w=[hTb[tt]])

                            load_x(0)
                            load_x(1)
                            pipeline(p1_body(tt) for tt in range(NTT))
                            S.barrier()
                            if upto == 1:
                                S.dead = True
                        with contextlib.ExitStack() as e23:
                            whd = sb(e23, "whd", (128, KD, 768), BF16)
                            abfb = sb(e23, "abfb", (128, 512))
                            abbb = sb(e23, "abbb", (128, 512))
                            abB = Buf()
                            S.dma("sp", abfb[:], abf_d.rearrange("(o n) -> o n", o=1).to_broadcast([128, 512]), dsl, w=[abB])
                            S.dma("sp", abbb[:], abb_d.rearrange("(o n) -> o n", o=1).to_broadcast([128, 512]), dsl, w=[abB])
                            wgl = sb(e23, "wgl", (128, 2, D), BF16)
                            whb = Buf()
                            qT = sb(e23, "qT", (128, SEQ), BF16)
                            qTb = bufs(4)
                            kT = sb(e23, "kT", (128, TOK), BF16)
                            kTb = bufs(5)
                            ktok = sb(e23, "ktok", (128, NTT, 128), BF16)
                            ktb = bufs(3)
                            vtok = sb(e23, "vtok", (128, NTT, HV), BF16)
                            vtb = bufs(NTT)
                            sg = sb(e23, "sg", (128, NXT, HV), BF16)
                            sgb = bufs(NXT)
                            SBs = sb(e23, "SBs", (128, NXT, HV), BF16)
                            SBb = bufs(NXT)
                            SF = [sb(e23, "SF%d" % i, (128, HV), BF16) for i in range(2)]
                            SFb = bufs(2)
                            Sst = sb(e23, "Sst", (128, HV))
                            Sstb = Buf()
                            NSL = 3
                            NBIG = 2
                            zz = [sb(e23, "zz%d" % p_, (128, 256)) for p_ in range(NBIG)]
                            zzb = bufs(NBIG)
                            EE = [sb(e23, "EE%d" % p_, (128, 386)) for p_ in range(NBIG)]
                            EEb = bufs(NBIG)
                            E2 = [sb(e23, "E2_%d" % p_, (128, 256)) for p_ in range(NBIG)]
                            E2b = bufs(NBIG)
                            NDEC = 6
                            decs = sb(e23, "decs", (128, NDEC))
                            decb = bufs(NDEC)
                            kr = [sb(e23, "kr%d" % p_, (128, 128), BF16) for p_ in range(NSL)]
                            krb = bufs(NSL)
                            abh = sb(e23, "abh", (128, 256))
                            abhb = Buf()
                            qd = [[sb(e23, "qd%d%d" % (p_, d_), (128, 128), BF16) for d_ in range(2)] for p_ in range(NSL)]
                            qdb = [bufs(2) for _ in range(NSL)]
                            ki = [[sb(e23, "ki%d%d" % (p_, d_), (128, 128), BF16) for d_ in range(2)] for p_ in range(NSL)]
                            kib = [bufs(2) for _ in range(NSL)]
                            sT = [[sb(e23, "sT%d%d" % (p_, d_), (128, 128), BF16) for d_ in range(2)] for p_ in range(NSL)]
                            sTb = [bufs(2) for _ in range(NSL)]
                            otmp = [sb(e23, "otmp%d" % p_, (128, HV)) for p_ in range(NBIG)]
                            otb = bufs(NBIG)
                            og = [sb(e23, "og%d" % p_, (128, HV), BF16) for p_ in range(NBIG)]
                            ogb = bufs(NBIG)
                            ogT = [sb(e23, "ogT%d" % p_, (128, 2, 128), BF16) for p_ in range(NBIG)]
                            ogTb = bufs(NBIG)
                            st2 = [sb(e23, "st2_%d" % p_, (128, 2)) for p_ in range(NSL)]
                            st2b = bufs(NSL)
                            junk2 = sb(e23, "junk2", (128, HV), BF16)
                            vglock = Buf()
                            j2b = Buf()
                            nbk = [0]

                            def rot():
                                v_ = nbk[0]
                                nbk[0] = (v_ + 1) % 7
                                return v_

                            def kgrp(tt):
                                return 0 if tt < NCT else 1 + (tt - NCT) // 4

                            tokgroups = [(0, 256)] + [(256 + g * 512, 512) for g in range(4)]

                            wglb = Buf()

                            def load_whd(h_):
                                for (dst0, src0, n_) in ((0, C_Q + h_ * 128, 128), (128, C_K + h_ * 128, 128),
                                                         (256, C_V + h_ * 256, 256), (512, C_G + h_ * 256, 256)):
                                    S.dma("pool", whd[:, :, dst0:dst0 + n_],
                                          w_in_d[:, src0:src0 + n_].rearrange("(k p) n -> p k n", p=128), wsem, w=[whb])

                            def load_wgl(h_):
                                S.dma("pool", wgl[:], wgb_d[h_ * 256:(h_ + 1) * 256, :].rearrange("(c p) n -> p c n", p=128),
                                      wgsem, w=[wglb])

                            for h in range(NHEAD):
                                if h == 0:
                                    load_whd(0)
                                    load_wgl(0)
                                for g in range(4):
                                    bk = rot()
                                    for k in range(KD):
                                        S.op("pe", lambda e, k=k, g=g, bk=bk: e.matmul(
                                            P[bk][:, :], lhsT=whd[:, k, 0:128], rhs=hT[:, k, 256 + g * 512:768 + g * 512],
                                            start=(k == 0), stop=(k == KD - 1)),
                                            r=[whb] + hTb[2 + 4 * g:6 + 4 * g], w=[Pb[bk]])
                                    S.op("act", lambda e, g=g, bk=bk: e.activation(
                                        out=qT[:, g * 512:(g + 1) * 512], in_=P[bk][:, :], func=AF.Copy, scale=HK ** -0.5),
                                        r=[Pb[bk]], w=[qTb[g]])
                                for g, (t0, n_) in enumerate(tokgroups):
                                    bk = rot()
                                    for k in range(KD):
                                        S.op("pe", lambda e, k=k, bk=bk, t0=t0, n_=n_: e.matmul(
                                            P[bk][:, 0:n_], lhsT=whd[:, k, 128:256], rhs=hT[:, k, t0:t0 + n_],
                                            start=(k == 0), stop=(k == KD - 1)),
                                            r=[whb] + hTb[t0 // 128:(t0 + n_) // 128], w=[Pb[bk]])
                                    S.op("dve", lambda e, bk=bk, t0=t0, n_=n_: e.tensor_copy(
                                        out=kT[:, t0:t0 + n_], in_=P[bk][:, 0:n_]), r=[Pb[bk]], w=[kTb[g]])
                                    if h == 0:
                                        bk = rot()
                                        for k in range(KD):
                                            S.op("pe", lambda e, k=k, bk=bk, t0=t0, n_=n_: e.matmul(
                                                P[bk][0:32, 0:n_], lhsT=wlr[:, k, :], rhs=hT[:, k, t0:t0 + n_],
                                                start=(k == 0), stop=(k == KD - 1)),
                                                r=[cb] + hTb[t0 // 128:(t0 + n_) // 128], w=[Pb[bk]])
                                        S.op("act", lambda e, bk=bk, t0=t0, n_=n_: e.activation(
                                            out=lrT[:, t0:t0 + n_], in_=P[bk][0:32, 0:n_], func=AF.Copy),
                                            r=[Pb[bk]], w=[lrb])
                                if upto == 20 and h == 0:
                                    S.dead = True
                                for bi, (tt0, nt_) in enumerate(((0, 8), (8, 8), (16, 2))):
                                    for j in range(nt_):
                                        tt = tt0 + j
                                        S.op("pe", lambda e, j=j, tt=tt: e.transpose(
                                            PB[:, j * 128:(j + 1) * 128], kT[:, tt * 128:(tt + 1) * 128], identb[:]),
                                            r=[kTb[kgrp(tt)], cb], w=[PBb])
                                    S.op("act" if bi % 2 else "dve", (lambda e, tt0=tt0, nt_=nt_: e.activation(
                                        out=ktok[:, tt0:tt0 + nt_, :], in_=PB[:, 0:nt_ * 128].rearrange("p (t d) -> p t d", d=128),
                                        func=AF.Copy)) if bi % 2 else (lambda e, tt0=tt0, nt_=nt_: e.tensor_copy(
                                            out=ktok[:, tt0:tt0 + nt_, :],
                                            in_=PB[:, 0:nt_ * 128].rearrange("p (t d) -> p t d", d=128))),
                                        r=[PBb], w=[ktb[bi]])
                                if upto == 21 and h == 0:
                                    S.dead = True
                                for tt in range(NTT):
                                    bk = rot()
                                    n_ = 256 if tt < NCT else 512
                                    for k in range(KD):
                                        S.op("pe", lambda e, k=k, bk=bk, tt=tt, n_=n_: e.matmul(
                                            P[bk][:, 0:n_], lhsT=hT[:, k, tt * 128:(tt + 1) * 128], rhs=whd[:, k, 256:256 + n_],
                                            start=(k == 0), stop=(k == KD - 1)), r=[whb, hTb[tt]], w=[Pb[bk]])
                                    S.op("dve", lambda e, bk=bk, tt=tt: e.tensor_copy(out=vtok[:, tt, :], in_=P[bk][:, 0:256]),
                                         r=[Pb[bk]], w=[vtb[tt], vglock])
                                    if tt >= NCT:
                                        S.op("act", lambda e, bk=bk, tt=tt: e.activation(
                                            out=sg[:, tt - NCT, :], in_=P[bk][:, 256:512], func=AF.Silu),
                                            r=[Pb[bk]], w=[sgb[tt - NCT], vglock])

                                if h + 1 < NHEAD:
                                    load_whd(h + 1)
                                if upto == 22 and h == 0:
                                    S.dead = True
                                S.op("dve", lambda e: e.tensor_copy(out=abh[:, 0:128], in_=abfb[:, h * 128:(h + 1) * 128]),
                                     r=[abB], w=[abhb])
                                S.op("dve", lambda e: e.tensor_copy(out=abh[:, 128:256], in_=abbb[:, h * 128:(h + 1) * 128]),
                                     r=[abB], w=[abhb])

                                def la_stage(tt, dirs, ix):
                                    lo, hi = dirs[0] * 128, (dirs[-1] + 1) * 128
                                    for di in dirs:
                                        a2p = a2pf if di == 0 else a2pb
                                        S.op("pe", lambda e, di=di, a2p=a2p: e.matmul(
                                            P[0][:, di * 128:(di + 1) * 128], lhsT=lrT[:, tt * 128:(tt + 1) * 128],
                                            rhs=a2p[:, h * 128:(h + 1) * 128], start=True, stop=True),
                                            r=[lrb, cb], w=[Pb[0]])
                                    yield
                                    S.op("dve", lambda e: e.tensor_tensor(out=zz[ix % NBIG][:, lo:hi], in0=P[0][:, lo:hi],
                                                                          in1=abh[:, lo:hi], op=ALU.add),
                                         r=[Pb[0], abhb], w=[zzb[ix % NBIG]])
                                    yield
                                    S.op("act", lambda e: e.activation(out=zz[ix % NBIG][:, lo:hi], in_=zz[ix % NBIG][:, lo:hi],
                                                                       func=AF.Exp, scale=-1.0), r=[zzb[ix % NBIG]], w=[zzb[ix % NBIG]])
                                    S.op("act", lambda e: e.activation(out=zz[ix % NBIG][:, lo:hi], in_=zz[ix % NBIG][:, lo:hi],
                                                                       func=AF.Ln, bias=1.0), r=[zzb[ix % NBIG]], w=[zzb[ix % NBIG]])
                                    yield

                                def rem_tot(di, ix):
                                    trm = tri(1) if di == 0 else tri(3)
                                    sp_ = zz[ix % NBIG][:, di * 128:(di + 1) * 128]
                                    S.op("pe", lambda e: e.matmul(P[1][:, 256:384], lhsT=trm, rhs=sp_,
                                                                  start=True, stop=True), r=[zzb[ix % NBIG], cb], w=[Pb[1]])
                                    S.op("pe", lambda e: e.matmul(P[1][:, 384:386], lhsT=sp_, rhs=negcol,
                                                                  start=True, stop=True), r=[zzb[ix % NBIG], cb], w=[Pb[1]])

                                def state_post(tt, ix, dst, dstb, bk=6, c0=0):
                                    S.op("pe", lambda e: e.matmul(P[bk][:, c0:c0 + HV], lhsT=kr[ix % NSL][:], rhs=vtok[:, tt, :],
                                                                  start=True, stop=True), r=[krb[ix % NSL], vtb[tt]], w=[Pb[bk]])
                                    yield
                                    S.op("dve", lambda e: e.scalar_tensor_tensor(
                                        out=Sst[:], in0=Sst[:], scalar=decs[:, ix % NDEC:ix % NDEC + 1], op0=ALU.mult,
                                        in1=P[bk][:, c0:c0 + HV], op1=ALU.add), r=[Sstb, decb[ix % NDEC], Pb[bk]], w=[Sstb])
                                    if dst is not None:
                                        S.op("dve", lambda e: e.tensor_copy(out=dst, in_=Sst[:]), r=[Sstb], w=[dstb])

                                def st_body(di, tt, ix, dst, dstb):
                                    yield from la_stage(tt, (di,), ix)
                                    rem_tot(di, ix)
                                    yield
                                    S.op("act", lambda e: e.activation(out=EE[ix % NBIG][:, 256:386], in_=P[1][:, 256:386], func=AF.Exp),
                                         r=[Pb[1]], w=[EEb[ix % NBIG]])
                                    yield
                                    S.op("dve", lambda e: e.tensor_copy(out=decs[:, ix % NDEC:ix % NDEC + 1], in_=EE[ix % NBIG][:, 384:385]),
                                         r=[EEb[ix % NBIG]], w=[decb[ix % NDEC]])
                                    S.op("dve", lambda e: e.tensor_tensor(out=kr[ix % NSL][:], in0=ktok[:, tt, :],
                                                                          in1=EE[ix % NBIG][:, 256:384], op=ALU.mult),
                                         r=[ktb[tt // 8], EEb[ix % NBIG]], w=[krb[ix % NSL]])
                                    yield
                                    yield from state_post(tt, ix, dst, dstb)

                                def f_body(n, ix):
                                    tt = n + NCT
                                    cur = n % 2
                                    yield from la_stage(tt, (0, 1), ix)
                                    for di in (0, 1):
                                        tin = tri(0) if di == 0 else tri(2)
                                        S.op("pe", lambda e, di=di, tin=tin: e.matmul(
                                            P[1][:, di * 128:(di + 1) * 128], lhsT=zz[ix % NBIG][:, di * 128:(di + 1) * 128], rhs=tin,
                                            start=True, stop=True), r=[zzb[ix % NBIG], cb], w=[Pb[1]])
                                    rem_tot(0, ix)
                                    yield
                                    S.op("act", lambda e: e.activation(out=EE[ix % NBIG][:, 0:386], in_=P[1][:, 0:386], func=AF.Exp),
                                         r=[Pb[1]], w=[EEb[ix % NBIG]])
                                    S.op("act", lambda e: e.activation(out=E2[ix % NBIG][:, 0:256], in_=P[1][:, 0:256], func=AF.Exp,
                                                                       scale=-1.0), r=[Pb[1]], w=[E2b[ix % NBIG]])
                                    yield
                                    for di in (0, 1):
                                        S.op("dve", lambda e, di=di: e.tensor_tensor(
                                            out=qd[ix % NSL][di][:], in0=qT[:, n * 128:(n + 1) * 128],
                                            in1=EE[ix % NBIG][:, di * 128:(di + 1) * 128],
                                            op=ALU.mult), r=[qTb[n // 4], EEb[ix % NBIG]], w=[qdb[ix % NSL][di]])
                                        S.op(POOLENG, lambda e, di=di: e.tensor_tensor(
                                            out=ki[ix % NSL][di][:], in0=kT[:, tt * 128:(tt + 1) * 128],
                                            in1=E2[ix % NBIG][:, di * 128:(di + 1) * 128],
                                            op=ALU.mult), r=[kTb[kgrp(tt)], E2b[ix % NBIG]], w=[kib[ix % NSL][di]])
                                    if n < NXT - 1:
                                        S.op("dve", lambda e: e.tensor_copy(out=decs[:, ix % NDEC:ix % NDEC + 1], in_=EE[ix % NBIG][:, 384:385]),
                                         r=[EEb[ix % NBIG]], w=[decb[ix % NDEC]])
                                    S.op("dve", lambda e: e.tensor_tensor(out=kr[ix % NSL][:], in0=ktok[:, tt, :],
                                                                              in1=EE[ix % NBIG][:, 256:384], op=ALU.mult),
                                             r=[ktb[tt // 8], EEb[ix % NBIG]], w=[krb[ix % NSL]])
                                    yield
                                    for di in (0, 1):
                                        S.op("pe", lambda e, di=di: e.matmul(
                                            P[2][:, di * 128:(di + 1) * 128], lhsT=ki[ix % NSL][di][:], rhs=qd[ix % NSL][di][:],
                                            start=True, stop=True), r=[kib[ix % NSL][di], qdb[ix % NSL][di]], w=[Pb[2]])
                                    yield
                                    for di in (0, 1):
                                        S.op("dve", lambda e, di=di: e.tensor_tensor(
                                            out=sT[ix % NSL][di][:], in0=P[2][:, di * 128:(di + 1) * 128], in1=msk(di),
                                            op=ALU.mult), r=[Pb[2], cb], w=[sTb[ix % NSL][di]])
                                    yield
                                    ops_o = ((sT[ix % NSL][0][:], vtok[:, tt, :], [sTb[ix % NSL][0], vtb[tt]]),
                                             (qd[ix % NSL][0][:], SF[cur][:], [qdb[ix % NSL][0], SFb[cur]]),
                                             (sT[ix % NSL][1][:], vtok[:, tt, :], [sTb[ix % NSL][1], vtb[tt]]),
                                             (qd[ix % NSL][1][:], SBs[:, n, :], [qdb[ix % NSL][1], SBb[n]]))
                                    ob = 3 if ix % 2 == 0 else 6
                                    for oi, (l_, r_, rb_) in enumerate(ops_o):
                                        S.op("pe", lambda e, l_=l_, r_=r_, oi=oi: e.matmul(
                                            P[ob][:, 0:HV], lhsT=l_, rhs=r_, start=(oi == 0), stop=(oi == 3)),
                                            r=rb_, w=[Pb[ob]])
                                    if n < NXT - 1:
                                        spost = state_post(tt, ix, SF[1 - cur][:], SFb[1 - cur], bk=ob, c0=256)
                                        next(spost)
                                    else:
                                        spost = iter(())
                                    yield
                                    S.op("act", lambda e: e.activation(out=junk2[:], in_=P[ob][:, 0:HV], func=AF.Square,
                                                                       accum_out=st2[ix % NSL][:, 0:1]),
                                         r=[Pb[ob]], w=[st2b[ix % NSL], j2b])
                                    S.op("act", lambda e: e.activation(out=st2[ix % NSL][:, 1:2], in_=st2[ix % NSL][:, 0:1], func=AF.Ln,
                                                                       bias=EPS, scale=1.0 / HV), r=[st2b[ix % NSL]], w=[st2b[ix % NSL]])
                                    S.op("act", lambda e: e.activation(out=st2[ix % NSL][:, 1:2], in_=st2[ix % NSL][:, 1:2], func=AF.Exp,
                                                                       scale=-0.5), r=[st2b[ix % NSL]], w=[st2b[ix % NSL]])
                                    next(spost, None)
                                    yield
                                    S.op("dve", lambda e: e.scalar_tensor_tensor(
                                        out=otmp[ix % NBIG][:], in0=P[ob][:, 0:HV], scalar=st2[ix % NSL][:, 1:2], op0=ALU.mult,
                                        in1=gnb[:], op1=ALU.mult), r=[Pb[ob], st2b[ix % NSL], cb], w=[otb[ix % NBIG]])
                                    S.op(POOLENG, lambda e: e.tensor_tensor(out=og[ix % NBIG][:], in0=otmp[ix % NBIG][:], in1=sg[:, n, :],
                                                                           op=ALU.mult), r=[otb[ix % NBIG], sgb[n]], w=[ogb[ix % NBIG]])
                                    yield
                                    for c in range(2):
                                        S.op("pe", lambda e, c=c: e.transpose(PB[:, c * 128:(c + 1) * 128],
                                                                              og[ix % NBIG][:, c * 128:(c + 1) * 128], identb[:]),
                                             r=[ogb[ix % NBIG], cb], w=[PBb])
                                    yield
                                    S.op("act", lambda e: e.activation(
                                        out=ogT[ix % NBIG][:, :, :], in_=PB[:, 0:256].rearrange("p (c t) -> p c t", t=128),
                                        func=AF.Copy), r=[PBb], w=[ogTb[ix % NBIG]])
                                    yield
                                    for hf in range(2):
                                        for c in range(2):
                                            S.op("pe", lambda e, hf=hf, c=c: e.matmul(
                                                P[4 + hf][:, :], lhsT=ogT[ix % NBIG][:, c, :], rhs=wgl[:, c, hf * 512:(hf + 1) * 512],
                                                start=(c == 0), stop=(c == 1)), r=[ogTb[ix % NBIG], wglb], w=[Pb[4 + hf]])
                                    yield
                                    for hf in range(2):
                                        dst = acc[:, n, hf * 512:(hf + 1) * 512]
                                        if h == 0:
                                            S.op("act", lambda e, hf=hf, dst=dst: e.activation(out=dst, in_=P[4 + hf][:, :],
                                                                                               func=AF.Copy),
                                                 r=[Pb[4 + hf]], w=[accb[n]])
                                        else:
                                            S.op("dve", lambda e, hf=hf, dst=dst: e.tensor_tensor(
                                                out=dst, in0=dst, in1=P[4 + hf][:, :], op=ALU.add),
                                                r=[Pb[4 + hf], accb[n]], w=[accb[n]])

                                S.op("dve", lambda e: e.memset(Sst[:], 0.0), w=[Sstb])
                                gens = [st_body(1, 1, 0, None, None), st_body(1, 0, 1, SBs[:, NXT - 1, :], SBb[NXT - 1])]
                                for n in range(NXT - 1, 0, -1):
                                    gens.append(st_body(1, n + NCT, len(gens), SBs[:, n - 1, :], SBb[n - 1]))
                                pipeline(gens)
                                if upto == 23 and h == 0:
                                    S.dead = True
                                S.op("dve", lambda e: e.memset(Sst[:], 0.0), w=[Sstb])
                                gens = [st_body(0, 0, 0, None, None), st_body(0, 1, 1, SF[0][:], SFb[0])]
                                for n in range(NXT):
                                    gens.append(f_body(n, len(gens)))
                                pipeline(gens)
                                if h + 1 < NHEAD:
                                    load_wgl(h + 1)
                            S.barrier()
                            if upto == 2:
                                S.dead = True
                        S.barrier()
                    h2T = sb(eb, "h2T", (128, NXT, KD, 128), BF16)
                    h2b = bufs(NXT)
                    cw = sb(eb, "cw", (128, NXT, 32))
                    cwb = bufs(NXT)

                    def bcast_row(dst, ci):
                        for hf in range(2):
                            S.op("pe", lambda e, hf=hf: e.matmul(
                                P[hf][:, :], lhsT=sel[0:NBC, b * 128:(b + 1) * 128], rhs=msb[:, ci, hf * 512:(hf + 1) * 512],
                                start=True, stop=True), r=[cb], w=[Pb[hf]])
                            S.op("act", lambda e, hf=hf: e.activation(out=dst[:, hf * 512:(hf + 1) * 512], in_=P[hf][:, :],
                                                                      func=AF.Copy), r=[Pb[hf]], w=[cb])

                    with contextlib.ExitStack() as e4:
                        wu = sb(e4, "wu", (128, KD, 512), BF16)
                        wgt = sb(e4, "wgt", (128, KD, 2048), BF16)
                        wpb = sb(e4, "wpb", (128, 4, D), BF16)
                        w4b = Buf()
                        S.dma("pool", wu[:], w_in_d[:, C_POOL:C_POOL + 512].rearrange("(k p) n -> p k n", p=128), wsem, w=[w4b])
                        for j in range(4):
                            S.dma("pool", wgt[:, :, j * 512:(j + 1) * 512],
                                  w_in_d[:, C_GATES + j * 512:C_GATES + (j + 1) * 512].rearrange("(k p) n -> p k n", p=128),
                                  wsem, w=[w4b])
                        S.dma("pool", wpb[:], wpb_d.rearrange("(g p) n -> p g n", p=128), wsem, w=[w4b])
                        xin = [sb(e4, "xin4_%d" % i, (128, D)) for i in range(2)]
                        xib = bufs(2)
                        xs = sb(e4, "xs4", (128, D))
                        xsb = Buf()
                        junk = sb(e4, "junk4", (128, D), BF16)
                        jb = Buf()
                        st = [sb(e4, "st4_%d" % i, (128, 2)) for i in range(2)]
                        stb = bufs(2)
                        hTt = [sb(e4, "hTt%d" % i, (128, KD, 128), BF16) for i in range(2)]
                        hTtb = bufs(2)
                        u_sb = sb(e4, "u_sb", (128, 512), BF16)
                        ub = Buf()
                        pT = sb(e4, "pT", (128, 512), BF16)
                        pTb = Buf()
                        y1T = sb(e4, "y1T", (128, 4, 128), BF16)
                        y1b = Buf()
                        sgt = [sb(e4, "sgt%d" % i, (128, 512)) for i in range(2)]
                        sgtb = bufs(2)
                        t1 = sb(e4, "t1", (128, D))
                        t1b = bufs(2)
                        tmp = [sb(e4, "tmp4_%d" % i, (128, 512)) for i in range(2)]
                        tmpb = bufs(2)

                        def load_x4(n):
                            S.dma("sp", xin[n % 2][:], x_d[b, n * 128:(n + 1) * 128, :], xid[n % 2], w=[xib[n % 2]])

                        def p4a_body(n):
                            if n + 1 < NXT:
                                load_x4(n + 1)
                            i = n % 2
                            par = n % 2
                            S.op("act", lambda e: e.activation(out=junk[:], in_=xin[i][:], func=AF.Square,
                                                               accum_out=st[par][:, 0:1]), r=[xib[i]], w=[stb[par], jb])
                            S.op("act", lambda e: e.activation(out=st[par][:, 1:2], in_=st[par][:, 0:1], func=AF.Sqrt,
                                                               bias=EPS, scale=1.0 / D), r=[stb[par]], w=[stb[par]])
                            S.op("dve", lambda e: e.reciprocal(out=st[par][:, 1:2], in_=st[par][:, 1:2]),
                                 r=[stb[par]], w=[stb[par]])
                            S.op("act", lambda e: e.activation(out=xs[:], in_=xin[i][:], func=AF.Copy, scale=st[par][:, 1:2]),
                                 r=[stb[par], xib[i]], w=[xsb])
                            for k in range(KD):
                                S.op("pe", lambda e, k=k: e.transpose(P[k // 4][:, (k % 4) * 128:(k % 4 + 1) * 128],
                                                                      xs[:, k * 128:(k + 1) * 128], ident),
                                     r=[xsb, cb], w=[Pb[k // 4]])
                            for k in range(KD):
                                src = P[k // 4][:, (k % 4) * 128:(k % 4 + 1) * 128]
                                dst = hTt[par][:, k, :]
                                if k // 4 == 0:
                                    S.op("act", lambda e, src=src, dst=dst, k=k: e.activation(
                                        out=dst, in_=src, func=AF.Identity, scale=A1[:, b, k:k + 1], bias=modT[:, b, k:k + 1]),
                                        r=[Pb[k // 4], cb], w=[hTtb[par]])
                                else:
                                    S.op("dve", lambda e, src=src, dst=dst, k=k: e.tensor_scalar(
                                        out=dst, in0=src, scalar1=A1[:, b, k:k + 1], scalar2=modT[:, b, k:k + 1],
                                        op0=ALU.mult, op1=ALU.add), r=[Pb[k // 4], cb], w=[hTtb[par]])
                            yield
                            for k in range(KD):
                                S.op("pe", lambda e, k=k: e.matmul(P[2][:, :], lhsT=hTt[par][:, k, :], rhs=wu[:, k, :],
                                                                   start=(k == 0), stop=(k == KD - 1)),
                                     r=[hTtb[par], w4b], w=[Pb[2]])
                            S.op("dve", lambda e: e.tensor_copy(out=u_sb[:], in_=P[2][:, :]), r=[Pb[2]], w=[ub])
                            for g in range(4):
                                S.op("pe", lambda e, g=g: e.matmul(P[3][:, g * 128:(g + 1) * 128],
                                                                   lhsT=u_sb[:, g * 128:(g + 1) * 128],
                                                                   rhs=poolP[:, g * 128:(g + 1) * 128], start=True, stop=True),
                                     r=[ub, cb], w=[Pb[3]])
                            S.op("act", lambda e: e.activation(out=pT[:], in_=P[3][:, :], func=AF.Copy), r=[Pb[3]], w=[pTb])
                            for g in range(4):
                                S.op("pe", lambda e, g=g: e.matmul(P[2][:, g * 128:(g + 1) * 128], lhsT=pwt[:, g, :],
                                                                   rhs=pT[:, g * 128:(g + 1) * 128], start=True, stop=True),
                                     r=[pTb, cb], w=[Pb[2]])
                            for g in range(4):
                                S.op("act", lambda e, g=g: e.activation(out=y1T[:, g, :], in_=P[2][:, g * 128:(g + 1) * 128],
                                                                        func=AF.Copy, scale=vT[:, 64 + g:65 + g]),
                                     r=[Pb[2], cb], w=[y1b])
                            for hf in range(2):
                                for g in range(4):
                                    S.op("pe", lambda e, hf=hf, g=g: e.matmul(
                                        P[4 + hf][:, :], lhsT=y1T[:, g, :], rhs=wpb[:, g, hf * 512:(hf + 1) * 512],
                                        start=(g == 0), stop=(g == 3)), r=[y1b, w4b], w=[Pb[4 + hf]])
                            yield
                            for j in range(4):
                                bk = 6 if j % 2 == 0 else 3
                                hf = j % 2
                                for k in range(KD):
                                    S.op("pe", lambda e, k=k, j=j, bk=bk: e.matmul(
                                        P[bk][:, :], lhsT=hTt[par][:, k, :], rhs=wgt[:, k, j * 512:(j + 1) * 512],
                                        start=(k == 0), stop=(k == KD - 1)), r=[hTtb[par], w4b], w=[Pb[bk]])
                                S.op("act", lambda e, bk=bk, hf=hf: e.activation(out=sgt[hf][:], in_=P[bk][:, :],
                                                                                 func=AF.Sigmoid),
                                     r=[Pb[bk]], w=[sgtb[hf]])
                                if j < 2:
                                    S.op("dve", lambda e, hf=hf: e.tensor_tensor(
                                        out=t1[:, hf * 512:(hf + 1) * 512], in0=sgt[hf][:], in1=P[4 + hf][:, :], op=ALU.mult),
                                        r=[sgtb[hf], Pb[4 + hf]], w=[t1b[hf]])
                                else:
                                    S.op("dve", lambda e, hf=hf: e.tensor_tensor(
                                        out=tmp[hf][:], in0=sgt[hf][:], in1=acc[:, n, hf * 512:(hf + 1) * 512], op=ALU.mult),
                                        r=[sgtb[hf], accb[n]], w=[tmpb[hf]])
                                    S.op("dve", lambda e, hf=hf: e.tensor_tensor(
                                        out=h2T[:, n, hf * 4:(hf + 1) * 4, :], in0=tmp[hf][:].rearrange("p (k t) -> p k t", t=128),
                                        in1=t1[:, hf * 512:(hf + 1) * 512].rearrange("p (k t) -> p k t", t=128), op=ALU.add),
                                        r=[tmpb[hf], t1b[hf]], w=[h2b[n]])

                        load_x4(0)
                        pipeline(p4a_body(n) for n in range(NXT))
                        S.barrier()
                        if upto == 3:
                            S.dead = True

                    with contextlib.ExitStack() as e4:
                        wo = sb(e4, "wo", (128, KD, D), BF16)
                        w4b = Buf()
                        S.dma("pool", wo[:], wo_d.rearrange("(k p) n -> p k n", p=128), wsem, w=[w4b])
                        g1b = sb(e4, "g1b", (128, D))
                        bcast_row(g1b, 0)
                        xin = [sb(e4, "xin5_%d" % i, (128, D)) for i in range(2)]
                        xib = bufs(2)
                        mTt = [sb(e4, "mTt%d" % i, (128, KD, 128), BF16) for i in range(2)]
                        mTb = bufs(2)
                        tmp2 = sb(e4, "tmp2", (128, D))
                        tmp2b = bufs(2)
                        xs = sb(e4, "xs5", (128, D))
                        xsb = Buf()
                        junk = sb(e4, "junk5", (128, D), BF16)
                        jb = Buf()
                        st = [sb(e4, "st5_%d" % i, (128, 2)) for i in range(2)]
                        stb = bufs(2)
                        lgall = sb(e4, "lgall", (128, NXT, 36))
                        lgb = Buf()
                        rs_ = sb(e4, "rs_", (128, 10, NXT))
                        rg_ = sb(e4, "rg_", (128, 3, NXT, 4))
                        re_ = sb(e4, "re_", (128, 5, NXT, 32))
                        rtb_ = Buf()

                        def load_x5(n):
                            S.dma("sp", xin[n % 2][:], x_d[b, n * 128:(n + 1) * 128, :], xid[n % 2], w=[xib[n % 2]])

                        def p4b_body(n):
                            i = n % 2
                            par = n % 2
                            mv = h2T[:, n, :, :]
                            for k in range(KD):
                                S.op("pe", lambda e, k=k: e.transpose(PB[:, k * 128:(k + 1) * 128], mv[:, k, :], identb[:]),
                                     r=[h2b[n], cb], w=[PBb])
                            yield
                            S.op("act", lambda e: e.activation(out=mTt[par][:, :, :],
                                                               in_=PB[:, :].rearrange("p (k t) -> p k t", t=128), func=AF.Copy),
                                 r=[PBb], w=[mTb[par]])
                            yield
                            if n + 1 < NXT:
                                load_x5(n + 1)
                            for hf in range(2):
                                for k in range(KD):
                                    S.op("pe", lambda e, hf=hf, k=k: e.matmul(
                                        P[hf][:, :], lhsT=mTt[par][:, k, :], rhs=wo[:, k, hf * 512:(hf + 1) * 512],
                                        start=(k == 0), stop=(k == KD - 1)), r=[mTb[par], w4b], w=[Pb[hf]])
                            yield
                            for hf in range(2):
                                sl = slice(hf * 512, (hf + 1) * 512)
                                S.op("dve", lambda e, hf=hf, sl=sl: e.tensor_tensor(out=tmp2[:, sl], in0=P[hf][:, :],
                                                                                    in1=g1b[:, sl], op=ALU.mult),
                                     r=[Pb[hf], cb], w=[tmp2b[hf]])
                                S.op("dve", lambda e, sl=sl: e.tensor_tensor(out=acc[:, n, sl], in0=tmp2[:, sl],
                                                                              in1=xin[i][:, sl], op=ALU.add),
                                     r=[tmp2b[hf], xib[i]], w=[accb[n]])
                            yield
                            S.op("act", lambda e: e.activation(out=junk[:], in_=acc[:, n, :], func=AF.Square,
                                                               accum_out=st[par][:, 0:1]), r=[accb[n]], w=[stb[par], jb])
                            S.op("act", lambda e: e.activation(out=st[par][:, 1:2], in_=st[par][:, 0:1], func=AF.Ln,
                                                               bias=EPS, scale=1.0 / D), r=[stb[par]], w=[stb[par]])
                            S.op("act", lambda e: e.activation(out=st[par][:, 1:2], in_=st[par][:, 1:2], func=AF.Exp,
                                                               scale=-0.5), r=[stb[par]], w=[stb[par]])
                            S.op("act", lambda e: e.activation(out=xs[:], in_=acc[:, n, :], func=AF.Copy,
                                                               scale=st[par][:, 1:2]), r=[stb[par], accb[n]], w=[xsb])
                            yield
                            for k in range(KD):
                                S.op("pe", lambda e, k=k: e.transpose(P[2 + k // 4][:, (k % 4) * 128:(k % 4 + 1) * 128],
                                                                      xs[:, k * 128:(k + 1) * 128], ident),
                                     r=[xsb, cb], w=[Pb[2 + k // 4]])
                            yield
                            for k in range(KD):
                                src = P[2 + k // 4][:, (k % 4) * 128:(k % 4 + 1) * 128]
                                dst = h2T[:, n, k, :]
                                if k // 4 == 0:
                                    S.op("act", lambda e, src=src, dst=dst, k=k: e.activation(
                                        out=dst, in_=src, func=AF.Identity, scale=A2[:, b, k:k + 1],
                                        bias=modT[:, b, 24 + k:25 + k]), r=[Pb[2 + k // 4], cb], w=[h2b[n]])
                                else:
                                    S.op("dve", lambda e, src=src, dst=dst, k=k: e.tensor_scalar(
                                        out=dst, in0=src, scalar1=A2[:, b, k:k + 1], scalar2=modT[:, b, 24 + k:25 + k],
                                        op0=ALU.mult, op1=ALU.add), r=[Pb[2 + k // 4], cb], w=[h2b[n]])
                            yield
                            for k in range(KD):
                                S.op("pe", lambda e, k=k: e.matmul(P[4][:, 0:36], lhsT=h2T[:, n, k, :], rhs=rw[:, k, :],
                                                                   start=(k == 0), stop=(k == KD - 1)),
                                     r=[h2b[n], cb], w=[Pb[4]])
                            yield
                            S.op("dve", lambda e: e.tensor_tensor(out=lgall[:, n, :], in0=P[4][:, 0:36], in1=rbb[:], op=ALU.add),
                                 r=[Pb[4], cb], w=[lgb])

                        load_x5(0)
                        pipeline(p4b_body(n) for n in range(NXT))
                        T_ = NXT
                        X = mybir.AxisListType.X
                        gl = lgall[:, :, 0:4]
                        el4 = lgall[:, :, 4:36].rearrange("p t (g e) -> p t g e", e=8)
                        gmax, ngs, pg, m1, m2, dd, e2, den, w1_, w2_ = (rs_[:, q, :] for q in range(10))
                        gm, pen, ge = (rg_[:, q, :, :] for q in range(3))
                        em, oh1, em2, oh2, cwt = (re_[:, q, :, :] for q in range(5))

                        def bc(ap2, width):
                            return ap2.unsqueeze(2).to_broadcast([128, T_, width])

                        def dv(fn):
                            S.op("dve", fn, r=[lgb, rtb_], w=[rtb_])

                        dv(lambda e: e.tensor_reduce(out=gmax, in_=gl, axis=X, op=ALU.max))
                        dv(lambda e: e.tensor_tensor(out=gm, in0=gl, in1=bc(gmax, 4), op=ALU.is_equal))
                        dv(lambda e: e.tensor_tensor(out=ge, in0=gl, in1=bc(gmax, 4), op=ALU.subtract))
                        S.op("act", lambda e: e.activation(out=ge, in_=ge, func=AF.Exp), r=[rtb_], w=[rtb_])
                        dv(lambda e: e.tensor_reduce(out=ngs, in_=ge, axis=X, op=ALU.add))
                        dv(lambda e: e.reciprocal(out=pg, in_=ngs))
                        dv(lambda e: e.tensor_scalar(out=pen, in0=gm, scalar1=-1.0, scalar2=1e30, op0=ALU.add, op1=ALU.mult))
                        em4 = em.rearrange("p t (g e) -> p t g e", e=8)
                        dv(lambda e: e.tensor_tensor(out=em4, in0=el4,
                                                     in1=pen.unsqueeze(3).to_broadcast([128, T_, 4, 8]), op=ALU.add))
                        dv(lambda e: e.tensor_reduce(out=m1, in_=em, axis=X, op=ALU.max))
                        dv(lambda e: e.tensor_tensor(out=oh1, in0=em, in1=bc(m1, 32), op=ALU.is_equal))
                        dv(lambda e: e.scalar_tensor_tensor(out=em2.rearrange("p t e -> p (t e)"),
                                                            in0=oh1.rearrange("p t e -> p (t e)"), scalar=-1e30, op0=ALU.mult,
                                                            in1=em.rearrange("p t e -> p (t e)"), op1=ALU.add))
                        dv(lambda e: e.tensor_reduce(out=m2, in_=em2, axis=X, op=ALU.max))
                        dv(lambda e: e.tensor_tensor(out=oh2, in0=em2, in1=bc(m2, 32), op=ALU.is_equal))
                        dv(lambda e: e.tensor_tensor(out=dd, in0=m2, in1=m1, op=ALU.subtract))
                        S.op("act", lambda e: e.activation(out=e2, in_=dd, func=AF.Exp), r=[rtb_], w=[rtb_])
                        dv(lambda e: e.tensor_scalar(out=den, in0=e2, scalar1=1.0, scalar2=None, op0=ALU.add))
                        dv(lambda e: e.reciprocal(out=den, in_=den))
                        dv(lambda e: e.tensor_tensor(out=w1_, in0=den, in1=pg, op=ALU.mult))
                        dv(lambda e: e.tensor_tensor(out=w2_, in0=w1_, in1=e2, op=ALU.mult))
                        dv(lambda e: e.tensor_tensor(out=cwt, in0=oh1, in1=bc(w1_, 32), op=ALU.mult))
                        dv(lambda e: e.tensor_tensor(out=oh2, in0=oh2, in1=bc(w2_, 32), op=ALU.mult))
                        S.op("dve", lambda e: e.tensor_tensor(out=cw[:, :, :], in0=cwt, in1=oh2, op=ALU.add),
                             r=[rtb_], w=cwb)
                        if debug:
                            for n in range(2):
                                dump(acc[:, n, :], D, accb)
                            dump(cw[:, 0, :], 32, cwb)
                            dump(cw[:, 1, :], 32, cwb)
                        S.barrier()
                        if upto == 4:
                            S.dead = True

                    with contextlib.ExitStack() as e5:
                        g2b = sb(e5, "g2b", (128, D))
                        bcast_row(g2b, 1)
                        w1b = [sb(e5, "w1b%d" % i, (128, KD, DE), BF16) for i in range(2)]
                        w3b = [sb(e5, "w3b%d" % i, (128, KD, DE), BF16) for i in range(2)]
                        w2s = sb(e5, "w2s", (128, 4, D))
                        w2b = [sb(e5, "w2b%d" % i, (128, 4, D), BF16) for i in range(2)]
                        wb13 = bufs(2)
                        w2sb = Buf()
                        w2bb = bufs(2)
                        sa = [sb(e5, "sa%d" % i, (128, 512)) for i in range(2)]
                        sab = bufs(2)
                        hid = [sb(e5, "hid%d" % i, (128, 4, 512), BF16) for i in range(2)]
                        hidb = bufs(2)

                        def load_w(e_):
                            sl_ = e_ % 2
                            S.dma("pool", w1b[sl_][:], w1_d[e_].rearrange("(k p) f -> p k f", p=128), wm13[sl_], w=[wb13[sl_]])
                            S.dma("pool", w3b[sl_][:], w3_d[e_].rearrange("(k p) f -> p k f", p=128), wm13[sl_], w=[wb13[sl_]])
                            S.dma("sp", w2s[:], w2_d[e_].rearrange("(c p) n -> p c n", p=128), w2sem, w=[w2sb])
                            for c in range(4):
                                S.op("dve", lambda e, c=c, sl_=sl_: e.tensor_tensor(out=w2b[sl_][:, c, :], in0=w2s[:, c, :],
                                                                                     in1=g2b[:], op=ALU.mult),
                                     r=[w2sb, cb], w=[w2bb[sl_]])

                        items = [(e_, tg) for e_ in range(NEXP) for tg in range(4)]

                        def emit_ab(idx):
                            e_, tg = items[idx]
                            sl_ = e_ % 2
                            hp = idx % 2
                            for f in range(4):
                                ba, bb_ = (0, 1) if f % 2 == 0 else (2, 3)
                                sp_ = f % 2
                                for (wsrc, bk) in ((w1b, ba), (w3b, bb_)):
                                    for k in range(KD):
                                        S.op("pe", lambda e, wsrc=wsrc, bk=bk, k=k, f=f: e.matmul(
                                            P[bk][:, :].rearrange("p (t d) -> p t d", d=128),
                                            lhsT=wsrc[sl_][:, k, f * 128:(f + 1) * 128],
                                            rhs=h2T[:, tg * 4:(tg + 1) * 4, k, :], start=(k == 0), stop=(k == KD - 1)),
                                            r=[wb13[sl_]] + h2b[tg * 4:(tg + 1) * 4], w=[Pb[bk]])
                                S.op("act", lambda e, ba=ba, sp_=sp_: e.activation(out=sa[sp_][:], in_=P[ba][:, :],
                                                                                   func=AF.Silu),
                                     r=[Pb[ba]], w=[sab[sp_]])
                                S.op("dve", lambda e, bb_=bb_, sp_=sp_, f=f: e.tensor_tensor(
                                    out=hid[hp][:, f, :], in0=sa[sp_][:], in1=P[bb_][:, :], op=ALU.mult),
                                    r=[sab[sp_], Pb[bb_]], w=[hidb[hp]])

                        def emit_w2(idx):
                            e_, tg = items[idx]
                            sl_ = e_ % 2
                            hp = idx % 2
                            for j in range(4):
                                n = tg * 4 + j
                                for hf in range(2):
                                    bk = 4 + (j * 2 + hf) % 3
                                    for f in range(4):
                                        S.op("pe", lambda e, bk=bk, f=f, j=j, hf=hf: e.matmul(
                                            P[bk][:, :], lhsT=hid[hp][:, f, j * 128:(j + 1) * 128],
                                            rhs=w2b[sl_][:, f, hf * 512:(hf + 1) * 512], start=(f == 0), stop=(f == 3)),
                                            r=[hidb[hp], w2bb[sl_]], w=[Pb[bk]])
                                    dst = acc[:, n, hf * 512:(hf + 1) * 512]
                                    S.op("dve", lambda e, bk=bk, dst=dst, n=n: e.scalar_tensor_tensor(
                                        out=dst, in0=P[bk][:, :], scalar=cw[:, n, e_:e_ + 1], op0=ALU.mult, in1=dst,
                                        op1=ALU.add), r=[Pb[bk], cwb[n], accb[n]], w=[accb[n]])

                        load_w(0)
                        if NEXP > 1:
                            load_w(1)
                        emit_ab(0)
                        for idx in range(len(items)):
                            if idx + 1 < len(items):
                                emit_ab(idx + 1)
                            emit_w2(idx)
                            e_, tg = items[idx]
                            if tg == 3 and e_ + 2 < NEXP:
                                load_w(e_ + 2)
                        S.barrier()
                        if upto == 5:
                            S.dead = True

                    with contextlib.ExitStack() as e6:
                        fgb = sb(e6, "fgb", (128, D))
                        fb = Buf()
                        S.dma("sp", fgb[:], fg_d.rearrange("(o n) -> o n", o=1).to_broadcast([128, D]), dsl, w=[fb])
                        ot = [sb(e6, "ot%d" % i, (128, D)) for i in range(2)]
                        otb_ = bufs(2)
                        junk = sb(e6, "junk6", (128, D), BF16)
                        jb = Buf()
                        st = [sb(e6, "st6_%d" % i, (128, 2)) for i in range(2)]
                        stb = bufs(2)
                        for n in range(NXT):
                            par = n % 2
                            S.op("act", lambda e: e.activation(out=junk[:], in_=acc[:, n, :], func=AF.Square,
                                                               accum_out=st[par][:, 0:1]), r=[accb[n]], w=[stb[par], jb])
                            S.op("act", lambda e: e.activation(out=st[par][:, 1:2], in_=st[par][:, 0:1], func=AF.Sqrt,
                                                               bias=EPS, scale=1.0 / D), r=[stb[par]], w=[stb[par]])
                            S.op("dve", lambda e: e.reciprocal(out=st[par][:, 1:2], in_=st[par][:, 1:2]),
                                 r=[stb[par]], w=[stb[par]])
                            S.op("dve", lambda e: e.scalar_tensor_tensor(out=ot[par][:], in0=acc[:, n, :],
                                                                         scalar=st[par][:, 1:2], op0=ALU.mult, in1=fgb[:],
                                                                         op1=ALU.mult), r=[accb[n], stb[par], fb], w=[otb_[par]])
                            S.dma("sp", out_d[b, n * 128:(n + 1) * 128, :], ot[par][:], osem[par], r=[otb_[par]])
                        S.barrier()
                        if upto == 6:
                            S.dead = True
                    S.barrier()
        except _Stop:
            pass
        S.dead = False
        S.barrier()
    return nc


_CONSTS = None


def _consts():
    global _CONSTS
    if _CONSTS is None:
        j = np.arange(128)[:, None]
        i = np.arange(128)[None, :]
        cst = np.zeros((128, 898), np.float32)
        cst[:, 0:128] = np.eye(128, dtype=np.float32)
        s = -1.0 / 16.0
        cst[:, 128:256] = (j <= i) * s
        cst[:, 256:384] = (j > i) * s
        cst[:, 384:512] = (j >= i) * s
        cst[:, 512:640] = (j < i) * s
        cst[:, 640:768] = (j <= i)
        cst[:, 768:896] = (j >= i)
        cst[:, 896:898] = s
        poolP = np.zeros((128, 4, 128), np.float32)
        for gi, w in enumerate((2, 4, 8, 16)):
            for t in range(128):
                r0 = (t // 64) * 64
                lo = max(t - w // 2, r0)
                hi = min(t + w // 2, r0 + 64)
                poolP[lo:hi, gi, t] = 1.0 / (hi - lo)
                poolP[t, gi, t] -= 1.0
        sel = np.zeros((8, 8, 128), np.float32)
        for r in range(8):
            sel[r, r, :] = 1.0
        _CONSTS = (cst, poolP.reshape(128, 512), sel.reshape(8, 1024))
    return _CONSTS


_PER_LAYER = ("w_mod", "b_mod", "norm1_g", "norm2_g", "w_in", "gla_a2_f", "gla_ab_f", "gla_a2_b", "gla_ab_b",
              "gla_onorm_g", "pool_w", "pool_scale", "w_pool_br", "w_gla_br", "w_o", "router_grp_w", "router_grp_b",
              "router_exp_w", "router_exp_b", "moe_w1", "moe_w3", "moe_w2")


def kernel(**inputs):
    NB = 4
    x = np.asarray(inputs["x"], np.float32)
    c = np.asarray(inputs["c"], np.float32)
    ctx = np.asarray(inputs["ctx"], np.float32)
    c_ctx = np.asarray(inputs["c_ctx"], np.float32)
    cst, poolP, sel = _consts()
    shared = {k: np.ascontiguousarray(np.asarray(inputs[k], np.float32)[0]) for k in _PER_LAYER}
    shared["final_norm_g"] = np.ascontiguousarray(np.asarray(inputs["final_norm_g"], np.float32))
    shared["cst"] = cst
    shared["poolP"] = poolP
    shared["sel"] = sel
    in_maps = []
    for core in range(NCORES):
        b0 = core * NB
        m = dict(shared)
        m["x"] = np.ascontiguousarray(x[b0:b0 + NB])
        m["ctx"] = np.ascontiguousarray(ctx[b0:b0 + NB])
        m["cT"] = np.ascontiguousarray(np.concatenate([c[b0:b0 + NB], c_ctx[None, :]], axis=0).T)
        in_maps.append(m)
    nc = build_program(NB=NB)
    res = run_bass_kernel_spmd(nc, in_maps, core_ids=list(range(NCORES)))
    return np.concatenate([np.asarray(r["out"], np.float32) for r in res.results], axis=0)
```

```python
import contextlib
import os
import numpy as np
import concourse.bass as bass
import concourse.mybir as mybir
from concourse.bass_utils import run_bass_kernel_spmd

F32 = mybir.dt.float32
BF16 = mybir.dt.bfloat16
AF = mybir.ActivationFunctionType
ALU = mybir.AluOpType

D = 1024
SEQ = 2048
CTXL = 256
KD = 8
NXT = SEQ // 128
NCT = CTXL // 128
NTT = NXT + NCT
TOK = NTT * 128
D_IN = 5664
C_POOL, C_Q, C_K, C_V, C_G, C_GATES, C_LR = 0, 512, 1024, 1536, 2560, 3584, 5632
NHEAD = 4
HK = 128
HV = 256
NEXP_FULL = 32
DE = 512
EPS = 1e-6
NCORES = 8
STRICT_SAME = not os.environ.get("K_NOSTRICT")
POOLENG = "dve" if os.environ.get("K_NOPOOL") else "pool"


class Buf:
    __slots__ = ("lw", "rd", "excl")

    def __init__(self, excl=False):
        self.lw = None
        self.rd = {}
        self.excl = excl


def bufs(n):
    return [Buf() for _ in range(n)]


class DSem:
    def __init__(self, sem):
        self.sem = sem
        self.count = 0
        self.q = None
        self.maxw = 0


class Sched:
    def __init__(self, nc, es):
        self.nc = nc
        self.es = es
        self.E = {}
        for name, eng in (("pe", nc.tensor), ("act", nc.scalar), ("dve", nc.vector),
                          ("pool", nc.gpsimd), ("sp", nc.sync)):
            sem = es.enter_context(nc.semaphore("s_" + name))
            self.E[name] = dict(eng=eng, sem=sem, count=0, waited={})
        self.dsems = []
        self.nds = 0
        self.dsmap = {}
        self.dead = False

    def new_dsem(self):
        self.nds += 1
        ds = DSem(self.es.enter_context(self.nc.semaphore("d%d" % self.nds)))
        self.dsems.append(ds)
        self.dsmap[id(ds.sem)] = ds
        return ds

    def _wait(self, en, toks):
        E = self.E[en]
        for (sem, val, owner) in toks:
            if owner == en and (en == "pe" or not STRICT_SAME):
                continue
            k = id(sem)
            if k in self.dsmap and val > self.dsmap[k].maxw:
                self.dsmap[k].maxw = val
            if E["waited"].get(k, 0) >= val:
                continue
            E["eng"].wait_ge(sem, val)
            E["waited"][k] = val

    @staticmethod
    def _deps(r, w, en=None):
        toks = []
        for b in r:
            if b.lw:
                toks.append(b.lw)
            if b.excl:
                toks.extend(t for e_, t in b.rd.items() if e_ != en)
        for b in w:
            if b.lw:
                toks.append(b.lw)
            toks.extend(b.rd.values())
        return toks

    def op(self, en, fn, r=(), w=()):
        if self.dead:
            return
        self._wait(en, self._deps(r, w, en))
        E = self.E[en]
        ins = fn(E["eng"])
        E["count"] += 1
        ins.then_inc(E["sem"], 1)
        tok = (E["sem"], E["count"], en)
        for b in w:
            b.lw = tok
            b.rd = {}
        for b in r:
            b.rd[en] = tok

    def dma(self, qn, out, in_, ds, r=(), w=()):
        if self.dead:
            return
        self._wait(qn, self._deps(r, w))
        E = self.E[qn]
        assert ds.q in (None, qn), "DMA semaphore shared between queues"
        ds.q = qn
        if ds.maxw > 0:
            self._wait(qn, [(ds.sem, ds.maxw, None)])
        ins = E["eng"].dma_start(out=out, in_=in_)
        ds.count += 16
        ins.then_inc(ds.sem, 16)
        tok = (ds.sem, ds.count, None)
        for b in w:
            b.lw = tok
            b.rd = {}
        for b in r:
            b.rd["dma%d" % id(ds)] = tok

    def barrier(self):
        if self.dead:
            return
        for en, E in self.E.items():
            toks = []
            for en2, E2 in self.E.items():
                if en2 != en and E2["count"] > 0:
                    toks.append((E2["sem"], E2["count"], en2))
            for ds in self.dsems:
                if ds.count > 0:
                    toks.append((ds.sem, ds.count, None))
            self._wait(en, toks)


def pipeline(gens):
    it = iter(gens)
    active = []
    while True:
        g = next(it, None)
        if g is not None:
            active.append(g)
        if not active:
            break
        for g_ in list(active):
            try:
                next(g_)
            except StopIteration:
                active.remove(g_)


class _Stop(Exception):
    pass


def build_program(NB=4, NEXP=NEXP_FULL, debug=None, upto=99):
    nc = bass.Bass("TRN2", target_bir_lowering=False)

    def din(name, shape):
        return nc.dram_tensor(name, list(shape), F32, kind="ExternalInput").ap()

    x_d = din("x", (NB, SEQ, D))
    ctx_d = din("ctx", (NB, CTXL, D))
    cT_d = din("cT", (D, NB + 1))
    w_mod_d = din("w_mod", (D, 6 * D))
    b_mod_d = din("b_mod", (6 * D,))
    n1g_d = din("norm1_g", (D,))
    n2g_d = din("norm2_g", (D,))
    w_in_d = din("w_in", (D, D_IN))
    a2f_d = din("gla_a2_f", (16, 512))
    abf_d = din("gla_ab_f", (512,))
    a2b_d = din("gla_a2_b", (16, 512))
    abb_d = din("gla_ab_b", (512,))
    gn_d = din("gla_onorm_g", (HV,))
    pw_d = din("pool_w", (4, 128, 128))
    psc_d = din("pool_scale", (512,))
    wpb_d = din("w_pool_br", (512, D))
    wgb_d = din("w_gla_br", (D, D))
    wo_d = din("w_o", (D, D))
    rgw_d = din("router_grp_w", (D, 4))
    rgb_d = din("router_grp_b", (4,))
    rew_d = din("router_exp_w", (D, 32))
    reb_d = din("router_exp_b", (32,))
    w1_d = din("moe_w1", (NEXP_FULL, D, DE))
    w3_d = din("moe_w3", (NEXP_FULL, D, DE))
    w2_d = din("moe_w2", (NEXP_FULL, DE, D))
    fg_d = din("final_norm_g", (D,))
    cst_d = din("cst", (128, 898))
    poolP_d = din("poolP", (128, 512))
    sel_d = din("sel", (8, 8 * 128))
    out_d = nc.dram_tensor("out", [NB, SEQ, D], F32, kind="ExternalOutput").ap()
    dbg_d = None
    if debug:
        dbg_d = nc.dram_tensor("dbg", [128, debug], F32, kind="ExternalOutput").ap()

    NBC = NB + 1

    with contextlib.ExitStack() as es:
        S = Sched(nc, es)

        uid = [0]

        def sb(es_, name, shape, dt=F32):
            uid[0] += 1
            return es_.enter_context(nc.sbuf_tensor("%s_s%d" % (name, uid[0]), list(shape), dt))

        P = [es.enter_context(nc.psum_tensor("P%d" % i, [128, 512], F32)) for i in range(7)]
        PB = es.enter_context(nc.psum_tensor("PB", [128, 1024], BF16))
        Pb = [Buf(excl=True) for _ in range(7)]
        PBb = Buf(excl=True)

        cst = sb(es, "cst", (128, 898))
        identb = sb(es, "identb", (128, 128), BF16)
        poolP = sb(es, "poolP", (128, 512), BF16)
        sel = sb(es, "sel", (8, 1024))
        vstage = sb(es, "vstage", (68, 128))
        vT = sb(es, "vT", (128, 68))
        gnb = sb(es, "gnb", (128, HV))
        rbb = sb(es, "rbb", (128, 36))
        a2pf = sb(es, "a2pf", (32, 512), BF16)
        a2pb = sb(es, "a2pb", (32, 512), BF16)
        wlr = sb(es, "wlr", (128, KD, 32), BF16)
        rw = sb(es, "rw", (128, KD, 36), BF16)
        pwt = sb(es, "pwt", (128, 4, 128), BF16)
        sct = sb(es, "sct", (128, KD, NBC))
        modT = sb(es, "modT", (128, NBC, 48))
        msb = sb(es, "msb", (NBC, 2, D))
        A1 = sb(es, "A1", (128, NBC, KD))
        A2 = sb(es, "A2", (128, NBC, KD))
        cb = Buf()
        dsc = S.new_dsem()
        dscp = S.new_dsem()
        xid = [S.new_dsem() for _ in range(3)]
        wsem = S.new_dsem()
        wgsem = S.new_dsem()
        w2sem = S.new_dsem()
        dsl = S.new_dsem()
        dbgsem = S.new_dsem()
        wm13 = [S.new_dsem() for _ in range(2)]
        osem = [S.new_dsem() for _ in range(2)]

        ident = cst[:, 0:128]

        def tri(i):
            return cst[:, 128 + i * 128: 256 + i * 128]

        def msk(i):
            return cst[:, 640 + i * 128: 768 + i * 128]

        negcol = cst[:, 896:898]

        a2B = Buf()
        S.op("dve", lambda e: e.memset(a2pf[:], 0.0), w=[a2B])
        S.op("dve", lambda e: e.memset(a2pb[:], 0.0), w=[a2B])
        S.dma("sp", cst[:], cst_d, dsc, w=[cb])
        S.dma("sp", sel[:], sel_d, dsc, w=[cb])
        S.dma("sp", vstage[0:48, :], b_mod_d.rearrange("(j p) -> j p", p=128), dsc, w=[cb])
        S.dma("sp", vstage[48:56, :], n1g_d.rearrange("(j p) -> j p", p=128), dsc, w=[cb])
        S.dma("sp", vstage[56:64, :], n2g_d.rearrange("(j p) -> j p", p=128), dsc, w=[cb])
        S.dma("sp", vstage[64:68, :], psc_d.rearrange("(j p) -> j p", p=128), dsc, w=[cb])
        S.dma("sp", gnb[:], gn_d.rearrange("(o n) -> o n", o=1).to_broadcast([128, HV]), dsc, w=[cb])
        S.dma("sp", rbb[:, 0:4], rgb_d.rearrange("(o n) -> o n", o=1).to_broadcast([128, 4]), dsc, w=[cb])
        S.dma("sp", rbb[:, 4:36], reb_d.rearrange("(o n) -> o n", o=1).to_broadcast([128, 32]), dsc, w=[cb])
        S.dma("sp", sct[:], cT_d.rearrange("(k p) b -> p k b", p=128), dsc, w=[cb])
        S.dma("pool", a2pf[0:16, :], a2f_d, dscp, w=[cb, a2B])
        S.dma("pool", a2pb[16:32, :], a2b_d, dscp, w=[cb, a2B])
        S.dma("pool", poolP[:], poolP_d, dscp, w=[cb])
        S.dma("pool", wlr[:], w_in_d[:, C_LR:C_LR + 32].rearrange("(k p) n -> p k n", p=128), dscp, w=[cb])
        S.dma("pool", rw[:, :, 0:4], rgw_d.rearrange("(k p) n -> p k n", p=128), dscp, w=[cb])
        S.dma("pool", rw[:, :, 4:36], rew_d.rearrange("(k p) n -> p k n", p=128), dscp, w=[cb])
        S.dma("pool", pwt[:], pw_d.rearrange("g c e -> c g e"), dscp, w=[cb])
        S.op("dve", lambda e: e.tensor_copy(out=identb[:], in_=ident), r=[cb], w=[cb])
        S.op("act", lambda e: e.activation(out=sct[:], in_=sct[:], func=AF.Silu), r=[cb], w=[cb])
        S.op("pe", lambda e: e.transpose(P[0][:, 0:68], vstage[:], cst[0:68, 0:68]), r=[cb], w=[Pb[0]])
        S.op("dve", lambda e: e.tensor_copy(out=vT[:], in_=P[0][:, 0:68]), r=[Pb[0]], w=[cb])

        with contextlib.ExitStack() as p0:
            wm = [sb(p0, "wm%d" % i, (128, KD, 512)) for i in range(2)]
            bm5 = sb(p0, "bm5", (NBC, 2, D))
            for ci, ch in enumerate((2, 5)):
                S.dma("sp", bm5[:, ci, :],
                      b_mod_d[ch * D:(ch + 1) * D].rearrange("(o n) -> o n", o=1).to_broadcast([NBC, D]),
                      dsl, w=[cb])
            wmb = bufs(2)
            wmd = [S.new_dsem() for _ in range(2)]
            for cg in range(12):
                i = cg % 2
                S.dma("sp", wm[i][:], w_mod_d[:, cg * 512:(cg + 1) * 512].rearrange("(k p) n -> p k n", p=128),
                      wmd[i], w=[wmb[i]])
                for m in range(4):
                    j = cg * 4 + m
                    for k in range(KD):
                        S.op("pe", lambda e, i=i, m=m, k=k, j=j: e.matmul(
                            P[1][:, j * 8:j * 8 + NBC], lhsT=wm[i][:, k, m * 128:(m + 1) * 128],
                            rhs=sct[:, k, :], start=(k == 0), stop=(k == KD - 1)),
                            r=[wmb[i], cb], w=[Pb[1]])
                if cg // 2 in (2, 5):
                    ci = 0 if cg // 2 == 2 else 1
                    hf = cg % 2
                    for k in range(KD):
                        S.op("pe", lambda e, i=i, k=k: e.matmul(
                            P[2][0:NBC, :], lhsT=sct[:, k, :], rhs=wm[i][:, k, :],
                            start=(k == 0), stop=(k == KD - 1)), r=[wmb[i], cb], w=[Pb[2]])
                    S.op("dve", lambda e, ci=ci, hf=hf: e.tensor_tensor(
                        out=msb[:, ci, hf * 512:(hf + 1) * 512], in0=P[2][0:NBC, :],
                        in1=bm5[:, ci, hf * 512:(hf + 1) * 512], op=ALU.add), r=[Pb[2], cb], w=[cb])
            p1v = P[1][:, 0:384].rearrange("p (j e) -> p j e", e=8)
            for b in range(NBC):
                S.op("dve", lambda e, b=b: e.tensor_tensor(out=modT[:, b, :], in0=p1v[:, :, b], in1=vT[:, 0:48],
                                                           op=ALU.add), r=[Pb[1], cb], w=[cb])
            for b in range(NBC):
                S.op("dve", lambda e, b=b: e.scalar_tensor_tensor(
                    out=A1[:, b, :], in0=modT[:, b, 8:16], scalar=1.0, op0=ALU.add, in1=vT[:, 48:56], op1=ALU.mult),
                    r=[cb], w=[cb])
                S.op("dve", lambda e, b=b: e.scalar_tensor_tensor(
                    out=A2[:, b, :], in0=modT[:, b, 32:40], scalar=1.0, op0=ALU.add, in1=vT[:, 56:64], op1=ALU.mult),
                    r=[cb], w=[cb])
            S.barrier()

        dbg_off = [0]

        def dump(ap_, n, rb, np_=128):
            if dbg_d is None:
                return
            o = dbg_off[0]
            S.dma("pool", dbg_d[0:np_, o:o + n], ap_, dbgsem, r=rb)
            dbg_off[0] = o + n

        def rms_rstd(es_unused, src_ap, width, ss, rs, junk, rb, sb_):
            S.op("act", lambda e: e.activation(out=junk, in_=src_ap, func=AF.Square, accum_out=ss[:, 0:1]),
                 r=rb, w=[sb_])
            S.op("act", lambda e: e.activation(out=rs[:, 0:1], in_=ss[:, 0:1], func=AF.Sqrt, bias=EPS,
                                               scale=1.0 / width), r=[sb_], w=[sb_])
            S.op("dve", lambda e: e.reciprocal(out=rs[:, 0:1], in_=rs[:, 0:1]), r=[sb_], w=[sb_])

        try:
            for b in range(NB):
                with contextlib.ExitStack() as eb:
                    acc = sb(eb, "acc", (128, NXT, D))
                    accb = bufs(NXT)
                    with contextlib.ExitStack() as e13:
                        hT = sb(e13, "hT", (128, KD, TOK), BF16)
                        hTb = bufs(NTT)
                        lrT = sb(e13, "lrT", (32, TOK), BF16)
                        lrb = Buf()
                        with contextlib.ExitStack() as e1:
                            xin = [sb(e1, "xin%d" % i, (128, D)) for i in range(3)]
                            xib = bufs(3)
                            junk = sb(e1, "junk1", (128, D), BF16)
                            st = [sb(e1, "st1_%d" % i, (128, 2)) for i in range(2)]
                            stb = bufs(2)
                            jb = Buf()

                            def load_x(tt):
                                i = tt % 3
                                src = ctx_d[b, tt * 128:(tt + 1) * 128, :] if tt < NCT else \
                                    x_d[b, (tt - NCT) * 128:(tt - NCT + 1) * 128, :]
                                S.dma("sp", xin[i][:], src, xid[i], w=[xib[i]])

                            def p1_body(tt):
                                if tt + 2 < NTT:
                                    load_x(tt + 2)
                                i = tt % 3
                                s_ = st[tt % 2]
                                sbb = stb[tt % 2]
                                mb = NB if tt < NCT else b
                                S.op("act", lambda e: e.activation(
                                    out=junk[:], in_=xin[i][:], func=AF.Square, accum_out=s_[:, 0:1]),
                                    r=[xib[i]], w=[sbb, jb])
                                S.op("act", lambda e: e.activation(
                                    out=s_[:, 1:2], in_=s_[:, 0:1], func=AF.Sqrt, bias=EPS, scale=1.0 / D),
                                    r=[sbb], w=[sbb])
                                S.op("dve", lambda e: e.reciprocal(out=s_[:, 1:2], in_=s_[:, 1:2]),
                                     r=[sbb], w=[sbb])
                                S.op("dve", lambda e: e.tensor_scalar(
                                    out=xin[i][:], in0=xin[i][:], scalar1=s_[:, 1:2], scalar2=None, op0=ALU.mult),
                                    r=[sbb, xib[i]], w=[xib[i]])
                                yield
                                pbase = (tt % 2) * 2
                                for k in range(KD):
                                    bk = pbase + k // 4
                                    S.op("pe", lambda e, k=k, bk=bk: e.transpose(
                                        P[bk][:, (k % 4) * 128:(k % 4 + 1) * 128], xin[i][:, k * 128:(k + 1) * 128], ident),
                                        r=[xib[i], cb], w=[Pb[bk]])
                                yield
                                for k in range(KD):
                                    bk = pbase + k // 4
                                    src = P[bk][:, (k % 4) * 128:(k % 4 + 1) * 128]
                                    dst = hT[:, k, tt * 128:(tt + 1) * 128]
                                    if k // 4 == 0:
                                        S.op("act", lambda e, src=src, dst=dst, k=k: e.activation(
                                            out=dst, in_=src, func=AF.Identity, scale=A1[:, mb, k:k + 1],
                                            bias=modT[:, mb, k:k + 1]), r=[Pb[bk], cb], w=[hTb[tt]])
                                    else:
                                        S.op("dve", lambda e, src=src, dst=dst, k=k: e.tensor_scalar(
                                            out=dst, in0=src, scalar1=A1[:, mb, k:k + 1], scalar2=modT[:, mb, k:k + 1],
                                            op0=ALU.mult, op1=ALU.add), r=[Pb[bk], cb], w=[hTb[tt]])

                            load_x(0)
                            load_x(1)
                            pipeline(p1_body(tt) for tt in range(NTT))
                            S.barrier()
                            if upto == 1:
                                S.dead = True
                        with contextlib.ExitStack() as e23:
                            whd = sb(e23, "whd", (128, KD, 768), BF16)
                            abfb = sb(e23, "abfb", (128, 512))
                            abbb = sb(e23, "abbb", (128, 512))
                            abB = Buf()
                            S.dma("sp", abfb[:], abf_d.rearrange("(o n) -> o n", o=1).to_broadcast([128, 512]), dsl, w=[abB])
                            S.dma("sp", abbb[:], abb_d.rearrange("(o n) -> o n", o=1).to_broadcast([128, 512]), dsl, w=[abB])
                            wgl = sb(e23, "wgl", (128, 2, D), BF16)
                            whb = Buf()
                            qT = sb(e23, "qT", (128, SEQ), BF16)
                            qTb = bufs(4)
                            kT = sb(e23, "kT", (128, TOK), BF16)
                            kTb = bufs(5)
                            ktok = sb(e23, "ktok", (128, NTT, 128), BF16)
                            ktb = bufs(3)
                            vtok = sb(e23, "vtok", (128, NTT, HV), BF16)
                            vtb = bufs(NTT)
                            sg = sb(e23, "sg", (128, NXT, HV), BF16)
                            sgb = bufs(NXT)
                            SBs = sb(e23, "SBs", (128, NXT, HV), BF16)
                            SBb = bufs(NXT)
                            SF = [sb(e23, "SF%d" % i, (128, HV), BF16) for i in range(2)]
                            SFb = bufs(2)
                            Sst = sb(e23, "Sst", (128, HV))
                            Sstb = Buf()
                            NSL = 3
                            NBIG = 2
                            zz = [sb(e23, "zz%d" % p_, (128, 256)) for p_ in range(NBIG)]
                            zzb = bufs(NBIG)
                            EE = [sb(e23, "EE%d" % p_, (128, 386)) for p_ in range(NBIG)]
                            EEb = bufs(NBIG)
                            E2 = [sb(e23, "E2_%d" % p_, (128, 256)) for p_ in range(NBIG)]
                            E2b = bufs(NBIG)
                            NDEC = 6
                            decs = sb(e23, "decs", (128, NDEC))
                            decb = bufs(NDEC)
                            kr = [sb(e23, "kr%d" % p_, (128, 128), BF16) for p_ in range(NSL)]
                            krb = bufs(NSL)
                            abh = sb(e23, "abh", (128, 256))
                            abhb = Buf()
                            qd = [sb(e23, "qd%d" % p_, (128, 2, 128), BF16) for p_ in range(NSL)]
                            qdb = bufs(NSL)
                            ki = [[sb(e23, "ki%d%d" % (p_, d_), (128, 128), BF16) for d_ in range(2)] for p_ in range(NSL)]
                            kib = [bufs(2) for _ in range(NSL)]
                            sT = [sb(e23, "sT%d" % p_, (128, 2, 128), BF16) for p_ in range(NSL)]
                            sTb = bufs(NSL)
                            otmp = [sb(e23, "otmp%d" % p_, (128, HV)) for p_ in range(NBIG)]
                            otb = bufs(NBIG)
                            og = [sb(e23, "og%d" % p_, (128, HV), BF16) for p_ in range(NBIG)]
                            ogb = bufs(NBIG)
                            ogT = [sb(e23, "ogT%d" % p_, (128, 2, 128), BF16) for p_ in range(NBIG)]
                            ogTb = bufs(NBIG)
                            st2 = [sb(e23, "st2_%d" % p_, (128, 2)) for p_ in range(NSL)]
                            st2b = bufs(NSL)
                            junk2 = sb(e23, "junk2", (128, HV), BF16)
                            vglock = Buf()
                            j2b = Buf()
                            nbk = [0]

                            def rot():
                                v_ = nbk[0]
                                nbk[0] = (v_ + 1) % 7
                                return v_

                            def kgrp(tt):
                                return 0 if tt < NCT else 1 + (tt - NCT) // 4

                            tokgroups = [(0, 256)] + [(256 + g * 512, 512) for g in range(4)]

                            wglb = Buf()

                            def load_whd(h_):
                                for (dst0, src0, n_) in ((0, C_Q + h_ * 128, 128), (128, C_K + h_ * 128, 128),
                                                         (256, C_V + h_ * 256, 256), (512, C_G + h_ * 256, 256)):
                                    S.dma("pool", whd[:, :, dst0:dst0 + n_],
                                          w_in_d[:, src0:src0 + n_].rearrange("(k p) n -> p k n", p=128), wsem, w=[whb])

                            def load_wgl(h_):
                                S.dma("pool", wgl[:], wgb_d[h_ * 256:(h_ + 1) * 256, :].rearrange("(c p) n -> p c n", p=128),
                                      wgsem, w=[wglb])

                            for h in range(NHEAD):
                                if h == 0:
                                    load_whd(0)
                                    load_wgl(0)
                                for g in range(4):
                                    bk = rot()
                                    for k in range(KD):
                                        S.op("pe", lambda e, k=k, g=g, bk=bk: e.matmul(
                                            P[bk][:, :], lhsT=whd[:, k, 0:128], rhs=hT[:, k, 256 + g * 512:768 + g * 512],
                                            start=(k == 0), stop=(k == KD - 1)),
                                            r=[whb] + hTb[2 + 4 * g:6 + 4 * g], w=[Pb[bk]])
                                    S.op("act", lambda e, g=g, bk=bk: e.activation(
                                        out=qT[:, g * 512:(g + 1) * 512], in_=P[bk][:, :], func=AF.Copy, scale=HK ** -0.5),
                                        r=[Pb[bk]], w=[qTb[g]])
                                for g, (t0, n_) in enumerate(tokgroups):
                                    bk = rot()
                                    for k in range(KD):
                                        S.op("pe", lambda e, k=k, bk=bk, t0=t0, n_=n_: e.matmul(
                                            P[bk][:, 0:n_], lhsT=whd[:, k, 128:256], rhs=hT[:, k, t0:t0 + n_],
                                            start=(k == 0), stop=(k == KD - 1)),
                                            r=[whb] + hTb[t0 // 128:(t0 + n_) // 128], w=[Pb[bk]])
                                    S.op("dve", lambda e, bk=bk, t0=t0, n_=n_: e.tensor_copy(
                                        out=kT[:, t0:t0 + n_], in_=P[bk][:, 0:n_]), r=[Pb[bk]], w=[kTb[g]])
                                    if h == 0:
                                        bk = rot()
                                        for k in range(KD):
                                            S.op("pe", lambda e, k=k, bk=bk, t0=t0, n_=n_: e.matmul(
                                                P[bk][0:32, 0:n_], lhsT=wlr[:, k, :], rhs=hT[:, k, t0:t0 + n_],
                                                start=(k == 0), stop=(k == KD - 1)),
                                                r=[cb] + hTb[t0 // 128:(t0 + n_) // 128], w=[Pb[bk]])
                                        S.op("act", lambda e, bk=bk, t0=t0, n_=n_: e.activation(
                                            out=lrT[:, t0:t0 + n_], in_=P[bk][0:32, 0:n_], func=AF.Copy),
                                            r=[Pb[bk]], w=[lrb])
                                if upto == 20 and h == 0:
                                    S.dead = True
                                for bi, (tt0, nt_) in enumerate(((0, 8), (8, 8), (16, 2))):
                                    for j in range(nt_):
                                        tt = tt0 + j
                                        S.op("pe", lambda e, j=j, tt=tt: e.transpose(
                                            PB[:, j * 128:(j + 1) * 128], kT[:, tt * 128:(tt + 1) * 128], identb[:]),
                                            r=[kTb[kgrp(tt)], cb], w=[PBb])
                                    S.op("act" if bi % 2 else "dve", (lambda e, tt0=tt0, nt_=nt_: e.activation(
                                        out=ktok[:, tt0:tt0 + nt_, :], in_=PB[:, 0:nt_ * 128].rearrange("p (t d) -> p t d", d=128),
                                        func=AF.Copy)) if bi % 2 else (lambda e, tt0=tt0, nt_=nt_: e.tensor_copy(
                                            out=ktok[:, tt0:tt0 + nt_, :],
                                            in_=PB[:, 0:nt_ * 128].rearrange("p (t d) -> p t d", d=128))),
                                        r=[PBb], w=[ktb[bi]])
                                if upto == 21 and h == 0:
                                    S.dead = True
                                for tt in range(NTT):
                                    bk = rot()
                                    n_ = 256 if tt < NCT else 512
                                    for k in range(KD):
                                        S.op("pe", lambda e, k=k, bk=bk, tt=tt, n_=n_: e.matmul(
                                            P[bk][:, 0:n_], lhsT=hT[:, k, tt * 128:(tt + 1) * 128], rhs=whd[:, k, 256:256 + n_],
                                            start=(k == 0), stop=(k == KD - 1)), r=[whb, hTb[tt]], w=[Pb[bk]])
                                    S.op("dve", lambda e, bk=bk, tt=tt: e.tensor_copy(out=vtok[:, tt, :], in_=P[bk][:, 0:256]),
                                         r=[Pb[bk]], w=[vtb[tt], vglock])
                                    if tt >= NCT:
                                        S.op("act", lambda e, bk=bk, tt=tt: e.activation(
                                            out=sg[:, tt - NCT, :], in_=P[bk][:, 256:512], func=AF.Silu),
                                            r=[Pb[bk]], w=[sgb[tt - NCT], vglock])

                                if h + 1 < NHEAD:
                                    load_whd(h + 1)
                                if upto == 22 and h == 0:
                                    S.dead = True
                                S.op("dve", lambda e: e.tensor_copy(out=abh[:, 0:128], in_=abfb[:, h * 128:(h + 1) * 128]),
                                     r=[abB], w=[abhb])
                                S.op("dve", lambda e: e.tensor_copy(out=abh[:, 128:256], in_=abbb[:, h * 128:(h + 1) * 128]),
                                     r=[abB], w=[abhb])

                                def la_stage(tt, dirs, ix):
                                    lo, hi = dirs[0] * 128, (dirs[-1] + 1) * 128
                                    for di in dirs:
                                        a2p = a2pf if di == 0 else a2pb
                                        S.op("pe", lambda e, di=di, a2p=a2p: e.matmul(
                                            P[0][:, di * 128:(di + 1) * 128], lhsT=lrT[:, tt * 128:(tt + 1) * 128],
                                            rhs=a2p[:, h * 128:(h + 1) * 128], start=True, stop=True),
                                            r=[lrb, cb], w=[Pb[0]])
                                    yield
                                    S.op("dve", lambda e: e.tensor_tensor(out=zz[ix % NBIG][:, lo:hi], in0=P[0][:, lo:hi],
                                                                          in1=abh[:, lo:hi], op=ALU.add),
                                         r=[Pb[0], abhb], w=[zzb[ix % NBIG]])
                                    yield
                                    S.op("act", lambda e: e.activation(out=zz[ix % NBIG][:, lo:hi], in_=zz[ix % NBIG][:, lo:hi],
                                                                       func=AF.Exp, scale=-1.0), r=[zzb[ix % NBIG]], w=[zzb[ix % NBIG]])
                                    S.op("act", lambda e: e.activation(out=zz[ix % NBIG][:, lo:hi], in_=zz[ix % NBIG][:, lo:hi],
                                                                       func=AF.Ln, bias=1.0), r=[zzb[ix % NBIG]], w=[zzb[ix % NBIG]])
                                    yield

                                def rem_tot(di, ix):
                                    trm = tri(1) if di == 0 else tri(3)
                                    sp_ = zz[ix % NBIG][:, di * 128:(di + 1) * 128]
                                    S.op("pe", lambda e: e.matmul(P[1][:, 256:384], lhsT=trm, rhs=sp_,
                                                                  start=True, stop=True), r=[zzb[ix % NBIG], cb], w=[Pb[1]])
                                    S.op("pe", lambda e: e.matmul(P[1][:, 384:386], lhsT=sp_, rhs=negcol,
                                                                  start=True, stop=True), r=[zzb[ix % NBIG], cb], w=[Pb[1]])

                                def state_post(tt, ix, dst, dstb, bk=6, c0=0):
                                    S.op("pe", lambda e: e.matmul(P[bk][:, c0:c0 + HV], lhsT=kr[ix % NSL][:], rhs=vtok[:, tt, :],
                                                                  start=True, stop=True), r=[krb[ix % NSL], vtb[tt]], w=[Pb[bk]])
                                    yield
                                    S.op("dve", lambda e: e.scalar_tensor_tensor(
                                        out=Sst[:], in0=Sst[:], scalar=decs[:, ix % NDEC:ix % NDEC + 1], op0=ALU.mult,
                                        in1=P[bk][:, c0:c0 + HV], op1=ALU.add), r=[Sstb, decb[ix % NDEC], Pb[bk]], w=[Sstb])
                                    if dst is not None:
                                        S.op("dve", lambda e: e.tensor_copy(out=dst, in_=Sst[:]), r=[Sstb], w=[dstb])

                                def st_body(di, tt, ix, dst, dstb):
                                    yield from la_stage(tt, (di,), ix)
                                    rem_tot(di, ix)
                                    yield
                                    S.op("act", lambda e: e.activation(out=EE[ix % NBIG][:, 256:386], in_=P[1][:, 256:386], func=AF.Exp),
                                         r=[Pb[1]], w=[EEb[ix % NBIG]])
                                    yield
                                    S.op("dve", lambda e: e.tensor_copy(out=decs[:, ix % NDEC:ix % NDEC + 1], in_=EE[ix % NBIG][:, 384:385]),
                                         r=[EEb[ix % NBIG]], w=[decb[ix % NDEC]])
                                    S.op("dve", lambda e: e.tensor_tensor(out=kr[ix % NSL][:], in0=ktok[:, tt, :],
                                                                          in1=EE[ix % NBIG][:, 256:384], op=ALU.mult),
                                         r=[ktb[tt // 8], EEb[ix % NBIG]], w=[krb[ix % NSL]])
                                    yield
                                    yield from state_post(tt, ix, dst, dstb)

                                def f_body(n, ix):
                                    tt = n + NCT
                                    cur = n % 2
                                    yield from la_stage(tt, (0, 1), ix)
                                    for di in (0, 1):
                                        tin = tri(0) if di == 0 else tri(2)
                                        S.op("pe", lambda e, di=di, tin=tin: e.matmul(
                                            P[1][:, di * 128:(di + 1) * 128], lhsT=zz[ix % NBIG][:, di * 128:(di + 1) * 128], rhs=tin,
                                            start=True, stop=True), r=[zzb[ix % NBIG], cb], w=[Pb[1]])
                                    rem_tot(0, ix)
                                    yield
                                    S.op("act", lambda e: e.activation(out=EE[ix % NBIG][:, 0:386], in_=P[1][:, 0:386], func=AF.Exp),
                                         r=[Pb[1]], w=[EEb[ix % NBIG]])
                                    S.op("act", lambda e: e.activation(out=E2[ix % NBIG][:, 0:256], in_=P[1][:, 0:256], func=AF.Exp,
                                                                       scale=-1.0), r=[Pb[1]], w=[E2b[ix % NBIG]])
                                    yield
                                    S.op("dve", lambda e: e.tensor_tensor(
                                        out=qd[ix % NSL][:, :, :],
                                        in0=qT[:, n * 128:(n + 1) * 128].unsqueeze(1).to_broadcast([128, 2, 128]),
                                        in1=EE[ix % NBIG][:, 0:256].rearrange("p (d t) -> p d t", t=128),
                                        op=ALU.mult), r=[qTb[n // 4], EEb[ix % NBIG]], w=[qdb[ix % NSL]])
                                    for di in (0, 1):
                                        S.op(POOLENG, lambda e, di=di: e.tensor_tensor(
                                            out=ki[ix % NSL][di][:], in0=kT[:, tt * 128:(tt + 1) * 128],
                                            in1=E2[ix % NBIG][:, di * 128:(di + 1) * 128],
                                            op=ALU.mult), r=[kTb[kgrp(tt)], E2b[ix % NBIG]], w=[kib[ix % NSL][di]])
                                    if n < NXT - 1:
                                        S.op("dve", lambda e: e.tensor_copy(out=decs[:, ix % NDEC:ix % NDEC + 1], in_=EE[ix % NBIG][:, 384:385]),
                                         r=[EEb[ix % NBIG]], w=[decb[ix % NDEC]])
                                    S.op("dve", lambda e: e.tensor_tensor(out=kr[ix % NSL][:], in0=ktok[:, tt, :],
                                                                              in1=EE[ix % NBIG][:, 256:384], op=ALU.mult),
                                             r=[ktb[tt // 8], EEb[ix % NBIG]], w=[krb[ix % NSL]])
                                    yield
                                    for di in (0, 1):
                                        S.op("pe", lambda e, di=di: e.matmul(
                                            P[2][:, di * 128:(di + 1) * 128], lhsT=ki[ix % NSL][di][:], rhs=qd[ix % NSL][:, di, :],
                                            start=True, stop=True), r=[kib[ix % NSL][di], qdb[ix % NSL]], w=[Pb[2]])
                                    yield
                                    S.op("dve", lambda e: e.tensor_tensor(
                                        out=sT[ix % NSL][:, :, :], in0=P[2][:, 0:256].rearrange("p (d t) -> p d t", t=128),
                                        in1=cst[:, 640:896].rearrange("p (d t) -> p d t", t=128),
                                        op=ALU.mult), r=[Pb[2], cb], w=[sTb[ix % NSL]])
                                    yield
                                    ops_o = ((sT[ix % NSL][:, 0, :], vtok[:, tt, :], [sTb[ix % NSL], vtb[tt]]),
                                             (qd[ix % NSL][:, 0, :], SF[cur][:], [qdb[ix % NSL], SFb[cur]]),
                                             (sT[ix % NSL][:, 1, :], vtok[:, tt, :], [sTb[ix % NSL], vtb[tt]]),
                                             (qd[ix % NSL][:, 1, :], SBs[:, n, :], [qdb[ix % NSL], SBb[n]]))
                                    ob = 3 if ix % 2 == 0 else 6
                                    for oi, (l_, r_, rb_) in enumerate(ops_o):
                                        S.op("pe", lambda e, l_=l_, r_=r_, oi=oi: e.matmul(
                                            P[ob][:, 0:HV], lhsT=l_, rhs=r_, start=(oi == 0), stop=(oi == 3)),
                                            r=rb_, w=[Pb[ob]])
                                    if n < NXT - 1:
                                        spost = state_post(tt, ix, SF[1 - cur][:], SFb[1 - cur], bk=ob, c0=256)
                                        next(spost)
                                    else:
                                        spost = iter(())
                                    yield
                                    S.op("act", lambda e: e.activation(out=junk2[:], in_=P[ob][:, 0:HV], func=AF.Square,
                                                                       accum_out=st2[ix % NSL][:, 0:1]),
                                         r=[Pb[ob]], w=[st2b[ix % NSL], j2b])
                                    S.op("act", lambda e: e.activation(out=st2[ix % NSL][:, 1:2], in_=st2[ix % NSL][:, 0:1], func=AF.Ln,
                                                                       bias=EPS, scale=1.0 / HV), r=[st2b[ix % NSL]], w=[st2b[ix % NSL]])
                                    S.op("act", lambda e: e.activation(out=st2[ix % NSL][:, 1:2], in_=st2[ix % NSL][:, 1:2], func=AF.Exp,
                                                                       scale=-0.5), r=[st2b[ix % NSL]], w=[st2b[ix % NSL]])
                                    next(spost, None)
                                    yield
                                    S.op("dve", lambda e: e.scalar_tensor_tensor(
                                        out=otmp[ix % NBIG][:], in0=P[ob][:, 0:HV], scalar=st2[ix % NSL][:, 1:2], op0=ALU.mult,
                                        in1=gnb[:], op1=ALU.mult), r=[Pb[ob], st2b[ix % NSL], cb], w=[otb[ix % NBIG]])
                                    S.op(POOLENG, lambda e: e.tensor_tensor(out=og[ix % NBIG][:], in0=otmp[ix % NBIG][:], in1=sg[:, n, :],
                                                                           op=ALU.mult), r=[otb[ix % NBIG], sgb[n]], w=[ogb[ix % NBIG]])
                                    yield
                                    for c in range(2):
                                        S.op("pe", lambda e, c=c: e.transpose(PB[:, c * 128:(c + 1) * 128],
                                                                              og[ix % NBIG][:, c * 128:(c + 1) * 128], identb[:]),
                                             r=[ogb[ix % NBIG], cb], w=[PBb])
                                    yield
                                    S.op("act", lambda e: e.activation(
                                        out=ogT[ix % NBIG][:, :, :], in_=PB[:, 0:256].rearrange("p (c t) -> p c t", t=128),
                                        func=AF.Copy), r=[PBb], w=[ogTb[ix % NBIG]])
                                    yield
                                    for hf in range(2):
                                        for c in range(2):
                                            S.op("pe", lambda e, hf=hf, c=c: e.matmul(
                                                P[4 + hf][:, :], lhsT=ogT[ix % NBIG][:, c, :], rhs=wgl[:, c, hf * 512:(hf + 1) * 512],
                                                start=(c == 0), stop=(c == 1)), r=[ogTb[ix % NBIG], wglb], w=[Pb[4 + hf]])
                                    yield
                                    for hf in range(2):
                                        dst = acc[:, n, hf * 512:(hf + 1) * 512]
                                        if h == 0:
                                            S.op("act", lambda e, hf=hf, dst=dst: e.activation(out=dst, in_=P[4 + hf][:, :],
                                                                                               func=AF.Copy),
                                                 r=[Pb[4 + hf]], w=[accb[n]])
                                        else:
                                            S.op("dve", lambda e, hf=hf, dst=dst: e.tensor_tensor(
                                                out=dst, in0=dst, in1=P[4 + hf][:, :], op=ALU.add),
                                                r=[Pb[4 + hf], accb[n]], w=[accb[n]])

                                S.op("dve", lambda e: e.memset(Sst[:], 0.0), w=[Sstb])
                                gens = [st_body(1, 1, 0, None, None), st_body(1, 0, 1, SBs[:, NXT - 1, :], SBb[NXT - 1])]
                                for n in range(NXT - 1, 0, -1):
                                    gens.append(st_body(1, n + NCT, len(gens), SBs[:, n - 1, :], SBb[n - 1]))
                                pipeline(gens)
                                if upto == 23 and h == 0:
                                    S.dead = True
                                S.op("dve", lambda e: e.memset(Sst[:], 0.0), w=[Sstb])
                                gens = [st_body(0, 0, 0, None, None), st_body(0, 1, 1, SF[0][:], SFb[0])]
                                for n in range(NXT):
                                    gens.append(f_body(n, len(gens)))
                                pipeline(gens)
                                if h + 1 < NHEAD:
                                    load_wgl(h + 1)
                            S.barrier()
                            if upto == 2:
                                S.dead = True
                        S.barrier()
                    h2T = sb(eb, "h2T", (128, NXT, KD, 128), BF16)
                    h2b = bufs(NXT)
                    cw = sb(eb, "cw", (128, NXT, 32))
                    cwb = bufs(NXT)

                    def bcast_row(dst, ci):
                        for hf in range(2):
                            S.op("pe", lambda e, hf=hf: e.matmul(
                                P[hf][:, :], lhsT=sel[0:NBC, b * 128:(b + 1) * 128], rhs=msb[:, ci, hf * 512:(hf + 1) * 512],
                                start=True, stop=True), r=[cb], w=[Pb[hf]])
                            S.op("act", lambda e, hf=hf: e.activation(out=dst[:, hf * 512:(hf + 1) * 512], in_=P[hf][:, :],
                                                                      func=AF.Copy), r=[Pb[hf]], w=[cb])

                    with contextlib.ExitStack() as e4:
                        wu = sb(e4, "wu", (128, KD, 512), BF16)
                        wgt = sb(e4, "wgt", (128, KD, 2048), BF16)
                        wpb = sb(e4, "wpb", (128, 4, D), BF16)
                        w4b = Buf()
                        S.dma("pool", wu[:], w_in_d[:, C_POOL:C_POOL + 512].rearrange("(k p) n -> p k n", p=128), wsem, w=[w4b])
                        for j in range(4):
                            S.dma("pool", wgt[:, :, j * 512:(j + 1) * 512],
                                  w_in_d[:, C_GATES + j * 512:C_GATES + (j + 1) * 512].rearrange("(k p) n -> p k n", p=128),
                                  wsem, w=[w4b])
                        S.dma("pool", wpb[:], wpb_d.rearrange("(g p) n -> p g n", p=128), wsem, w=[w4b])
                        xin = [sb(e4, "xin4_%d" % i, (128, D)) for i in range(2)]
                        xib = bufs(2)
                        xs = sb(e4, "xs4", (128, D))
                        xsb = Buf()
                        junk = sb(e4, "junk4", (128, D), BF16)
                        jb = Buf()
                        st = [sb(e4, "st4_%d" % i, (128, 2)) for i in range(2)]
                        stb = bufs(2)
                        hTt = [sb(e4, "hTt%d" % i, (128, KD, 128), BF16) for i in range(2)]
                        hTtb = bufs(2)
                        u_sb = sb(e4, "u_sb", (128, 512), BF16)
                        ub = Buf()
                        pT = sb(e4, "pT", (128, 512), BF16)
                        pTb = Buf()
                        y1T = sb(e4, "y1T", (128, 4, 128), BF16)
                        y1b = Buf()
                        sgt = [sb(e4, "sgt%d" % i, (128, 512)) for i in range(2)]
                        sgtb = bufs(2)
                        t1 = sb(e4, "t1", (128, D))
                        t1b = bufs(2)
                        tmp = [sb(e4, "tmp4_%d" % i, (128, 512)) for i in range(2)]
                        tmpb = bufs(2)

                        def load_x4(n):
                            S.dma("sp", xin[n % 2][:], x_d[b, n * 128:(n + 1) * 128, :], xid[n % 2], w=[xib[n % 2]])

                        def p4a_body(n):
                            if n + 1 < NXT:
                                load_x4(n + 1)
                            i = n % 2
                            par = n % 2
                            S.op("act", lambda e: e.activation(out=junk[:], in_=xin[i][:], func=AF.Square,
                                                               accum_out=st[par][:, 0:1]), r=[xib[i]], w=[stb[par], jb])
                            S.op("act", lambda e: e.activation(out=st[par][:, 1:2], in_=st[par][:, 0:1], func=AF.Sqrt,
                                                               bias=EPS, scale=1.0 / D), r=[stb[par]], w=[stb[par]])
                            S.op("dve", lambda e: e.reciprocal(out=st[par][:, 1:2], in_=st[par][:, 1:2]),
                                 r=[stb[par]], w=[stb[par]])
                            S.op("act", lambda e: e.activation(out=xs[:], in_=xin[i][:], func=AF.Copy, scale=st[par][:, 1:2]),
                                 r=[stb[par], xib[i]], w=[xsb])
                            for k in range(KD):
                                S.op("pe", lambda e, k=k: e.transpose(P[k // 4][:, (k % 4) * 128:(k % 4 + 1) * 128],
                                                                      xs[:, k * 128:(k + 1) * 128], ident),
                                     r=[xsb, cb], w=[Pb[k // 4]])
                            for k in range(KD):
                                src = P[k // 4][:, (k % 4) * 128:(k % 4 + 1) * 128]
                                dst = hTt[par][:, k, :]
                                if k // 4 == 0:
                                    S.op("act", lambda e, src=src, dst=dst, k=k: e.activation(
                                        out=dst, in_=src, func=AF.Identity, scale=A1[:, b, k:k + 1], bias=modT[:, b, k:k + 1]),
                                        r=[Pb[k // 4], cb], w=[hTtb[par]])
                                else:
                                    S.op("dve", lambda e, src=src, dst=dst, k=k: e.tensor_scalar(
                                        out=dst, in0=src, scalar1=A1[:, b, k:k + 1], scalar2=modT[:, b, k:k + 1],
                                        op0=ALU.mult, op1=ALU.add), r=[Pb[k // 4], cb], w=[hTtb[par]])
                            yield
                            for k in range(KD):
                                S.op("pe", lambda e, k=k: e.matmul(P[2][:, :], lhsT=hTt[par][:, k, :], rhs=wu[:, k, :],
                                                                   start=(k == 0), stop=(k == KD - 1)),
                                     r=[hTtb[par], w4b], w=[Pb[2]])
                            S.op("dve", lambda e: e.tensor_copy(out=u_sb[:], in_=P[2][:, :]), r=[Pb[2]], w=[ub])
                            for g in range(4):
                                S.op("pe", lambda e, g=g: e.matmul(P[3][:, g * 128:(g + 1) * 128],
                                                                   lhsT=u_sb[:, g * 128:(g + 1) * 128],
                                                                   rhs=poolP[:, g * 128:(g + 1) * 128], start=True, stop=True),
                                     r=[ub, cb], w=[Pb[3]])
                            S.op("act", lambda e: e.activation(out=pT[:], in_=P[3][:, :], func=AF.Copy), r=[Pb[3]], w=[pTb])
                            for g in range(4):
                                S.op("pe", lambda e, g=g: e.matmul(P[2][:, g * 128:(g + 1) * 128], lhsT=pwt[:, g, :],
                                                                   rhs=pT[:, g * 128:(g + 1) * 128], start=True, stop=True),
                                     r=[pTb, cb], w=[Pb[2]])
                            for g in range(4):
                                S.op("act", lambda e, g=g: e.activation(out=y1T[:, g, :], in_=P[2][:, g * 128:(g + 1) * 128],
                                                                        func=AF.Copy, scale=vT[:, 64 + g:65 + g]),
                                     r=[Pb[2], cb], w=[y1b])
                            for hf in range(2):
                                for g in range(4):
                                    S.op("pe", lambda e, hf=hf, g=g: e.matmul(
                                        P[4 + hf][:, :], lhsT=y1T[:, g, :], rhs=wpb[:, g, hf * 512:(hf + 1) * 512],
                                        start=(g == 0), stop=(g == 3)), r=[y1b, w4b], w=[Pb[4 + hf]])
                            yield
                            for j in range(4):
                                bk = 6 if j % 2 == 0 else 3
                                hf = j % 2
                                for k in range(KD):
                                    S.op("pe", lambda e, k=k, j=j, bk=bk: e.matmul(
                                        P[bk][:, :], lhsT=hTt[par][:, k, :], rhs=wgt[:, k, j * 512:(j + 1) * 512],
                                        start=(k == 0), stop=(k == KD - 1)), r=[hTtb[par], w4b], w=[Pb[bk]])
                                S.op("act", lambda e, bk=bk, hf=hf: e.activation(out=sgt[hf][:], in_=P[bk][:, :],
                                                                                 func=AF.Sigmoid),
                                     r=[Pb[bk]], w=[sgtb[hf]])
                                if j < 2:
                                    S.op("dve", lambda e, hf=hf: e.tensor_tensor(
                                        out=t1[:, hf * 512:(hf + 1) * 512], in0=sgt[hf][:], in1=P[4 + hf][:, :], op=ALU.mult),
                                        r=[sgtb[hf], Pb[4 + hf]], w=[t1b[hf]])
                                else:
                                    S.op("dve", lambda e, hf=hf: e.tensor_tensor(
                                        out=tmp[hf][:], in0=sgt[hf][:], in1=acc[:, n, hf * 512:(hf + 1) * 512], op=ALU.mult),
                                        r=[sgtb[hf], accb[n]], w=[tmpb[hf]])
                                    S.op("dve", lambda e, hf=hf: e.tensor_tensor(
                                        out=h2T[:, n, hf * 4:(hf + 1) * 4, :], in0=tmp[hf][:].rearrange("p (k t) -> p k t", t=128),
                                        in1=t1[:, hf * 512:(hf + 1) * 512].rearrange("p (k t) -> p k t", t=128), op=ALU.add),
                                        r=[tmpb[hf], t1b[hf]], w=[h2b[n]])

                        load_x4(0)
                        pipeline(p4a_body(n) for n in range(NXT))
                        S.barrier()
                        if upto == 3:
                            S.dead = True

                    with contextlib.ExitStack() as e4:
                        wo = sb(e4, "wo", (128, KD, D), BF16)
                        w4b = Buf()
                        S.dma("pool", wo[:], wo_d.rearrange("(k p) n -> p k n", p=128), wsem, w=[w4b])
                        g1b = sb(e4, "g1b", (128, D))
                        bcast_row(g1b, 0)
                        xin = [sb(e4, "xin5_%d" % i, (128, D)) for i in range(2)]
                        xib = bufs(2)
                        mTt = [sb(e4, "mTt%d" % i, (128, KD, 128), BF16) for i in range(2)]
                        mTb = bufs(2)
                        tmp2 = sb(e4, "tmp2", (128, D))
                        tmp2b = bufs(2)
                        xs = sb(e4, "xs5", (128, D))
                        xsb = Buf()
                        junk = sb(e4, "junk5", (128, D), BF16)
                        jb = Buf()
                        st = [sb(e4, "st5_%d" % i, (128, 2)) for i in range(2)]
                        stb = bufs(2)
                        lgall = sb(e4, "lgall", (128, NXT, 36))
                        lgb = Buf()
                        rs_ = sb(e4, "rs_", (128, 10, NXT))
                        rg_ = sb(e4, "rg_", (128, 3, NXT, 4))
                        re_ = sb(e4, "re_", (128, 5, NXT, 32))
                        rtb_ = Buf()

                        def load_x5(n):
                            S.dma("sp", xin[n % 2][:], x_d[b, n * 128:(n + 1) * 128, :], xid[n % 2], w=[xib[n % 2]])

                        def p4b_body(n):
                            i = n % 2
                            par = n % 2
                            mv = h2T[:, n, :, :]
                            for k in range(KD):
                                S.op("pe", lambda e, k=k: e.transpose(PB[:, k * 128:(k + 1) * 128], mv[:, k, :], identb[:]),
                                     r=[h2b[n], cb], w=[PBb])
                            yield
                            S.op("act", lambda e: e.activation(out=mTt[par][:, :, :],
                                                               in_=PB[:, :].rearrange("p (k t) -> p k t", t=128), func=AF.Copy),
                                 r=[PBb], w=[mTb[par]])
                            yield
                            if n + 1 < NXT:
                                load_x5(n + 1)
                            for hf in range(2):
                                for k in range(KD):
                                    S.op("pe", lambda e, hf=hf, k=k: e.matmul(
                                        P[hf][:, :], lhsT=mTt[par][:, k, :], rhs=wo[:, k, hf * 512:(hf + 1) * 512],
                                        start=(k == 0), stop=(k == KD - 1)), r=[mTb[par], w4b], w=[Pb[hf]])
                            yield
                            for hf in range(2):
                                sl = slice(hf * 512, (hf + 1) * 512)
                                S.op("dve", lambda e, hf=hf, sl=sl: e.tensor_tensor(out=tmp2[:, sl], in0=P[hf][:, :],
                                                                                    in1=g1b[:, sl], op=ALU.mult),
                                     r=[Pb[hf], cb], w=[tmp2b[hf]])
                                S.op("dve", lambda e, sl=sl: e.tensor_tensor(out=acc[:, n, sl], in0=tmp2[:, sl],
                                                                              in1=xin[i][:, sl], op=ALU.add),
                                     r=[tmp2b[hf], xib[i]], w=[accb[n]])
                            yield
                            S.op("act", lambda e: e.activation(out=junk[:], in_=acc[:, n, :], func=AF.Square,
                                                               accum_out=st[par][:, 0:1]), r=[accb[n]], w=[stb[par], jb])
                            S.op("act", lambda e: e.activation(out=st[par][:, 1:2], in_=st[par][:, 0:1], func=AF.Ln,
                                                               bias=EPS, scale=1.0 / D), r=[stb[par]], w=[stb[par]])
                            S.op("act", lambda e: e.activation(out=st[par][:, 1:2], in_=st[par][:, 1:2], func=AF.Exp,
                                                               scale=-0.5), r=[stb[par]], w=[stb[par]])
                            S.op("act", lambda e: e.activation(out=xs[:], in_=acc[:, n, :], func=AF.Copy,
                                                               scale=st[par][:, 1:2]), r=[stb[par], accb[n]], w=[xsb])
                            yield
                            for k in range(KD):
                                S.op("pe", lambda e, k=k: e.transpose(P[2 + k // 4][:, (k % 4) * 128:(k % 4 + 1) * 128],
                                                                      xs[:, k * 128:(k + 1) * 128], ident),
                                     r=[xsb, cb], w=[Pb[2 + k // 4]])
                            yield
                            for k in range(KD):
                                src = P[2 + k // 4][:, (k % 4) * 128:(k % 4 + 1) * 128]
                                dst = h2T[:, n, k, :]
                                if k // 4 == 0:
                                    S.op("act", lambda e, src=src, dst=dst, k=k: e.activation(
                                        out=dst, in_=src, func=AF.Identity, scale=A2[:, b, k:k + 1],
                                        bias=modT[:, b, 24 + k:25 + k]), r=[Pb[2 + k // 4], cb], w=[h2b[n]])
                                else:
                                    S.op("dve", lambda e, src=src, dst=dst, k=k: e.tensor_scalar(
                                        out=dst, in0=src, scalar1=A2[:, b, k:k + 1], scalar2=modT[:, b, 24 + k:25 + k],
                                        op0=ALU.mult, op1=ALU.add), r=[Pb[2 + k // 4], cb], w=[h2b[n]])
                            yield
                            for k in range(KD):
                                S.op("pe", lambda e, k=k: e.matmul(P[4][:, 0:36], lhsT=h2T[:, n, k, :], rhs=rw[:, k, :],
                                                                   start=(k == 0), stop=(k == KD - 1)),
                                     r=[h2b[n], cb], w=[Pb[4]])
                            yield
                            S.op("dve", lambda e: e.tensor_tensor(out=lgall[:, n, :], in0=P[4][:, 0:36], in1=rbb[:], op=ALU.add),
                                 r=[Pb[4], cb], w=[lgb])

                        load_x5(0)
                        pipeline(p4b_body(n) for n in range(NXT))
                        T_ = NXT
                        X = mybir.AxisListType.X
                        gl = lgall[:, :, 0:4]
                        el4 = lgall[:, :, 4:36].rearrange("p t (g e) -> p t g e", e=8)
                        gmax, ngs, pg, m1, m2, dd, e2, den, w1_, w2_ = (rs_[:, q, :] for q in range(10))
                        gm, pen, ge = (rg_[:, q, :, :] for q in range(3))
                        em, oh1, em2, oh2, cwt = (re_[:, q, :, :] for q in range(5))

                        def bc(ap2, width):
                            return ap2.unsqueeze(2).to_broadcast([128, T_, width])

                        def dv(fn):
                            S.op("dve", fn, r=[lgb, rtb_], w=[rtb_])

                        dv(lambda e: e.tensor_reduce(out=gmax, in_=gl, axis=X, op=ALU.max))
                        dv(lambda e: e.tensor_tensor(out=gm, in0=gl, in1=bc(gmax, 4), op=ALU.is_equal))
                        dv(lambda e: e.tensor_tensor(out=ge, in0=gl, in1=bc(gmax, 4), op=ALU.subtract))
                        S.op("act", lambda e: e.activation(out=ge, in_=ge, func=AF.Exp), r=[rtb_], w=[rtb_])
                        dv(lambda e: e.tensor_reduce(out=ngs, in_=ge, axis=X, op=ALU.add))
                        dv(lambda e: e.reciprocal(out=pg, in_=ngs))
                        dv(lambda e: e.tensor_scalar(out=pen, in0=gm, scalar1=-1.0, scalar2=1e30, op0=ALU.add, op1=ALU.mult))
                        em4 = em.rearrange("p t (g e) -> p t g e", e=8)
                        dv(lambda e: e.tensor_tensor(out=em4, in0=el4,
                                                     in1=pen.unsqueeze(3).to_broadcast([128, T_, 4, 8]), op=ALU.add))
                        dv(lambda e: e.tensor_reduce(out=m1, in_=em, axis=X, op=ALU.max))
                        dv(lambda e: e.tensor_tensor(out=oh1, in0=em, in1=bc(m1, 32), op=ALU.is_equal))
                        dv(lambda e: e.scalar_tensor_tensor(out=em2.rearrange("p t e -> p (t e)"),
                                                            in0=oh1.rearrange("p t e -> p (t e)"), scalar=-1e30, op0=ALU.mult,
                                                            in1=em.rearrange("p t e -> p (t e)"), op1=ALU.add))
                        dv(lambda e: e.tensor_reduce(out=m2, in_=em2, axis=X, op=ALU.max))
                        dv(lambda e: e.tensor_tensor(out=oh2, in0=em2, in1=bc(m2, 32), op=ALU.is_equal))
                        dv(lambda e: e.tensor_tensor(out=dd, in0=m2, in1=m1, op=ALU.subtract))
                        S.op("act", lambda e: e.activation(out=e2, in_=dd, func=AF.Exp), r=[rtb_], w=[rtb_])
                        dv(lambda e: e.tensor_scalar(out=den, in0=e2, scalar1=1.0, scalar2=None, op0=ALU.add))
                        dv(lambda e: e.reciprocal(out=den, in_=den))
                        dv(lambda e: e.tensor_tensor(out=w1_, in0=den, in1=pg, op=ALU.mult))
                        dv(lambda e: e.tensor_tensor(out=w2_, in0=w1_, in1=e2, op=ALU.mult))
                        dv(lambda e: e.tensor_tensor(out=cwt, in0=oh1, in1=bc(w1_, 32), op=ALU.mult))
                        dv(lambda e: e.tensor_tensor(out=oh2, in0=oh2, in1=bc(w2_, 32), op=ALU.mult))
                        S.op("dve", lambda e: e.tensor_tensor(out=cw[:, :, :], in0=cwt, in1=oh2, op=ALU.add),
                             r=[rtb_], w=cwb)
                        if debug:
                            for n in range(2):
                                dump(acc[:, n, :], D, accb)
                            dump(cw[:, 0, :], 32, cwb)
                            dump(cw[:, 1, :], 32, cwb)
                        S.barrier()
                        if upto == 4:
                            S.dead = True

                    with contextlib.ExitStack() as e5:
                        g2b = sb(e5, "g2b", (128, D))
                        bcast_row(g2b, 1)
                        w1b = [sb(e5, "w1b%d" % i, (128, KD, DE), BF16) for i in range(2)]
                        w3b = [sb(e5, "w3b%d" % i, (128, KD, DE), BF16) for i in range(2)]
                        w2s = sb(e5, "w2s", (128, 4, D))
                        w2b = [sb(e5, "w2b%d" % i, (128, 4, D), BF16) for i in range(2)]
                        wb13 = bufs(2)
                        w2sb = Buf()
                        w2bb = bufs(2)
                        sa = [sb(e5, "sa%d" % i, (128, 512)) for i in range(2)]
                        sab = bufs(2)
                        hid = [sb(e5, "hid%d" % i, (128, 4, 512), BF16) for i in range(2)]
                        hidb = bufs(2)

                        def load_w(e_):
                            sl_ = e_ % 2
                            S.dma("pool", w1b[sl_][:], w1_d[e_].rearrange("(k p) f -> p k f", p=128), wm13[sl_], w=[wb13[sl_]])
                            S.dma("pool", w3b[sl_][:], w3_d[e_].rearrange("(k p) f -> p k f", p=128), wm13[sl_], w=[wb13[sl_]])
                            S.dma("sp", w2s[:], w2_d[e_].rearrange("(c p) n -> p c n", p=128), w2sem, w=[w2sb])
                            for c in range(4):
                                S.op("dve", lambda e, c=c, sl_=sl_: e.tensor_tensor(out=w2b[sl_][:, c, :], in0=w2s[:, c, :],
                                                                                     in1=g2b[:], op=ALU.mult),
                                     r=[w2sb, cb], w=[w2bb[sl_]])

                        items = [(e_, tg) for e_ in range(NEXP) for tg in range(4)]

                        def emit_ab(idx):
                            e_, tg = items[idx]
                            sl_ = e_ % 2
                            hp = idx % 2
                            for f in range(4):
                                ba, bb_ = (0, 1) if f % 2 == 0 else (2, 3)
                                sp_ = f % 2
                                for (wsrc, bk) in ((w1b, ba), (w3b, bb_)):
                                    for k in range(KD):
                                        S.op("pe", lambda e, wsrc=wsrc, bk=bk, k=k, f=f: e.matmul(
                                            P[bk][:, :].rearrange("p (t d) -> p t d", d=128),
                                            lhsT=wsrc[sl_][:, k, f * 128:(f + 1) * 128],
                                            rhs=h2T[:, tg * 4:(tg + 1) * 4, k, :], start=(k == 0), stop=(k == KD - 1)),
                                            r=[wb13[sl_]] + h2b[tg * 4:(tg + 1) * 4], w=[Pb[bk]])
                                S.op("act", lambda e, ba=ba, sp_=sp_: e.activation(out=sa[sp_][:], in_=P[ba][:, :],
                                                                                   func=AF.Silu),
                                     r=[Pb[ba]], w=[sab[sp_]])
                                S.op("dve", lambda e, bb_=bb_, sp_=sp_, f=f: e.tensor_tensor(
                                    out=hid[hp][:, f, :], in0=sa[sp_][:], in1=P[bb_][:, :], op=ALU.mult),
                                    r=[sab[sp_], Pb[bb_]], w=[hidb[hp]])

                        def emit_w2(idx):
                            e_, tg = items[idx]
                            sl_ = e_ % 2
                            hp = idx % 2
                            for j in range(4):
                                n = tg * 4 + j
                                for hf in range(2):
                                    bk = 4 + (j * 2 + hf) % 3
                                    for f in range(4):
                                        S.op("pe", lambda e, bk=bk, f=f, j=j, hf=hf: e.matmul(
                                            P[bk][:, :], lhsT=hid[hp][:, f, j * 128:(j + 1) * 128],
                                            rhs=w2b[sl_][:, f, hf * 512:(hf + 1) * 512], start=(f == 0), stop=(f == 3)),
                                            r=[hidb[hp], w2bb[sl_]], w=[Pb[bk]])
                                    dst = acc[:, n, hf * 512:(hf + 1) * 512]
                                    S.op("dve", lambda e, bk=bk, dst=dst, n=n: e.scalar_tensor_tensor(
                                        out=dst, in0=P[bk][:, :], scalar=cw[:, n, e_:e_ + 1], op0=ALU.mult, in1=dst,
                                        op1=ALU.add), r=[Pb[bk], cwb[n], accb[n]], w=[accb[n]])

                        load_w(0)
                        if NEXP > 1:
                            load_w(1)
                        emit_ab(0)
                        for idx in range(len(items)):
                            if idx + 1 < len(items):
                                emit_ab(idx + 1)
                            emit_w2(idx)
                            e_, tg = items[idx]
                            if tg == 3 and e_ + 2 < NEXP:
                                load_w(e_ + 2)
                        S.barrier()
                        if upto == 5:
                            S.dead = True

                    with contextlib.ExitStack() as e6:
                        fgb = sb(e6, "fgb", (128, D))
                        fb = Buf()
                        S.dma("sp", fgb[:], fg_d.rearrange("(o n) -> o n", o=1).to_broadcast([128, D]), dsl, w=[fb])
                        ot = [sb(e6, "ot%d" % i, (128, D)) for i in range(2)]
                        otb_ = bufs(2)
                        junk = sb(e6, "junk6", (128, D), BF16)
                        jb = Buf()
                        st = [sb(e6, "st6_%d" % i, (128, 2)) for i in range(2)]
                        stb = bufs(2)
                        for n in range(NXT):
                            par = n % 2
                            S.op("act", lambda e: e.activation(out=junk[:], in_=acc[:, n, :], func=AF.Square,
                                                               accum_out=st[par][:, 0:1]), r=[accb[n]], w=[stb[par], jb])
                            S.op("act", lambda e: e.activation(out=st[par][:, 1:2], in_=st[par][:, 0:1], func=AF.Sqrt,
                                                               bias=EPS, scale=1.0 / D), r=[stb[par]], w=[stb[par]])
                            S.op("dve", lambda e: e.reciprocal(out=st[par][:, 1:2], in_=st[par][:, 1:2]),
                                 r=[stb[par]], w=[stb[par]])
                            S.op("dve", lambda e: e.scalar_tensor_tensor(out=ot[par][:], in0=acc[:, n, :],
                                                                         scalar=st[par][:, 1:2], op0=ALU.mult, in1=fgb[:],
                                                                         op1=ALU.mult), r=[accb[n], stb[par], fb], w=[otb_[par]])
                            S.dma("sp", out_d[b, n * 128:(n + 1) * 128, :], ot[par][:], osem[par], r=[otb_[par]])
                        S.barrier()
                        if upto == 6:
                            S.dead = True
                    S.barrier()
        except _Stop:
            pass
        S.dead = False
        S.barrier()
    return nc


_CONSTS = None


def _consts():
    global _CONSTS
    if _CONSTS is None:
        j = np.arange(128)[:, None]
        i = np.arange(128)[None, :]
        cst = np.zeros((128, 898), np.float32)
        cst[:, 0:128] = np.eye(128, dtype=np.float32)
        s = -1.0 / 16.0
        cst[:, 128:256] = (j <= i) * s
        cst[:, 256:384] = (j > i) * s
        cst[:, 384:512] = (j >= i) * s
        cst[:, 512:640] = (j < i) * s
        cst[:, 640:768] = (j <= i)
        cst[:, 768:896] = (j >= i)
        cst[:, 896:898] = s
        poolP = np.zeros((128, 4, 128), np.float32)
        for gi, w in enumerate((2, 4, 8, 16)):
            for t in range(128):
                r0 = (t // 64) * 64
                lo = max(t - w // 2, r0)
                hi = min(t + w // 2, r0 + 64)
                poolP[lo:hi, gi, t] = 1.0 / (hi - lo)
                poolP[t, gi, t] -= 1.0
        sel = np.zeros((8, 8, 128), np.float32)
        for r in range(8):
            sel[r, r, :] = 1.0
        _CONSTS = (cst, poolP.reshape(128, 512), sel.reshape(8, 1024))
    return _CONSTS


_PER_LAYER = ("w_mod", "b_mod", "norm1_g", "norm2_g", "w_in", "gla_a2_f", "gla_ab_f", "gla_a2_b", "gla_ab_b",
              "gla_onorm_g", "pool_w", "pool_scale", "w_pool_br", "w_gla_br", "w_o", "router_grp_w", "router_grp_b",
              "router_exp_w", "router_exp_b", "moe_w1", "moe_w3", "moe_w2")


def kernel(**inputs):
    NB = 4
    x = np.asarray(inputs["x"], np.float32)
    c = np.asarray(inputs["c"], np.float32)
    ctx = np.asarray(inputs["ctx"], np.float32)
    c_ctx = np.asarray(inputs["c_ctx"], np.float32)
    cst, poolP, sel = _consts()
    shared = {k: np.ascontiguousarray(np.asarray(inputs[k], np.float32)[0]) for k in _PER_LAYER}
    shared["final_norm_g"] = np.ascontiguousarray(np.asarray(inputs["final_norm_g"], np.float32))
    shared["cst"] = cst
    shared["poolP"] = poolP
    shared["sel"] = sel
    in_maps = []
    for core in range(NCORES):
        b0 = core * NB
        m = dict(shared)
        m["x"] = np.ascontiguousarray(x[b0:b0 + NB])
        m["ctx"] = np.ascontiguousarray(ctx[b0:b0 + NB])
        m["cT"] = np.ascontiguousarray(np.concatenate([c[b0:b0 + NB], c_ctx[None, :]], axis=0).T)
        in_maps.append(m)
    nc = build_program(NB=NB)
    res = run_bass_kernel_spmd(nc, in_maps, core_ids=list(range(NCORES)))
    return np.concatenate([np.asarray(r["out"], np.float32) for r in res.results], axis=0)
```

```python
import contextlib
import os
import numpy as np
import concourse.bass as bass
import concourse.mybir as mybir
from concourse.bass_utils import run_bass_kernel_spmd

F32 = mybir.dt.float32
BF16 = mybir.dt.bfloat16
AF = mybir.ActivationFunctionType
ALU = mybir.AluOpType

D = 1024
SEQ = 2048
CTXL = 256
KD = 8
NXT = SEQ // 128
NCT = CTXL // 128
NTT = NXT + NCT
TOK = NTT * 128
D_IN = 5664
C_POOL, C_Q, C_K, C_V, C_G, C_GATES, C_LR = 0, 512, 1024, 1536, 2560, 3584, 5632
NHEAD = 4
HK = 128
HV = 256
NEXP_FULL = 32
DE = 512
EPS = 1e-6
NCORES = 8
STRICT_SAME = not os.environ.get("K_NOSTRICT")
POOLENG = "dve" if os.environ.get("K_NOPOOL") else "pool"


class Buf:
    __slots__ = ("lw", "rd", "excl")

    def __init__(self, excl=False):
        self.lw = None
        self.rd = {}
        self.excl = excl


def bufs(n):
    return [Buf() for _ in range(n)]


class DSem:
    def __init__(self, sem):
        self.sem = sem
        self.count = 0
        self.q = None
        self.maxw = 0


class Sched:
    def __init__(self, nc, es):
        self.nc = nc
        self.es = es
        self.E = {}
        for name, eng in (("pe", nc.tensor), ("act", nc.scalar), ("dve", nc.vector),
                          ("pool", nc.gpsimd), ("sp", nc.sync)):
            sem = es.enter_context(nc.semaphore("s_" + name))
            self.E[name] = dict(eng=eng, sem=sem, count=0, waited={})
        self.dsems = []
        self.nds = 0
        self.dsmap = {}
        self.dead = False

    def new_dsem(self):
        self.nds += 1
        ds = DSem(self.es.enter_context(self.nc.semaphore("d%d" % self.nds)))
        self.dsems.append(ds)
        self.dsmap[id(ds.sem)] = ds
        return ds

    def _wait(self, en, toks):
        E = self.E[en]
        for (sem, val, owner) in toks:
            if owner == en and (en == "pe" or not STRICT_SAME):
                continue
            k = id(sem)
            if k in self.dsmap and val > self.dsmap[k].maxw:
                self.dsmap[k].maxw = val
            if E["waited"].get(k, 0) >= val:
                continue
            E["eng"].wait_ge(sem, val)
            E["waited"][k] = val

    @staticmethod
    def _deps(r, w, en=None):
        toks = []
        for b in r:
            if b.lw:
                toks.append(b.lw)
            if b.excl:
                toks.extend(t for e_, t in b.rd.items() if e_ != en)
        for b in w:
            if b.lw:
                toks.append(b.lw)
            toks.extend(b.rd.values())
        return toks

    def op(self, en, fn, r=(), w=()):
        if self.dead:
            return
        self._wait(en, self._deps(r, w, en))
        E = self.E[en]
        ins = fn(E["eng"])
        E["count"] += 1
        ins.then_inc(E["sem"], 1)
        tok = (E["sem"], E["count"], en)
        for b in w:
            b.lw = tok
            b.rd = {}
        for b in r:
            b.rd[en] = tok

    def dma(self, qn, out, in_, ds, r=(), w=()):
        if self.dead:
            return
        self._wait(qn, self._deps(r, w))
        E = self.E[qn]
        assert ds.q in (None, qn), "DMA semaphore shared between queues"
        ds.q = qn
        if ds.maxw > 0:
            self._wait(qn, [(ds.sem, ds.maxw, None)])
        ins = E["eng"].dma_start(out=out, in_=in_)
        ds.count += 16
        ins.then_inc(ds.sem, 16)
        tok = (ds.sem, ds.count, None)
        for b in w:
            b.lw = tok
            b.rd = {}
        for b in r:
            b.rd["dma%d" % id(ds)] = tok

    def barrier(self):
        if self.dead:
            return
        for en, E in self.E.items():
            toks = []
            for en2, E2 in self.E.items():
                if en2 != en and E2["count"] > 0:
                    toks.append((E2["sem"], E2["count"], en2))
            for ds in self.dsems:
                if ds.count > 0:
                    toks.append((ds.sem, ds.count, None))
            self._wait(en, toks)


def pipeline(gens):
    it = iter(gens)
    active = []
    while True:
        g = next(it, None)
        if g is not None:
            active.append(g)
        if not active:
            break
        for g_ in list(active):
            try:
                next(g_)
            except StopIteration:
                active.remove(g_)


class _Stop(Exception):
    pass


def build_program(NB=4, NEXP=NEXP_FULL, debug=None, upto=99):
    nc = bass.Bass("TRN2", target_bir_lowering=False)

    def din(name, shape):
        return nc.dram_tensor(name, list(shape), F32, kind="ExternalInput").ap()

    x_d = din("x", (NB, SEQ, D))
    ctx_d = din("ctx", (NB, CTXL, D))
    cT_d = din("cT", (D, NB + 1))
    w_mod_d = din("w_mod", (D, 6 * D))
    b_mod_d = din("b_mod", (6 * D,))
    n1g_d = din("norm1_g", (D,))
    n2g_d = din("norm2_g", (D,))
    w_in_d = din("w_in", (D, D_IN))
    a2f_d = din("gla_a2_f", (16, 512))
    abf_d = din("gla_ab_f", (512,))
    a2b_d = din("gla_a2_b", (16, 512))
    abb_d = din("gla_ab_b", (512,))
    gn_d = din("gla_onorm_g", (HV,))
    pw_d = din("pool_w", (4, 128, 128))
    psc_d = din("pool_scale", (512,))
    wpb_d = din("w_pool_br", (512, D))
    wgb_d = din("w_gla_br", (D, D))
    wo_d = din("w_o", (D, D))
    rgw_d = din("router_grp_w", (D, 4))
    rgb_d = din("router_grp_b", (4,))
    rew_d = din("router_exp_w", (D, 32))
    reb_d = din("router_exp_b", (32,))
    w1_d = din("moe_w1", (NEXP_FULL, D, DE))
    w3_d = din("moe_w3", (NEXP_FULL, D, DE))
    w2_d = din("moe_w2", (NEXP_FULL, DE, D))
    fg_d = din("final_norm_g", (D,))
    cst_d = din("cst", (128, 898))
    poolP_d = din("poolP", (128, 512))
    sel_d = din("sel", (8, 8 * 128))
    out_d = nc.dram_tensor("out", [NB, SEQ, D], F32, kind="ExternalOutput").ap()
    dbg_d = None
    if debug:
        dbg_d = nc.dram_tensor("dbg", [128, debug], F32, kind="ExternalOutput").ap()

    NBC = NB + 1

    with contextlib.ExitStack() as es:
        S = Sched(nc, es)

        uid = [0]

        def sb(es_, name, shape, dt=F32):
            uid[0] += 1
            return es_.enter_context(nc.sbuf_tensor("%s_s%d" % (name, uid[0]), list(shape), dt))

        P = [es.enter_context(nc.psum_tensor("P%d" % i, [128, 512], F32)) for i in range(7)]
        PB = es.enter_context(nc.psum_tensor("PB", [128, 1024], BF16))
        Pb = [Buf(excl=True) for _ in range(7)]
        PBb = Buf(excl=True)

        cst = sb(es, "cst", (128, 898))
        identb = sb(es, "identb", (128, 128), BF16)
        poolP = sb(es, "poolP", (128, 512), BF16)
        sel = sb(es, "sel", (8, 1024))
        vstage = sb(es, "vstage", (68, 128))
        vT = sb(es, "vT", (128, 68))
        gnb = sb(es, "gnb", (128, HV))
        rbb = sb(es, "rbb", (128, 36))
        a2pf = sb(es, "a2pf", (32, 512), BF16)
        a2pb = sb(es, "a2pb", (32, 512), BF16)
        wlr = sb(es, "wlr", (128, KD, 32), BF16)
        rw = sb(es, "rw", (128, KD, 36), BF16)
        pwt = sb(es, "pwt", (128, 4, 128), BF16)
        sct = sb(es, "sct", (128, KD, NBC))
        modT = sb(es, "modT", (128, NBC, 48))
        msb = sb(es, "msb", (NBC, 2, D))
        A1 = sb(es, "A1", (128, NBC, KD))
        A2 = sb(es, "A2", (128, NBC, KD))
        cb = Buf()
        dsc = S.new_dsem()
        dscp = S.new_dsem()
        xid = [S.new_dsem() for _ in range(3)]
        wsem = S.new_dsem()
        wgsem = S.new_dsem()
        w2sem = S.new_dsem()
        dsl = S.new_dsem()
        dbgsem = S.new_dsem()
        wm13 = [S.new_dsem() for _ in range(2)]
        osem = [S.new_dsem() for _ in range(2)]

        ident = cst[:, 0:128]

        def tri(i):
            return cst[:, 128 + i * 128: 256 + i * 128]

        def msk(i):
            return cst[:, 640 + i * 128: 768 + i * 128]

        negcol = cst[:, 896:898]

        a2B = Buf()
        S.op("dve", lambda e: e.memset(a2pf[:], 0.0), w=[a2B])
        S.op("dve", lambda e: e.memset(a2pb[:], 0.0), w=[a2B])
        S.dma("sp", cst[:], cst_d, dsc, w=[cb])
        S.dma("sp", sel[:], sel_d, dsc, w=[cb])
        S.dma("sp", vstage[0:48, :], b_mod_d.rearrange("(j p) -> j p", p=128), dsc, w=[cb])
        S.dma("sp", vstage[48:56, :], n1g_d.rearrange("(j p) -> j p", p=128), dsc, w=[cb])
        S.dma("sp", vstage[56:64, :], n2g_d.rearrange("(j p) -> j p", p=128), dsc, w=[cb])
        S.dma("sp", vstage[64:68, :], psc_d.rearrange("(j p) -> j p", p=128), dsc, w=[cb])
        S.dma("sp", gnb[:], gn_d.rearrange("(o n) -> o n", o=1).to_broadcast([128, HV]), dsc, w=[cb])
        S.dma("sp", rbb[:, 0:4], rgb_d.rearrange("(o n) -> o n", o=1).to_broadcast([128, 4]), dsc, w=[cb])
        S.dma("sp", rbb[:, 4:36], reb_d.rearrange("(o n) -> o n", o=1).to_broadcast([128, 32]), dsc, w=[cb])
        S.dma("sp", sct[:], cT_d.rearrange("(k p) b -> p k b", p=128), dsc, w=[cb])
        S.dma("pool", a2pf[0:16, :], a2f_d, dscp, w=[cb, a2B])
        S.dma("pool", a2pb[16:32, :], a2b_d, dscp, w=[cb, a2B])
        S.dma("pool", poolP[:], poolP_d, dscp, w=[cb])
        S.dma("pool", wlr[:], w_in_d[:, C_LR:C_LR + 32].rearrange("(k p) n -> p k n", p=128), dscp, w=[cb])
        S.dma("pool", rw[:, :, 0:4], rgw_d.rearrange("(k p) n -> p k n", p=128), dscp, w=[cb])
        S.dma("pool", rw[:, :, 4:36], rew_d.rearrange("(k p) n -> p k n", p=128), dscp, w=[cb])
        S.dma("pool", pwt[:], pw_d.rearrange("g c e -> c g e"), dscp, w=[cb])
        S.op("dve", lambda e: e.tensor_copy(out=identb[:], in_=ident), r=[cb], w=[cb])
        S.op("act", lambda e: e.activation(out=sct[:], in_=sct[:], func=AF.Silu), r=[cb], w=[cb])
        S.op("pe", lambda e: e.transpose(P[0][:, 0:68], vstage[:], cst[0:68, 0:68]), r=[cb], w=[Pb[0]])
        S.op("dve", lambda e: e.tensor_copy(out=vT[:], in_=P[0][:, 0:68]), r=[Pb[0]], w=[cb])

        with contextlib.ExitStack() as p0:
            wm = [sb(p0, "wm%d" % i, (128, KD, 512)) for i in range(2)]
            bm5 = sb(p0, "bm5", (NBC, 2, D))
            for ci, ch in enumerate((2, 5)):
                S.dma("sp", bm5[:, ci, :],
                      b_mod_d[ch * D:(ch + 1) * D].rearrange("(o n) -> o n", o=1).to_broadcast([NBC, D]),
                      dsl, w=[cb])
            wmb = bufs(2)
            wmd = [S.new_dsem() for _ in range(2)]
            for cg in range(12):
                i = cg % 2
                S.dma("sp", wm[i][:], w_mod_d[:, cg * 512:(cg + 1) * 512].rearrange("(k p) n -> p k n", p=128),
                      wmd[i], w=[wmb[i]])
                for m in range(4):
                    j = cg * 4 + m
                    for k in range(KD):
                        S.op("pe", lambda e, i=i, m=m, k=k, j=j: e.matmul(
                            P[1][:, j * 8:j * 8 + NBC], lhsT=wm[i][:, k, m * 128:(m + 1) * 128],
                            rhs=sct[:, k, :], start=(k == 0), stop=(k == KD - 1)),
                            r=[wmb[i], cb], w=[Pb[1]])
                if cg // 2 in (2, 5):
                    ci = 0 if cg // 2 == 2 else 1
                    hf = cg % 2
                    for k in range(KD):
                        S.op("pe", lambda e, i=i, k=k: e.matmul(
                            P[2][0:NBC, :], lhsT=sct[:, k, :], rhs=wm[i][:, k, :],
                            start=(k == 0), stop=(k == KD - 1)), r=[wmb[i], cb], w=[Pb[2]])
                    S.op("dve", lambda e, ci=ci, hf=hf: e.tensor_tensor(
                        out=msb[:, ci, hf * 512:(hf + 1) * 512], in0=P[2][0:NBC, :],
                        in1=bm5[:, ci, hf * 512:(hf + 1) * 512], op=ALU.add), r=[Pb[2], cb], w=[cb])
            p1v = P[1][:, 0:384].rearrange("p (j e) -> p j e", e=8)
            for b in range(NBC):
                S.op("dve", lambda e, b=b: e.tensor_tensor(out=modT[:, b, :], in0=p1v[:, :, b], in1=vT[:, 0:48],
                                                           op=ALU.add), r=[Pb[1], cb], w=[cb])
            for b in range(NBC):
                S.op("dve", lambda e, b=b: e.scalar_tensor_tensor(
                    out=A1[:, b, :], in0=modT[:, b, 8:16], scalar=1.0, op0=ALU.add, in1=vT[:, 48:56], op1=ALU.mult),
                    r=[cb], w=[cb])
                S.op("dve", lambda e, b=b: e.scalar_tensor_tensor(
                    out=A2[:, b, :], in0=modT[:, b, 32:40], scalar=1.0, op0=ALU.add, in1=vT[:, 56:64], op1=ALU.mult),
                    r=[cb], w=[cb])
            S.barrier()

        dbg_off = [0]

        def dump(ap_, n, rb, np_=128):
            if dbg_d is None:
                return
            o = dbg_off[0]
            S.dma("pool", dbg_d[0:np_, o:o + n], ap_, dbgsem, r=rb)
            dbg_off[0] = o + n

        def rms_rstd(es_unused, src_ap, width, ss, rs, junk, rb, sb_):
            S.op("act", lambda e: e.activation(out=junk, in_=src_ap, func=AF.Square, accum_out=ss[:, 0:1]),
                 r=rb, w=[sb_])
            S.op("act", lambda e: e.activation(out=rs[:, 0:1], in_=ss[:, 0:1], func=AF.Sqrt, bias=EPS,
                                               scale=1.0 / width), r=[sb_], w=[sb_])
            S.op("dve", lambda e: e.reciprocal(out=rs[:, 0:1], in_=rs[:, 0:1]), r=[sb_], w=[sb_])

        try:
            for b in range(NB):
                with contextlib.ExitStack() as eb:
                    acc = sb(eb, "acc", (128, NXT, D))
                    accb = bufs(NXT)
                    with contextlib.ExitStack() as e13:
                        hT = sb(e13, "hT", (128, KD, TOK), BF16)
                        hTb = bufs(NTT)
                        lrT = sb(e13, "lrT", (32, TOK), BF16)
                        lrb = Buf()
                        with contextlib.ExitStack() as e1:
                            xin = [sb(e1, "xin%d" % i, (128, D)) for i in range(3)]
                            xib = bufs(3)
                            junk = sb(e1, "junk1", (128, D), BF16)
                            st = [sb(e1, "st1_%d" % i, (128, 2)) for i in range(2)]
                            stb = bufs(2)
                            jb = Buf()

                            def load_x(tt):
                                i = tt % 3
                                src = ctx_d[b, tt * 128:(tt + 1) * 128, :] if tt < NCT else \
                                    x_d[b, (tt - NCT) * 128:(tt - NCT + 1) * 128, :]
                                S.dma("sp", xin[i][:], src, xid[i], w=[xib[i]])

                            def p1_body(tt):
                                if tt + 2 < NTT:
                                    load_x(tt + 2)
                                i = tt % 3
                                s_ = st[tt % 2]
                                sbb = stb[tt % 2]
                                mb = NB if tt < NCT else b
                                S.op("act", lambda e: e.activation(
                                    out=junk[:], in_=xin[i][:], func=AF.Square, accum_out=s_[:, 0:1]),
                                    r=[xib[i]], w=[sbb, jb])
                                S.op("act", lambda e: e.activation(
                                    out=s_[:, 1:2], in_=s_[:, 0:1], func=AF.Sqrt, bias=EPS, scale=1.0 / D),
                                    r=[sbb], w=[sbb])
                                S.op("dve", lambda e: e.reciprocal(out=s_[:, 1:2], in_=s_[:, 1:2]),
                                     r=[sbb], w=[sbb])
                                S.op("dve", lambda e: e.tensor_scalar(
                                    out=xin[i][:], in0=xin[i][:], scalar1=s_[:, 1:2], scalar2=None, op0=ALU.mult),
                                    r=[sbb, xib[i]], w=[xib[i]])
                                yield
                                pbase = (tt % 2) * 2
                                for k in range(KD):
                                    bk = pbase + k // 4
                                    S.op("pe", lambda e, k=k, bk=bk: e.transpose(
                                        P[bk][:, (k % 4) * 128:(k % 4 + 1) * 128], xin[i][:, k * 128:(k + 1) * 128], ident),
                                        r=[xib[i], cb], w=[Pb[bk]])
                                yield
                                for k in range(KD):
                                    bk = pbase + k // 4
                                    src = P[bk][:, (k % 4) * 128:(k % 4 + 1) * 128]
                                    dst = hT[:, k, tt * 128:(tt + 1) * 128]
                                    if k // 4 == 0:
                                        S.op("act", lambda e, src=src, dst=dst, k=k: e.activation(
                                            out=dst, in_=src, func=AF.Identity, scale=A1[:, mb, k:k + 1],
                                            bias=modT[:, mb, k:k + 1]), r=[Pb[bk], cb], w=[hTb[tt]])
                                    else:
                                        S.op("dve", lambda e, src=src, dst=dst, k=k: e.tensor_scalar(
                                            out=dst, in0=src, scalar1=A1[:, mb, k:k + 1], scalar2=modT[:, mb, k:k + 1],
                                            op0=ALU.mult, op1=ALU.add), r=[Pb[bk], cb], w=[hTb[tt]])

                            load_x(0)
                            load_x(1)
                            pipeline(p1_body(tt) for tt in range(NTT))
                            S.barrier()
                            if upto == 1:
                                S.dead = True
                        with contextlib.ExitStack() as e23:
                            whd = sb(e23, "whd", (128, KD, 768), BF16)
                            abfb = sb(e23, "abfb", (128, 512))
                            abbb = sb(e23, "abbb", (128, 512))
                            abB = Buf()
                            S.dma("sp", abfb[:], abf_d.rearrange("(o n) -> o n", o=1).to_broadcast([128, 512]), dsl, w=[abB])
                            S.dma("sp", abbb[:], abb_d.rearrange("(o n) -> o n", o=1).to_broadcast([128, 512]), dsl, w=[abB])
                            wgl = sb(e23, "wgl", (128, 2, D), BF16)
                            whb = Buf()
                            qT = sb(e23, "qT", (128, SEQ), BF16)
                            qTb = bufs(4)
                            kT = sb(e23, "kT", (128, TOK), BF16)
                            kTb = bufs(5)
                            ktok = sb(e23, "ktok", (128, NTT, 128), BF16)
                            ktb = bufs(3)
                            vtok = sb(e23, "vtok", (128, NTT, HV), BF16)
                            vtb = bufs(NTT)
                            sg = sb(e23, "sg", (128, NXT, HV), BF16)
                            sgb = bufs(NXT)
                            SBs = sb(e23, "SBs", (128, NXT, HV), BF16)
                            SBb = bufs(NXT)
                            SF = [sb(e23, "SF%d" % i, (128, HV), BF16) for i in range(2)]
                            SFb = bufs(2)
                            Sst = sb(e23, "Sst", (128, HV))
                            Sstb = Buf()
                            NSL = 3
                            NBIG = 2
                            zz = [sb(e23, "zz%d" % p_, (128, 256)) for p_ in range(NBIG)]
                            zzb = bufs(NBIG)
                            EE = [sb(e23, "EE%d" % p_, (128, 386)) for p_ in range(NBIG)]
                            EEb = bufs(NBIG)
                            E2 = [sb(e23, "E2_%d" % p_, (128, 256)) for p_ in range(NBIG)]
                            E2b = bufs(NBIG)
                            NDEC = 6
                            decs = sb(e23, "decs", (128, NDEC))
                            decb = bufs(NDEC)
                            kr = [sb(e23, "kr%d" % p_, (128, 128), BF16) for p_ in range(NSL)]
                            krb = bufs(NSL)
                            abh = sb(e23, "abh", (128, 256))
                            abhb = Buf()
                            qd = [sb(e23, "qd%d" % p_, (128, 2, 128), BF16) for p_ in range(NSL)]
                            qdb = bufs(NSL)
                            ki = [[sb(e23, "ki%d%d" % (p_, d_), (128, 128), BF16) for d_ in range(2)] for p_ in range(NSL)]
                            kib = [bufs(2) for _ in range(NSL)]
                            sT = [sb(e23, "sT%d" % p_, (128, 2, 128), BF16) for p_ in range(NSL)]
                            sTb = bufs(NSL)
                            otmp = [sb(e23, "otmp%d" % p_, (128, HV)) for p_ in range(NBIG)]
                            otb = bufs(NBIG)
                            og = [sb(e23, "og%d" % p_, (128, HV), BF16) for p_ in range(NBIG)]
                            ogb = bufs(NBIG)
                            ogT = [sb(e23, "ogT%d" % p_, (128, 2, 128), BF16) for p_ in range(NBIG)]
                            ogTb = bufs(NBIG)
                            st2 = [sb(e23, "st2_%d" % p_, (128, 2)) for p_ in range(NSL)]
                            st2b = bufs(NSL)
                            junk2 = sb(e23, "junk2", (128, HV), BF16)
                            vglock = Buf()
                            j2b = Buf()
                            nbk = [0]

                            def rot():
                                v_ = nbk[0]
                                nbk[0] = (v_ + 1) % 7
                                return v_

                            def kgrp(tt):
                                return 0 if tt < NCT else 1 + (tt - NCT) // 4

                            tokgroups = [(0, 256)] + [(256 + g * 512, 512) for g in range(4)]

                            wglb = Buf()

                            def load_whd(h_):
                                for (dst0, src0, n_) in ((0, C_Q + h_ * 128, 128), (128, C_K + h_ * 128, 128),
                                                         (256, C_V + h_ * 256, 256), (512, C_G + h_ * 256, 256)):
                                    S.dma("pool", whd[:, :, dst0:dst0 + n_],
                                          w_in_d[:, src0:src0 + n_].rearrange("(k p) n -> p k n", p=128), wsem, w=[whb])

                            def load_wgl(h_):
                                S.dma("pool", wgl[:], wgb_d[h_ * 256:(h_ + 1) * 256, :].rearrange("(c p) n -> p c n", p=128),
                                      wgsem, w=[wglb])

                            for h in range(NHEAD):
                                if h == 0:
                                    load_whd(0)
                                    load_wgl(0)
                                for g in range(4):
                                    bk = rot()
                                    for k in range(KD):
                                        S.op("pe", lambda e, k=k, g=g, bk=bk: e.matmul(
                                            P[bk][:, :], lhsT=whd[:, k, 0:128], rhs=hT[:, k, 256 + g * 512:768 + g * 512],
                                            start=(k == 0), stop=(k == KD - 1)),
                                            r=[whb] + hTb[2 + 4 * g:6 + 4 * g], w=[Pb[bk]])
                                    S.op("act", lambda e, g=g, bk=bk: e.activation(
                                        out=qT[:, g * 512:(g + 1) * 512], in_=P[bk][:, :], func=AF.Copy, scale=HK ** -0.5),
                                        r=[Pb[bk]], w=[qTb[g]])
                                for g, (t0, n_) in enumerate(tokgroups):
                                    bk = rot()
                                    for k in range(KD):
                                        S.op("pe", lambda e, k=k, bk=bk, t0=t0, n_=n_: e.matmul(
                                            P[bk][:, 0:n_], lhsT=whd[:, k, 128:256], rhs=hT[:, k, t0:t0 + n_],
                                            start=(k == 0), stop=(k == KD - 1)),
                                            r=[whb] + hTb[t0 // 128:(t0 + n_) // 128], w=[Pb[bk]])
                                    S.op("dve", lambda e, bk=bk, t0=t0, n_=n_: e.tensor_copy(
                                        out=kT[:, t0:t0 + n_], in_=P[bk][:, 0:n_]), r=[Pb[bk]], w=[kTb[g]])
                                    if h == 0:
                                        bk = rot()
                                        for k in range(KD):
                                            S.op("pe", lambda e, k=k, bk=bk, t0=t0, n_=n_: e.matmul(
                                                P[bk][0:32, 0:n_], lhsT=wlr[:, k, :], rhs=hT[:, k, t0:t0 + n_],
                                                start=(k == 0), stop=(k == KD - 1)),
                                                r=[cb] + hTb[t0 // 128:(t0 + n_) // 128], w=[Pb[bk]])
                                        S.op("act", lambda e, bk=bk, t0=t0, n_=n_: e.activation(
                                            out=lrT[:, t0:t0 + n_], in_=P[bk][0:32, 0:n_], func=AF.Copy),
                                            r=[Pb[bk]], w=[lrb])
                                if upto == 20 and h == 0:
                                    S.dead = True
                                for bi, (tt0, nt_) in enumerate(((0, 8), (8, 8), (16, 2))):
                                    for j in range(nt_):
                                        tt = tt0 + j
                                        S.op("pe", lambda e, j=j, tt=tt: e.transpose(
                                            PB[:, j * 128:(j + 1) * 128], kT[:, tt * 128:(tt + 1) * 128], identb[:]),
                                            r=[kTb[kgrp(tt)], cb], w=[PBb])
                                    S.op("act" if bi % 2 else "dve", (lambda e, tt0=tt0, nt_=nt_: e.activation(
                                        out=ktok[:, tt0:tt0 + nt_, :], in_=PB[:, 0:nt_ * 128].rearrange("p (t d) -> p t d", d=128),
                                        func=AF.Copy)) if bi % 2 else (lambda e, tt0=tt0, nt_=nt_: e.tensor_copy(
                                            out=ktok[:, tt0:tt0 + nt_, :],
                                            in_=PB[:, 0:nt_ * 128].rearrange("p (t d) -> p t d", d=128))),
                                        r=[PBb], w=[ktb[bi]])
                                if upto == 21 and h == 0:
                                    S.dead = True
                                for tt in range(NTT):
                                    bk = rot()
                                    n_ = 256 if tt < NCT else 512
                                    for k in range(KD):
                                        S.op("pe", lambda e, k=k, bk=bk, tt=tt, n_=n_: e.matmul(
                                            P[bk][:, 0:n_], lhsT=hT[:, k, tt * 128:(tt + 1) * 128], rhs=whd[:, k, 256:256 + n_],
                                            start=(k == 0), stop=(k == KD - 1)), r=[whb, hTb[tt]], w=[Pb[bk]])
                                    S.op("dve", lambda e, bk=bk, tt=tt: e.tensor_copy(out=vtok[:, tt, :], in_=P[bk][:, 0:256]),
                                         r=[Pb[bk]], w=[vtb[tt], vglock])
                                    if tt >= NCT:
                                        S.op("act", lambda e, bk=bk, tt=tt: e.activation(
                                            out=sg[:, tt - NCT, :], in_=P[bk][:, 256:512], func=AF.Silu),
                                            r=[Pb[bk]], w=[sgb[tt - NCT], vglock])

                                if h + 1 < NHEAD:
                                    load_whd(h + 1)
                                if upto == 22 and h == 0:
                                    S.dead = True
                                S.op("dve", lambda e: e.tensor_copy(out=abh[:, 0:128], in_=abfb[:, h * 128:(h + 1) * 128]),
                                     r=[abB], w=[abhb])
                                S.op("dve", lambda e: e.tensor_copy(out=abh[:, 128:256], in_=abbb[:, h * 128:(h + 1) * 128]),
                                     r=[abB], w=[abhb])

                                def la_stage(tt, dirs, ix):
                                    lo, hi = dirs[0] * 128, (dirs[-1] + 1) * 128
                                    for di in dirs:
                                        a2p = a2pf if di == 0 else a2pb
                                        S.op("pe", lambda e, di=di, a2p=a2p: e.matmul(
                                            P[0][:, di * 128:(di + 1) * 128], lhsT=lrT[:, tt * 128:(tt + 1) * 128],
                                            rhs=a2p[:, h * 128:(h + 1) * 128], start=True, stop=True),
                                            r=[lrb, cb], w=[Pb[0]])
                                    yield
                                    S.op("dve", lambda e: e.tensor_tensor(out=zz[ix % NBIG][:, lo:hi], in0=P[0][:, lo:hi],
                                                                          in1=abh[:, lo:hi], op=ALU.add),
                                         r=[Pb[0], abhb], w=[zzb[ix % NBIG]])
                                    yield
                                    S.op("act", lambda e: e.activation(out=zz[ix % NBIG][:, lo:hi], in_=zz[ix % NBIG][:, lo:hi],
                                                                       func=AF.Exp, scale=-1.0), r=[zzb[ix % NBIG]], w=[zzb[ix % NBIG]])
                                    S.op("act", lambda e: e.activation(out=zz[ix % NBIG][:, lo:hi], in_=zz[ix % NBIG][:, lo:hi],
                                                                       func=AF.Ln, bias=1.0), r=[zzb[ix % NBIG]], w=[zzb[ix % NBIG]])
                                    yield

                                def rem_tot(di, ix):
                                    trm = tri(1) if di == 0 else tri(3)
                                    sp_ = zz[ix % NBIG][:, di * 128:(di + 1) * 128]
                                    S.op("pe", lambda e: e.matmul(P[1][:, 256:384], lhsT=trm, rhs=sp_,
                                                                  start=True, stop=True), r=[zzb[ix % NBIG], cb], w=[Pb[1]])
                                    S.op("pe", lambda e: e.matmul(P[1][:, 384:386], lhsT=sp_, rhs=negcol,
                                                                  start=True, stop=True), r=[zzb[ix % NBIG], cb], w=[Pb[1]])

                                def state_post(tt, ix, dst, dstb, bk=6, c0=0):
                                    S.op("pe", lambda e: e.matmul(P[bk][:, c0:c0 + HV], lhsT=kr[ix % NSL][:], rhs=vtok[:, tt, :],
                                                                  start=True, stop=True), r=[krb[ix % NSL], vtb[tt]], w=[Pb[bk]])
                                    yield
                                    S.op("dve", lambda e: e.scalar_tensor_tensor(
                                        out=Sst[:], in0=Sst[:], scalar=decs[:, ix % NDEC:ix % NDEC + 1], op0=ALU.mult,
                                        in1=P[bk][:, c0:c0 + HV], op1=ALU.add), r=[Sstb, decb[ix % NDEC], Pb[bk]], w=[Sstb])
                                    if dst is not None:
                                        S.op("dve", lambda e: e.tensor_copy(out=dst, in_=Sst[:]), r=[Sstb], w=[dstb])

                                def st_body(di, tt, ix, dst, dstb):
                                    yield from la_stage(tt, (di,), ix)
                                    rem_tot(di, ix)
                                    yield
                                    S.op("act", lambda e: e.activation(out=EE[ix % NBIG][:, 256:386], in_=P[1][:, 256:386], func=AF.Exp),
                                         r=[Pb[1]], w=[EEb[ix % NBIG]])
                                    yield
                                    S.op("dve", lambda e: e.tensor_copy(out=decs[:, ix % NDEC:ix % NDEC + 1], in_=EE[ix % NBIG][:, 384:385]),
                                         r=[EEb[ix % NBIG]], w=[decb[ix % NDEC]])
                                    S.op("dve", lambda e: e.tensor_tensor(out=kr[ix % NSL][:], in0=ktok[:, tt, :],
                                                                          in1=EE[ix % NBIG][:, 256:384], op=ALU.mult),
                                         r=[ktb[tt // 8], EEb[ix % NBIG]], w=[krb[ix % NSL]])
                                    yield
                                    yield from state_post(tt, ix, dst, dstb)

                                def f_body(n, ix):
                                    tt = n + NCT
                                    cur = n % 2
                                    yield from la_stage(tt, (0, 1), ix)
                                    for di in (0, 1):
                                        tin = tri(0) if di == 0 else tri(2)
                                        S.op("pe", lambda e, di=di, tin=tin: e.matmul(
                                            P[1][:, di * 128:(di + 1) * 128], lhsT=zz[ix % NBIG][:, di * 128:(di + 1) * 128], rhs=tin,
                                            start=True, stop=True), r=[zzb[ix % NBIG], cb], w=[Pb[1]])
                                    rem_tot(0, ix)
                                    yield
                                    S.op("act", lambda e: e.activation(out=EE[ix % NBIG][:, 0:386], in_=P[1][:, 0:386], func=AF.Exp),
                                         r=[Pb[1]], w=[EEb[ix % NBIG]])
                                    S.op("act", lambda e: e.activation(out=E2[ix % NBIG][:, 0:256], in_=P[1][:, 0:256], func=AF.Exp,
                                                                       scale=-1.0), r=[Pb[1]], w=[E2b[ix % NBIG]])
                                    yield
                                    S.op("dve", lambda e: e.tensor_tensor(
                                        out=qd[ix % NSL][:, :, :],
                                        in0=qT[:, n * 128:(n + 1) * 128].unsqueeze(1).to_broadcast([128, 2, 128]),
                                        in1=EE[ix % NBIG][:, 0:256].rearrange("p (d t) -> p d t", t=128),
                                        op=ALU.mult), r=[qTb[n // 4], EEb[ix % NBIG]], w=[qdb[ix % NSL]])
                                    for di in (0, 1):
                                        S.op(POOLENG, lambda e, di=di: e.tensor_tensor(
                                            out=ki[ix % NSL][di][:], in0=kT[:, tt * 128:(tt + 1) * 128],
                                            in1=E2[ix % NBIG][:, di * 128:(di + 1) * 128],
                                            op=ALU.mult), r=[kTb[kgrp(tt)], E2b[ix % NBIG]], w=[kib[ix % NSL][di]])
                                    if n < NXT - 1:
                                        S.op("dve", lambda e: e.tensor_copy(out=decs[:, ix % NDEC:ix % NDEC + 1], in_=EE[ix % NBIG][:, 384:385]),
                                         r=[EEb[ix % NBIG]], w=[decb[ix % NDEC]])
                                    S.op("dve", lambda e: e.tensor_tensor(out=kr[ix % NSL][:], in0=ktok[:, tt, :],
                                                                              in1=EE[ix % NBIG][:, 256:384], op=ALU.mult),
                                             r=[ktb[tt // 8], EEb[ix % NBIG]], w=[krb[ix % NSL]])
                                    yield
                                    for di in (0, 1):
                                        S.op("pe", lambda e, di=di: e.matmul(
                                            P[2][:, di * 128:(di + 1) * 128], lhsT=ki[ix % NSL][di][:], rhs=qd[ix % NSL][:, di, :],
                                            start=True, stop=True), r=[kib[ix % NSL][di], qdb[ix % NSL]], w=[Pb[2]])
                                    yield
                                    S.op("dve", lambda e: e.tensor_tensor(
                                        out=sT[ix % NSL][:, :, :], in0=P[2][:, 0:256].rearrange("p (d t) -> p d t", t=128),
                                        in1=cst[:, 640:896].rearrange("p (d t) -> p d t", t=128),
                                        op=ALU.mult), r=[Pb[2], cb], w=[sTb[ix % NSL]])
                                    yield
                                    ops_o = ((sT[ix % NSL][:, 0, :], vtok[:, tt, :], [sTb[ix % NSL], vtb[tt]]),
                                             (qd[ix % NSL][:, 0, :], SF[cur][:], [qdb[ix % NSL], SFb[cur]]),
                                             (sT[ix % NSL][:, 1, :], vtok[:, tt, :], [sTb[ix % NSL], vtb[tt]]),
                                             (qd[ix % NSL][:, 1, :], SBs[:, n, :], [qdb[ix % NSL], SBb[n]]))
                                    ob = 3 if ix % 2 == 0 else 6
                                    for oi, (l_, r_, rb_) in enumerate(ops_o):
                                        S.op("pe", lambda e, l_=l_, r_=r_, oi=oi: e.matmul(
                                            P[ob][:, 0:HV], lhsT=l_, rhs=r_, start=(oi == 0), stop=(oi == 3)),
                                            r=rb_, w=[Pb[ob]])
                                    if n < NXT - 1:
                                        spost = state_post(tt, ix, SF[1 - cur][:], SFb[1 - cur], bk=ob, c0=256)
                                        next(spost)
                                    else:
                                        spost = iter(())
                                    yield
                                    S.op("act", lambda e: e.activation(out=junk2[:], in_=P[ob][:, 0:HV], func=AF.Square,
                                                                       accum_out=st2[ix % NSL][:, 0:1]),
                                         r=[Pb[ob]], w=[st2b[ix % NSL], j2b])
                                    S.op("act", lambda e: e.activation(out=st2[ix % NSL][:, 1:2], in_=st2[ix % NSL][:, 0:1], func=AF.Ln,
                                                                       bias=EPS, scale=1.0 / HV), r=[st2b[ix % NSL]], w=[st2b[ix % NSL]])
                                    S.op("act", lambda e: e.activation(out=st2[ix % NSL][:, 1:2], in_=st2[ix % NSL][:, 1:2], func=AF.Exp,
                                                                       scale=-0.5), r=[st2b[ix % NSL]], w=[st2b[ix % NSL]])
                                    next(spost, None)
                                    yield
                                    S.op("dve", lambda e: e.scalar_tensor_tensor(
                                        out=otmp[ix % NBIG][:], in0=P[ob][:, 0:HV], scalar=st2[ix % NSL][:, 1:2], op0=ALU.mult,
                                        in1=gnb[:], op1=ALU.mult), r=[Pb[ob], st2b[ix % NSL], cb], w=[otb[ix % NBIG]])
                                    S.op(POOLENG, lambda e: e.tensor_tensor(out=og[ix % NBIG][:], in0=otmp[ix % NBIG][:], in1=sg[:, n, :],
                                                                           op=ALU.mult), r=[otb[ix % NBIG], sgb[n]], w=[ogb[ix % NBIG]])
                                    yield
                                    for c in range(2):
                                        S.op("pe", lambda e, c=c: e.transpose(PB[:, c * 128:(c + 1) * 128],
                                                                              og[ix % NBIG][:, c * 128:(c + 1) * 128], identb[:]),
                                             r=[ogb[ix % NBIG], cb], w=[PBb])
                                    yield
                                    S.op("act", lambda e: e.activation(
                                        out=ogT[ix % NBIG][:, :, :], in_=PB[:, 0:256].rearrange("p (c t) -> p c t", t=128),
                                        func=AF.Copy), r=[PBb], w=[ogTb[ix % NBIG]])
                                    yield
                                    for hf in range(2):
                                        for c in range(2):
                                            S.op("pe", lambda e, hf=hf, c=c: e.matmul(
                                                P[4 + hf][:, :], lhsT=ogT[ix % NBIG][:, c, :], rhs=wgl[:, c, hf * 512:(hf + 1) * 512],
                                                start=(c == 0), stop=(c == 1)), r=[ogTb[ix % NBIG], wglb], w=[Pb[4 + hf]])
                                    yield
                                    for hf in range(2):
                                        dst = acc[:, n, hf * 512:(hf + 1) * 512]
                                        if h == 0:
                                            S.op("act", lambda e, hf=hf, dst=dst: e.activation(out=dst, in_=P[4 + hf][:, :],
                                                                                               func=AF.Copy),
                                                 r=[Pb[4 + hf]], w=[accb[n]])
                                        else:
                                            S.op("dve", lambda e, hf=hf, dst=dst: e.tensor_tensor(
                                                out=dst, in0=dst, in1=P[4 + hf][:, :], op=ALU.add),
                                                r=[Pb[4 + hf], accb[n]], w=[accb[n]])

                                S.op("dve", lambda e: e.memset(Sst[:], 0.0), w=[Sstb])
                                gens = [st_body(1, 1, 0, None, None), st_body(1, 0, 1, SBs[:, NXT - 1, :], SBb[NXT - 1])]
                                for n in range(NXT - 1, 0, -1):
                                    gens.append(st_body(1, n + NCT, len(gens), SBs[:, n - 1, :], SBb[n - 1]))
                                pipeline(gens)
                                if upto == 23 and h == 0:
                                    S.dead = True
                                S.op("dve", lambda e: e.memset(Sst[:], 0.0), w=[Sstb])
                                gens = [st_body(0, 0, 0, None, None), st_body(0, 1, 1, SF[0][:], SFb[0])]
                                for n in range(NXT):
                                    gens.append(f_body(n, len(gens)))
                                pipeline(gens)
                                if h + 1 < NHEAD:
                                    load_wgl(h + 1)
                            S.barrier()
                            if upto == 2:
                                S.dead = True
                        S.barrier()
                    h2T = sb(eb, "h2T", (128, NXT, KD, 128), BF16)
                    h2b = bufs(NXT)
                    cw = sb(eb, "cw", (128, NXT, 32))
                    cwb = bufs(NXT)

                    def bcast_row(dst, ci):
                        for hf in range(2):
                            S.op("pe", lambda e, hf=hf: e.matmul(
                                P[hf][:, :], lhsT=sel[0:NBC, b * 128:(b + 1) * 128], rhs=msb[:, ci, hf * 512:(hf + 1) * 512],
                                start=True, stop=True), r=[cb], w=[Pb[hf]])
                            S.op("act", lambda e, hf=hf: e.activation(out=dst[:, hf * 512:(hf + 1) * 512], in_=P[hf][:, :],
                                                                      func=AF.Copy), r=[Pb[hf]], w=[cb])

                    with contextlib.ExitStack() as e4:
                        wu = sb(e4, "wu", (128, KD, 512), BF16)
                        wgt = sb(e4, "wgt", (128, KD, 2048), BF16)
                        wpb = sb(e4, "wpb", (128, 4, D), BF16)
                        w4b = Buf()
                        S.dma("pool", wu[:], w_in_d[:, C_POOL:C_POOL + 512].rearrange("(k p) n -> p k n", p=128), wsem, w=[w4b])
                        for j in range(4):
                            S.dma("pool", wgt[:, :, j * 512:(j + 1) * 512],
                                  w_in_d[:, C_GATES + j * 512:C_GATES + (j + 1) * 512].rearrange("(k p) n -> p k n", p=128),
                                  wsem, w=[w4b])
                        S.dma("pool", wpb[:], wpb_d.rearrange("(g p) n -> p g n", p=128), wsem, w=[w4b])
                        xin = [sb(e4, "xin4_%d" % i, (128, D)) for i in range(2)]
                        xib = bufs(2)
                        xs = sb(e4, "xs4", (128, D))
                        xsb = Buf()
                        junk = sb(e4, "junk4", (128, D), BF16)
                        jb = Buf()
                        st = [sb(e4, "st4_%d" % i, (128, 2)) for i in range(2)]
                        stb = bufs(2)
                        hTt = [sb(e4, "hTt%d" % i, (128, KD, 128), BF16) for i in range(2)]
                        hTtb = bufs(2)
                        u_sb = sb(e4, "u_sb", (128, 512), BF16)
                        ub = Buf()
                        pT = sb(e4, "pT", (128, 512), BF16)
                        pTb = Buf()
                        y1T = sb(e4, "y1T", (128, 4, 128), BF16)
                        y1b = Buf()
                        sgt = [sb(e4, "sgt%d" % i, (128, 512)) for i in range(2)]
                        sgtb = bufs(2)
                        t1 = sb(e4, "t1", (128, D))
                        t1b = bufs(2)
                        tmp = [sb(e4, "tmp4_%d" % i, (128, 512)) for i in range(2)]
                        tmpb = bufs(2)

                        def load_x4(n):
                            S.dma("sp", xin[n % 2][:], x_d[b, n * 128:(n + 1) * 128, :], xid[n % 2], w=[xib[n % 2]])

                        def p4a_body(n):
                            if n + 1 < NXT:
                                load_x4(n + 1)
                            i = n % 2
                            par = n % 2
                            S.op("act", lambda e: e.activation(out=junk[:], in_=xin[i][:], func=AF.Square,
                                                               accum_out=st[par][:, 0:1]), r=[xib[i]], w=[stb[par], jb])
                            S.op("act", lambda e: e.activation(out=st[par][:, 1:2], in_=st[par][:, 0:1], func=AF.Sqrt,
                                                               bias=EPS, scale=1.0 / D), r=[stb[par]], w=[stb[par]])
                            S.op("dve", lambda e: e.reciprocal(out=st[par][:, 1:2], in_=st[par][:, 1:2]),
                                 r=[stb[par]], w=[stb[par]])
                            S.op("act", lambda e: e.activation(out=xs[:], in_=xin[i][:], func=AF.Copy, scale=st[par][:, 1:2]),
                                 r=[stb[par], xib[i]], w=[xsb])
                            for k in range(KD):
                                S.op("pe", lambda e, k=k: e.transpose(P[k // 4][:, (k % 4) * 128:(k % 4 + 1) * 128],
                                                                      xs[:, k * 128:(k + 1) * 128], ident),
                                     r=[xsb, cb], w=[Pb[k // 4]])
                            for k in range(KD):
                                src = P[k // 4][:, (k % 4) * 128:(k % 4 + 1) * 128]
                                dst = hTt[par][:, k, :]
                                if k // 4 == 0:
                                    S.op("act", lambda e, src=src, dst=dst, k=k: e.activation(
                                        out=dst, in_=src, func=AF.Identity, scale=A1[:, b, k:k + 1], bias=modT[:, b, k:k + 1]),
                                        r=[Pb[k // 4], cb], w=[hTtb[par]])
                                else:
                                    S.op("dve", lambda e, src=src, dst=dst, k=k: e.tensor_scalar(
                                        out=dst, in0=src, scalar1=A1[:, b, k:k + 1], scalar2=modT[:, b, k:k + 1],
                                        op0=ALU.mult, op1=ALU.add), r=[Pb[k // 4], cb], w=[hTtb[par]])
                            yield
                            for k in range(KD):
                                S.op("pe", lambda e, k=k: e.matmul(P[2][:, :], lhsT=hTt[par][:, k, :], rhs=wu[:, k, :],
                                                                   start=(k == 0), stop=(k == KD - 1)),
                                     r=[hTtb[par], w4b], w=[Pb[2]])
                            S.op("dve", lambda e: e.tensor_copy(out=u_sb[:], in_=P[2][:, :]), r=[Pb[2]], w=[ub])
                            for g in range(4):
                                S.op("pe", lambda e, g=g: e.matmul(P[3][:, g * 128:(g + 1) * 128],
                                                                   lhsT=u_sb[:, g * 128:(g + 1) * 128],
                                                                   rhs=poolP[:, g * 128:(g + 1) * 128], start=True, stop=True),
                                     r=[ub, cb], w=[Pb[3]])
                            S.op("act", lambda e: e.activation(out=pT[:], in_=P[3][:, :], func=AF.Copy), r=[Pb[3]], w=[pTb])
                            for g in range(4):
                                S.op("pe", lambda e, g=g: e.matmul(P[2][:, g * 128:(g + 1) * 128], lhsT=pwt[:, g, :],
                                                                   rhs=pT[:, g * 128:(g + 1) * 128], start=True, stop=True),
                                     r=[pTb, cb], w=[Pb[2]])
                            for g in range(4):
                                S.op("act", lambda e, g=g: e.activation(out=y1T[:, g, :], in_=P[2][:, g * 128:(g + 1) * 128],
                                                                        func=AF.Copy, scale=vT[:, 64 + g:65 + g]),
                                     r=[Pb[2], cb], w=[y1b])
                            for hf in range(2):
                                for g in range(4):
                                    S.op("pe", lambda e, hf=hf, g=g: e.matmul(
                                        P[4 + hf][:, :], lhsT=y1T[:, g, :], rhs=wpb[:, g, hf * 512:(hf + 1) * 512],
                                        start=(g == 0), stop=(g == 3)), r=[y1b, w4b], w=[Pb[4 + hf]])
                            yield
                            for j in range(4):
                                bk = 6 if j % 2 == 0 else 3
                                hf = j % 2
                                for k in range(KD):
                                    S.op("pe", lambda e, k=k, j=j, bk=bk: e.matmul(
                                        P[bk][:, :], lhsT=hTt[par][:, k, :], rhs=wgt[:, k, j * 512:(j + 1) * 512],
                                        start=(k == 0), stop=(k == KD - 1)), r=[hTtb[par], w4b], w=[Pb[bk]])
                                S.op("act", lambda e, bk=bk, hf=hf: e.activation(out=sgt[hf][:], in_=P[bk][:, :],
                                                                                 func=AF.Sigmoid),
                                     r=[Pb[bk]], w=[sgtb[hf]])
                                if j < 2:
                                    S.op("dve", lambda e, hf=hf: e.tensor_tensor(
                                        out=t1[:, hf * 512:(hf + 1) * 512], in0=sgt[hf][:], in1=P[4 + hf][:, :], op=ALU.mult),
                                        r=[sgtb[hf], Pb[4 + hf]], w=[t1b[hf]])
                                else:
                                    S.op("dve", lambda e, hf=hf: e.tensor_tensor(
                                        out=tmp[hf][:], in0=sgt[hf][:], in1=acc[:, n, hf * 512:(hf + 1) * 512], op=ALU.mult),
                                        r=[sgtb[hf], accb[n]], w=[tmpb[hf]])
                                    S.op("dve", lambda e, hf=hf: e.tensor_tensor(
                                        out=h2T[:, n, hf * 4:(hf + 1) * 4, :], in0=tmp[hf][:].rearrange("p (k t) -> p k t", t=128),
                                        in1=t1[:, hf * 512:(hf + 1) * 512].rearrange("p (k t) -> p k t", t=128), op=ALU.add),
                                        r=[tmpb[hf], t1b[hf]], w=[h2b[n]])

                        load_x4(0)
                        pipeline(p4a_body(n) for n in range(NXT))
                        S.barrier()
                        if upto == 3:
                            S.dead = True

                    with contextlib.ExitStack() as e4:
                        wo = sb(e4, "wo", (128, KD, D), BF16)
                        w4h = bufs(2)
                        for hf_, ds_ in ((0, wsem), (1, wgsem)):
                            S.dma("pool", wo[:, :, hf_ * 512:(hf_ + 1) * 512],
                                  wo_d[:, hf_ * 512:(hf_ + 1) * 512].rearrange("(k p) n -> p k n", p=128), ds_, w=[w4h[hf_]])
                        g1b = sb(e4, "g1b", (128, D))
                        bcast_row(g1b, 0)
                        xin = [sb(e4, "xin5_%d" % i, (128, D)) for i in range(2)]
                        xib = bufs(2)
                        mTt = [sb(e4, "mTt%d" % i, (128, KD, 128), BF16) for i in range(2)]
                        mTb = bufs(2)
                        tmp2 = sb(e4, "tmp2", (128, D))
                        tmp2b = bufs(2)
                        xs = sb(e4, "xs5", (128, D))
                        xsb = Buf()
                        junk = sb(e4, "junk5", (128, D), BF16)
                        jb = Buf()
                        st = [sb(e4, "st5_%d" % i, (128, 2)) for i in range(2)]
                        stb = bufs(2)
                        lgall = sb(e4, "lgall", (128, NXT, 36))
                        lgb = Buf()
                        rs_ = sb(e4, "rs_", (128, 10, NXT))
                        rg_ = sb(e4, "rg_", (128, 3, NXT, 4))
                        re_ = sb(e4, "re_", (128, 5, NXT, 32))
                        rtb_ = Buf()

                        def load_x5(n):
                            S.dma("sp", xin[n % 2][:], x_d[b, n * 128:(n + 1) * 128, :], xid[n % 2], w=[xib[n % 2]])

                        def p4b_body(n):
                            i = n % 2
                            par = n % 2
                            mv = h2T[:, n, :, :]
                            for k in range(KD):
                                S.op("pe", lambda e, k=k: e.transpose(PB[:, k * 128:(k + 1) * 128], mv[:, k, :], identb[:]),
                                     r=[h2b[n], cb], w=[PBb])
                            yield
                            S.op("act", lambda e: e.activation(out=mTt[par][:, :, :],
                                                               in_=PB[:, :].rearrange("p (k t) -> p k t", t=128), func=AF.Copy),
                                 r=[PBb], w=[mTb[par]])
                            yield
                            if n + 1 < NXT:
                                load_x5(n + 1)
                            for hf in range(2):
                                for k in range(KD):
                                    S.op("pe", lambda e, hf=hf, k=k: e.matmul(
                                        P[hf][:, :], lhsT=mTt[par][:, k, :], rhs=wo[:, k, hf * 512:(hf + 1) * 512],
                                        start=(k == 0), stop=(k == KD - 1)), r=[mTb[par], w4h[hf]], w=[Pb[hf]])
                            yield
                            for hf in range(2):
                                sl = slice(hf * 512, (hf + 1) * 512)
                                S.op("dve", lambda e, hf=hf, sl=sl: e.tensor_tensor(out=tmp2[:, sl], in0=P[hf][:, :],
                                                                                    in1=g1b[:, sl], op=ALU.mult),
                                     r=[Pb[hf], cb], w=[tmp2b[hf]])
                                S.op("dve", lambda e, sl=sl: e.tensor_tensor(out=acc[:, n, sl], in0=tmp2[:, sl],
                                                                              in1=xin[i][:, sl], op=ALU.add),
                                     r=[tmp2b[hf], xib[i]], w=[accb[n]])
                            yield
                            S.op("act", lambda e: e.activation(out=junk[:], in_=acc[:, n, :], func=AF.Square,
                                                               accum_out=st[par][:, 0:1]), r=[accb[n]], w=[stb[par], jb])
                            S.op("act", lambda e: e.activation(out=st[par][:, 1:2], in_=st[par][:, 0:1], func=AF.Ln,
                                                               bias=EPS, scale=1.0 / D), r=[stb[par]], w=[stb[par]])
                            S.op("act", lambda e: e.activation(out=st[par][:, 1:2], in_=st[par][:, 1:2], func=AF.Exp,
                                                               scale=-0.5), r=[stb[par]], w=[stb[par]])
                            S.op("act", lambda e: e.activation(out=xs[:], in_=acc[:, n, :], func=AF.Copy,
                                                               scale=st[par][:, 1:2]), r=[stb[par], accb[n]], w=[xsb])
                            yield
                            for k in range(KD):
                                S.op("pe", lambda e, k=k: e.transpose(P[2 + k // 4][:, (k % 4) * 128:(k % 4 + 1) * 128],
                                                                      xs[:, k * 128:(k + 1) * 128], ident),
                                     r=[xsb, cb], w=[Pb[2 + k // 4]])
                            yield
                            for k in range(KD):
                                src = P[2 + k // 4][:, (k % 4) * 128:(k % 4 + 1) * 128]
                                dst = h2T[:, n, k, :]
                                if k // 4 == 0:
                                    S.op("act", lambda e, src=src, dst=dst, k=k: e.activation(
                                        out=dst, in_=src, func=AF.Identity, scale=A2[:, b, k:k + 1],
                                        bias=modT[:, b, 24 + k:25 + k]), r=[Pb[2 + k // 4], cb], w=[h2b[n]])
                                else:
                                    S.op("dve", lambda e, src=src, dst=dst, k=k: e.tensor_scalar(
                                        out=dst, in0=src, scalar1=A2[:, b, k:k + 1], scalar2=modT[:, b, 24 + k:25 + k],
                                        op0=ALU.mult, op1=ALU.add), r=[Pb[2 + k // 4], cb], w=[h2b[n]])
                            yield
                            for k in range(KD):
                                S.op("pe", lambda e, k=k: e.matmul(P[4][:, 0:36], lhsT=h2T[:, n, k, :], rhs=rw[:, k, :],
                                                                   start=(k == 0), stop=(k == KD - 1)),
                                     r=[h2b[n], cb], w=[Pb[4]])
                            yield
                            S.op("dve", lambda e: e.tensor_tensor(out=lgall[:, n, :], in0=P[4][:, 0:36], in1=rbb[:], op=ALU.add),
                                 r=[Pb[4], cb], w=[lgb])

                        load_x5(0)
                        pipeline(p4b_body(n) for n in range(NXT))
                        T_ = NXT
                        X = mybir.AxisListType.X
                        gl = lgall[:, :, 0:4]
                        el4 = lgall[:, :, 4:36].rearrange("p t (g e) -> p t g e", e=8)
                        gmax, ngs, pg, m1, m2, dd, e2, den, w1_, w2_ = (rs_[:, q, :] for q in range(10))
                        gm, pen, ge = (rg_[:, q, :, :] for q in range(3))
                        em, oh1, em2, oh2, cwt = (re_[:, q, :, :] for q in range(5))

                        def bc(ap2, width):
                            return ap2.unsqueeze(2).to_broadcast([128, T_, width])

                        def dv(fn):
                            S.op("dve", fn, r=[lgb, rtb_], w=[rtb_])

                        dv(lambda e: e.tensor_reduce(out=gmax, in_=gl, axis=X, op=ALU.max))
                        dv(lambda e: e.tensor_tensor(out=gm, in0=gl, in1=bc(gmax, 4), op=ALU.is_equal))
                        dv(lambda e: e.tensor_tensor(out=ge, in0=gl, in1=bc(gmax, 4), op=ALU.subtract))
                        S.op("act", lambda e: e.activation(out=ge, in_=ge, func=AF.Exp), r=[rtb_], w=[rtb_])
                        dv(lambda e: e.tensor_reduce(out=ngs, in_=ge, axis=X, op=ALU.add))
                        dv(lambda e: e.reciprocal(out=pg, in_=ngs))
                        dv(lambda e: e.tensor_scalar(out=pen, in0=gm, scalar1=-1.0, scalar2=1e30, op0=ALU.add, op1=ALU.mult))
                        em4 = em.rearrange("p t (g e) -> p t g e", e=8)
                        dv(lambda e: e.tensor_tensor(out=em4, in0=el4,
                                                     in1=pen.unsqueeze(3).to_broadcast([128, T_, 4, 8]), op=ALU.add))
                        dv(lambda e: e.tensor_reduce(out=m1, in_=em, axis=X, op=ALU.max))
                        dv(lambda e: e.tensor_tensor(out=oh1, in0=em, in1=bc(m1, 32), op=ALU.is_equal))
                        dv(lambda e: e.scalar_tensor_tensor(out=em2.rearrange("p t e -> p (t e)"),
                                                            in0=oh1.rearrange("p t e -> p (t e)"), scalar=-1e30, op0=ALU.mult,
                                                            in1=em.rearrange("p t e -> p (t e)"), op1=ALU.add))
                        dv(lambda e: e.tensor_reduce(out=m2, in_=em2, axis=X, op=ALU.max))
                        dv(lambda e: e.tensor_tensor(out=oh2, in0=em2, in1=bc(m2, 32), op=ALU.is_equal))
                        dv(lambda e: e.tensor_tensor(out=dd, in0=m2, in1=m1, op=ALU.subtract))
                        S.op("act", lambda e: e.activation(out=e2, in_=dd, func=AF.Exp), r=[rtb_], w=[rtb_])
                        dv(lambda e: e.tensor_scalar(out=den, in0=e2, scalar1=1.0, scalar2=None, op0=ALU.add))
                        dv(lambda e: e.reciprocal(out=den, in_=den))
                        dv(lambda e: e.tensor_tensor(out=w1_, in0=den, in1=pg, op=ALU.mult))
                        dv(lambda e: e.tensor_tensor(out=w2_, in0=w1_, in1=e2, op=ALU.mult))
                        dv(lambda e: e.tensor_tensor(out=cwt, in0=oh1, in1=bc(w1_, 32), op=ALU.mult))
                        dv(lambda e: e.tensor_tensor(out=oh2, in0=oh2, in1=bc(w2_, 32), op=ALU.mult))
                        S.op("dve", lambda e: e.tensor_tensor(out=cw[:, :, :], in0=cwt, in1=oh2, op=ALU.add),
                             r=[rtb_], w=cwb)
                        if debug:
                            for n in range(2):
                                dump(acc[:, n, :], D, accb)
                            dump(cw[:, 0, :], 32, cwb)
                            dump(cw[:, 1, :], 32, cwb)
                        S.barrier()
                        if upto == 4:
                            S.dead = True

                    with contextlib.ExitStack() as e5:
                        g2b = sb(e5, "g2b", (128, D))
                        bcast_row(g2b, 1)
                        w1b = [sb(e5, "w1b%d" % i, (128, KD, DE), BF16) for i in range(2)]
                        w3b = [sb(e5, "w3b%d" % i, (128, KD, DE), BF16) for i in range(2)]
                        w2s = sb(e5, "w2s", (128, 4, D))
                        w2b = [sb(e5, "w2b%d" % i, (128, 4, D), BF16) for i in range(2)]
                        wb13 = bufs(2)
                        w2sb = Buf()
                        w2bb = bufs(2)
                        sa = [sb(e5, "sa%d" % i, (128, 512)) for i in range(2)]
                        sab = bufs(2)
                        hid = [sb(e5, "hid%d" % i, (128, 4, 512), BF16) for i in range(2)]
                        hidb = bufs(2)

                        def load_w(e_):
                            sl_ = e_ % 2
                            S.dma("pool", w1b[sl_][:], w1_d[e_].rearrange("(k p) f -> p k f", p=128), wm13[sl_], w=[wb13[sl_]])
                            S.dma("pool", w3b[sl_][:], w3_d[e_].rearrange("(k p) f -> p k f", p=128), wm13[sl_], w=[wb13[sl_]])
                            S.dma("sp", w2s[:], w2_d[e_].rearrange("(c p) n -> p c n", p=128), w2sem, w=[w2sb])
                            for c in range(4):
                                S.op("dve", lambda e, c=c, sl_=sl_: e.tensor_tensor(out=w2b[sl_][:, c, :], in0=w2s[:, c, :],
                                                                                     in1=g2b[:], op=ALU.mult),
                                     r=[w2sb, cb], w=[w2bb[sl_]])

                        items = [(e_, tg) for e_ in range(NEXP) for tg in range(4)]

                        def emit_ab(idx):
                            e_, tg = items[idx]
                            sl_ = e_ % 2
                            hp = idx % 2
                            for f in range(4):
                                ba, bb_ = (0, 1) if f % 2 == 0 else (2, 3)
                                sp_ = f % 2
                                for (wsrc, bk) in ((w1b, ba), (w3b, bb_)):
                                    for k in range(KD):
                                        S.op("pe", lambda e, wsrc=wsrc, bk=bk, k=k, f=f: e.matmul(
                                            P[bk][:, :].rearrange("p (t d) -> p t d", d=128),
                                            lhsT=wsrc[sl_][:, k, f * 128:(f + 1) * 128],
                                            rhs=h2T[:, tg * 4:(tg + 1) * 4, k, :], start=(k == 0), stop=(k == KD - 1)),
                                            r=[wb13[sl_]] + h2b[tg * 4:(tg + 1) * 4], w=[Pb[bk]])
                                S.op("act", lambda e, ba=ba, sp_=sp_: e.activation(out=sa[sp_][:], in_=P[ba][:, :],
                                                                                   func=AF.Silu),
                                     r=[Pb[ba]], w=[sab[sp_]])
                                S.op("dve", lambda e, bb_=bb_, sp_=sp_, f=f: e.tensor_tensor(
                                    out=hid[hp][:, f, :], in0=sa[sp_][:], in1=P[bb_][:, :], op=ALU.mult),
                                    r=[sab[sp_], Pb[bb_]], w=[hidb[hp]])

                        def emit_w2(idx):
                            e_, tg = items[idx]
                            sl_ = e_ % 2
                            hp = idx % 2
                            for j in range(4):
                                n = tg * 4 + j
                                for hf in range(2):
                                    bk = 4 + (j * 2 + hf) % 3
                                    for f in range(4):
                                        S.op("pe", lambda e, bk=bk, f=f, j=j, hf=hf: e.matmul(
                                            P[bk][:, :], lhsT=hid[hp][:, f, j * 128:(j + 1) * 128],
                                            rhs=w2b[sl_][:, f, hf * 512:(hf + 1) * 512], start=(f == 0), stop=(f == 3)),
                                            r=[hidb[hp], w2bb[sl_]], w=[Pb[bk]])
                                    dst = acc[:, n, hf * 512:(hf + 1) * 512]
                                    S.op("dve", lambda e, bk=bk, dst=dst, n=n: e.scalar_tensor_tensor(
                                        out=dst, in0=P[bk][:, :], scalar=cw[:, n, e_:e_ + 1], op0=ALU.mult, in1=dst,
                                        op1=ALU.add), r=[Pb[bk], cwb[n], accb[n]], w=[accb[n]])

                        load_w(0)
                        if NEXP > 1:
                            load_w(1)
                        emit_ab(0)
                        for idx in range(len(items)):
                            if idx + 1 < len(items):
                                emit_ab(idx + 1)
                            emit_w2(idx)
                            e_, tg = items[idx]
                            if tg == 3 and e_ + 2 < NEXP:
                                load_w(e_ + 2)
                        S.barrier()
                        if upto == 5:
                            S.dead = True

                    with contextlib.ExitStack() as e6:
                        fgb = sb(e6, "fgb", (128, D))
                        fb = Buf()
                        S.dma("sp", fgb[:], fg_d.rearrange("(o n) -> o n", o=1).to_broadcast([128, D]), dsl, w=[fb])
                        ot = [sb(e6, "ot%d" % i, (128, D)) for i in range(2)]
                        otb_ = bufs(2)
                        junk = sb(e6, "junk6", (128, D), BF16)
                        jb = Buf()
                        st = [sb(e6, "st6_%d" % i, (128, 2)) for i in range(2)]
                        stb = bufs(2)
                        for n in range(NXT):
                            par = n % 2
                            S.op("act", lambda e: e.activation(out=junk[:], in_=acc[:, n, :], func=AF.Square,
                                                               accum_out=st[par][:, 0:1]), r=[accb[n]], w=[stb[par], jb])
                            S.op("act", lambda e: e.activation(out=st[par][:, 1:2], in_=st[par][:, 0:1], func=AF.Sqrt,
                                                               bias=EPS, scale=1.0 / D), r=[stb[par]], w=[stb[par]])
                            S.op("dve", lambda e: e.reciprocal(out=st[par][:, 1:2], in_=st[par][:, 1:2]),
                                 r=[stb[par]], w=[stb[par]])
                            S.op("dve", lambda e: e.scalar_tensor_tensor(out=ot[par][:], in0=acc[:, n, :],
                                                                         scalar=st[par][:, 1:2], op0=ALU.mult, in1=fgb[:],
                                                                         op1=ALU.mult), r=[accb[n], stb[par], fb], w=[otb_[par]])
                            S.dma("sp", out_d[b, n * 128:(n + 1) * 128, :], ot[par][:], osem[par], r=[otb_[par]])
                        S.barrier()
                        if upto == 6:
                            S.dead = True
                    S.barrier()
        except _Stop:
            pass
        S.dead = False
        S.barrier()
    return nc


_CONSTS = None


def _consts():
    global _CONSTS
    if _CONSTS is None:
        j = np.arange(128)[:, None]
        i = np.arange(128)[None, :]
        cst = np.zeros((128, 898), np.float32)
        cst[:, 0:128] = np.eye(128, dtype=np.float32)
        s = -1.0 / 16.0
        cst[:, 128:256] = (j <= i) * s
        cst[:, 256:384] = (j > i) * s
        cst[:, 384:512] = (j >= i) * s
        cst[:, 512:640] = (j < i) * s
        cst[:, 640:768] = (j <= i)
        cst[:, 768:896] = (j >= i)
        cst[:, 896:898] = s
        poolP = np.zeros((128, 4, 128), np.float32)
        for gi, w in enumerate((2, 4, 8, 16)):
            for t in range(128):
                r0 = (t // 64) * 64
                lo = max(t - w // 2, r0)
                hi = min(t + w // 2, r0 + 64)
                poolP[lo:hi, gi, t] = 1.0 / (hi - lo)
                poolP[t, gi, t] -= 1.0
        sel = np.zeros((8, 8, 128), np.float32)
        for r in range(8):
            sel[r, r, :] = 1.0
        _CONSTS = (cst, poolP.reshape(128, 512), sel.reshape(8, 1024))
    return _CONSTS


_PER_LAYER = ("w_mod", "b_mod", "norm1_g", "norm2_g", "w_in", "gla_a2_f", "gla_ab_f", "gla_a2_b", "gla_ab_b",
              "gla_onorm_g", "pool_w", "pool_scale", "w_pool_br", "w_gla_br", "w_o", "router_grp_w", "router_grp_b",
              "router_exp_w", "router_exp_b", "moe_w1", "moe_w3", "moe_w2")


def kernel(**inputs):
    NB = 4
    x = np.asarray(inputs["x"], np.float32)
    c = np.asarray(inputs["c"], np.float32)
    ctx = np.asarray(inputs["ctx"], np.float32)
    c_ctx = np.asarray(inputs["c_ctx"], np.float32)
    cst, poolP, sel = _consts()
    shared = {k: np.ascontiguousarray(np.asarray(inputs[k], np.float32)[0]) for k in _PER_LAYER}
    shared["final_norm_g"] = np.ascontiguousarray(np.asarray(inputs["final_norm_g"], np.float32))
    shared["cst"] = cst
    shared["poolP"] = poolP
    shared["sel"] = sel
    in_maps = []
    for core in range(NCORES):
        b0 = core * NB
        m = dict(shared)
        m["x"] = np.ascontiguousarray(x[b0:b0 + NB])
        m["ctx"] = np.ascontiguousarray(ctx[b0:b0 + NB])
        m["cT"] = np.ascontiguousarray(np.concatenate([c[b0:b0 + NB], c_ctx[None, :]], axis=0).T)
        in_maps.append(m)
    nc = build_program(NB=NB)
    res = run_bass_kernel_spmd(nc, in_maps, core_ids=list(range(NCORES)))
    return np.concatenate([np.asarray(r["out"], np.float32) for r in res.results], axis=0)
```
